# Optimizing a Trainium2 kernel written in Bass

```python
import math
import jax, jax.numpy as jnp
from jax import lax
import numpy as np

D_MODEL = 1024
BATCH = 2
SEQ = 8192
DEPTH = 2

GRID_W = 64
CTX_LEN = 256

N_FOURIER_GROUPS = 4
FOURIER_GROUP = 128
FOURIER_WIDTH = N_FOURIER_GROUPS * FOURIER_GROUP

N_Q_HEADS = 8
N_KV_HEADS = 2
HEAD_DIM = 64
GROUP = N_Q_HEADS // N_KV_HEADS
ATTN_WIDTH = N_Q_HEADS * HEAD_DIM
KV_WIDTH = N_KV_HEADS * HEAD_DIM
WINDOW = 128
BLOCK = 128
ROPE_BASE = 10000.0

OFF_Q = FOURIER_WIDTH
OFF_K = OFF_Q + ATTN_WIDTH
OFF_V = OFF_K + KV_WIDTH
OFF_G = OFF_V + KV_WIDTH
IN_WIDTH = OFF_G + 2 * D_MODEL

D_FF_DENSE = 2816
N_EXPERTS = 8
TOP_K = 2
D_FF_EXPERT = 3584
N_DENSE = (DEPTH + 1) // 2
N_MOE = DEPTH // 2

N_MOD = 6
EPS = 1e-6
NEG = -1e30

kernel_name = "hybrid_fourier_swa_moe_dit"


def rmsnorm(x, g):
    xf = x.astype(jnp.float32)
    r = lax.rsqrt(jnp.mean(xf * xf, axis=-1, keepdims=True) + EPS)
    return (xf * r).astype(x.dtype) * g


def adaln(cvec, w_mod, b_mod, n_chunks):
    m = jax.nn.silu(cvec) @ w_mod[:, :n_chunks * D_MODEL] + b_mod[:n_chunks * D_MODEL]
    return jnp.split(m, n_chunks, axis=-1)


def modulate(h, shift, scale):
    return h * (1.0 + scale) + shift


def axial_rope_tables(n_rows):
    rows = jnp.broadcast_to(jnp.arange(n_rows)[:, None], (n_rows, GRID_W)).reshape(-1).astype(jnp.float32)
    cols = jnp.broadcast_to(jnp.arange(GRID_W)[None, :], (n_rows, GRID_W)).reshape(-1).astype(jnp.float32)
    n_freq = HEAD_DIM // 4
    inv = ROPE_BASE ** (-jnp.arange(n_freq, dtype=jnp.float32) / n_freq)
    ar = rows[:, None] * inv
    ac = cols[:, None] * inv
    return jnp.cos(ar), jnp.sin(ar), jnp.cos(ac), jnp.sin(ac)


def rope_rotate(x, cos, sin):
    h = x.shape[-1] // 2
    x1, x2 = x[..., :h], x[..., h:]
    c = cos[None, :, None, :].astype(x.dtype)
    s = sin[None, :, None, :].astype(x.dtype)
    return jnp.concatenate([x1 * c - x2 * s, x1 * s + x2 * c], axis=-1)


def axial_rope(x, tabs):
    cr, sr, cc, sc = tabs
    a = HEAD_DIM // 2
    return jnp.concatenate([rope_rotate(x[..., :a], cr, sr), rope_rotate(x[..., a:], cc, sc)], axis=-1)


def fourier_mix(u):
    b, t, _ = u.shape
    ug = u.astype(jnp.float32).reshape(b, t, N_FOURIER_GROUPS, FOURIER_GROUP)
    f = jnp.fft.fft2(ug, axes=(1, 3), norm="ortho").real
    return f.reshape(b, t, FOURIER_WIDTH).astype(u.dtype)


def sink_logits(sink, lead_shape, q_len):
    s = sink.astype(jnp.float32).reshape(N_KV_HEADS, GROUP)[:, :, None, None]
    return jnp.broadcast_to(s, lead_shape + (N_KV_HEADS, GROUP, q_len, 1))


def latent_attention(q, k, v, kc, vc, sink):
    b, t = q.shape[:2]
    nb = t // BLOCK
    scale = HEAD_DIM ** -0.5
    qb = q.reshape(b, nb, BLOCK, N_KV_HEADS, GROUP, HEAD_DIM)

    def band(a):
        ap = jnp.pad(a, ((0, 0), (BLOCK, BLOCK), (0, 0), (0, 0)))
        return jnp.concatenate(
            [ap[:, j * BLOCK:j * BLOCK + t].reshape(b, nb, BLOCK, N_KV_HEADS, HEAD_DIM) for j in range(3)],
            axis=2)

    kb, vb = band(k), band(v)
    s_loc = jnp.einsum('bnqhgd,bnkhd->bnhgqk', qb, kb).astype(jnp.float32) * scale
    qpos = jnp.arange(BLOCK)
    kpos = jnp.arange(3 * BLOCK) - BLOCK
    kabs = (jnp.arange(nb) * BLOCK)[:, None] + kpos[None, :]
    in_win = jnp.abs(kpos[None, :] - qpos[:, None]) <= WINDOW
    valid = in_win[None] & ((kabs >= 0) & (kabs < t))[:, None, :]
    s_loc = jnp.where(valid[None, :, None, None], s_loc, NEG)
    s_ctx = jnp.einsum('bnqhgd,bchd->bnhgqc', qb, kc).astype(jnp.float32) * scale
    s = jnp.concatenate([s_loc, s_ctx, sink_logits(sink, (b, nb), BLOCK)], axis=-1)
    p = jax.nn.softmax(s, axis=-1).astype(v.dtype)
    n_loc = 3 * BLOCK
    n_ctx = kc.shape[1]
    o = (jnp.einsum('bnhgqk,bnkhd->bnqhgd', p[..., :n_loc], vb)
         + jnp.einsum('bnhgqc,bchd->bnqhgd', p[..., n_loc:n_loc + n_ctx], vc))
    return o.reshape(b, t, ATTN_WIDTH)


def context_attention(qc, kc, vc, sink):
    b, c = qc.shape[:2]
    scale = HEAD_DIM ** -0.5
    q = qc.reshape(b, c, N_KV_HEADS, GROUP, HEAD_DIM)
    s = jnp.einsum('bqhgd,bkhd->bhgqk', q, kc).astype(jnp.float32) * scale
    s = jnp.concatenate([s, sink_logits(sink, (b,), c)], axis=-1)
    p = jax.nn.softmax(s, axis=-1).astype(vc.dtype)
    o = jnp.einsum('bhgqk,bkhd->bqhgd', p[..., :c], vc)
    return o.reshape(b, c, ATTN_WIDTH)


def split_heads(a, n_heads):
    return a.reshape(a.shape[:-1] + (n_heads, HEAD_DIM))


def merge_branches(u, o_attn, g_f, g_a, w_f, w_a, w_o):
    y = jax.nn.sigmoid(g_f) * (fourier_mix(u) @ w_f) + jax.nn.sigmoid(g_a) * (o_attn @ w_a)
    return y @ w_o


def swiglu(h, w_g, w_u, w_d):
    return (jax.nn.silu(h @ w_g) * (h @ w_u)) @ w_d


def moe_swiglu(h, w_router, w_g, w_u, w_d):
    shp = h.shape
    t = h.reshape(-1, shp[-1])
    logits = (t @ w_router).astype(jnp.float32)
    topv, topi = lax.top_k(logits, TOP_K)
    wts = jax.nn.softmax(topv, axis=-1)
    combine = jnp.sum(wts[..., None] * jax.nn.one_hot(topi, N_EXPERTS, dtype=jnp.float32), axis=1).astype(h.dtype)
    y = jnp.zeros_like(t)
    for e in range(N_EXPERTS):
        y = y + combine[:, e:e + 1] * swiglu(t, w_g[e], w_u[e], w_d[e])
    return y.reshape(shp)


def setup_inputs(seed: int = 0) -> dict:
    key = jax.random.key(seed)
    ks = jax.random.split(key, 21)
    f32 = jnp.float32
    nrm = lambda k, shp, s: jax.random.normal(k, shp, f32) * s
    D = D_MODEL
    return {
        "x": nrm(ks[0], (BATCH, SEQ, D), 1.0),
        "c": nrm(ks[1], (BATCH, D), 1.0),
        "ctx": nrm(ks[2], (BATCH, CTX_LEN, D), 1.0),
        "c_ctx": nrm(ks[3], (D,), 1.0),
        "w_mod": nrm(ks[4], (DEPTH, D, N_MOD * D), 0.5 * D ** -0.5),
        "b_mod": nrm(ks[5], (DEPTH, N_MOD * D), 0.02),
        "norm1_g": 1.0 + nrm(ks[6], (DEPTH, D), 0.02),
        "norm2_g": 1.0 + nrm(ks[7], (DEPTH, D), 0.02),
        "w_in": nrm(ks[8], (DEPTH, D, IN_WIDTH), D ** -0.5),
        "sink": nrm(ks[9], (DEPTH, N_Q_HEADS), 0.5),
        "w_fourier": nrm(ks[10], (DEPTH, FOURIER_WIDTH, D), FOURIER_WIDTH ** -0.5),
        "w_attn": nrm(ks[11], (DEPTH, ATTN_WIDTH, D), ATTN_WIDTH ** -0.5),
        "w_out": nrm(ks[12], (DEPTH, D, D), D ** -0.5),
        "w_gate_d": nrm(ks[13], (N_DENSE, D, D_FF_DENSE), D ** -0.5),
        "w_up_d": nrm(ks[14], (N_DENSE, D, D_FF_DENSE), D ** -0.5),
        "w_down_d": nrm(ks[15], (N_DENSE, D_FF_DENSE, D), D_FF_DENSE ** -0.5),
        "w_router": nrm(ks[16], (N_MOE, D, N_EXPERTS), D ** -0.5),
        "w_gate_e": nrm(ks[17], (N_MOE, N_EXPERTS, D, D_FF_EXPERT), D ** -0.5),
        "w_up_e": nrm(ks[18], (N_MOE, N_EXPERTS, D, D_FF_EXPERT), D ** -0.5),
        "w_down_e": nrm(ks[19], (N_MOE, N_EXPERTS, D_FF_EXPERT, D), D_FF_EXPERT ** -0.5),
        "final_g": 1.0 + nrm(ks[20], (D,), 0.02),
    }


def reference(x, c, ctx, c_ctx, w_mod, b_mod, norm1_g, norm2_g, w_in, sink, w_fourier, w_attn, w_out,
              w_gate_d, w_up_d, w_down_d, w_router, w_gate_e, w_up_e, w_down_e, final_g):
    xl, xc = x, ctx
    b, t = xl.shape[:2]
    n_rows = t // GRID_W
    tabs = axial_rope_tables(n_rows)
    c_lat = c[:, None, :]
    c_cx = c_ctx[None, None, :]
    for l in range(DEPTH):
        last = l == DEPTH - 1
        sh1, sc1, ga1, sh2, sc2, ga2 = adaln(c_lat, w_mod[l], b_mod[l], N_MOD)
        hl = modulate(rmsnorm(xl, norm1_g[l]), sh1, sc1)
        pl = hl @ w_in[l]
        q = axial_rope(split_heads(pl[..., OFF_Q:OFF_K], N_Q_HEADS), tabs)
        k = axial_rope(split_heads(pl[..., OFF_K:OFF_V], N_KV_HEADS), tabs)
        v = split_heads(pl[..., OFF_V:OFF_G], N_KV_HEADS)
        if last:
            csh1, csc1 = adaln(c_cx, w_mod[l], b_mod[l], 2)
            hc = modulate(rmsnorm(xc, norm1_g[l]), csh1, csc1)
            pkv = hc @ w_in[l][:, OFF_K:OFF_G]
            kc = split_heads(pkv[..., :KV_WIDTH], N_KV_HEADS)
            vc = split_heads(pkv[..., KV_WIDTH:], N_KV_HEADS)
        else:
            csh1, csc1, cga1, csh2, csc2, cga2 = adaln(c_cx, w_mod[l], b_mod[l], N_MOD)
            hc = modulate(rmsnorm(xc, norm1_g[l]), csh1, csc1)
            pc = hc @ w_in[l]
            qc = split_heads(pc[..., OFF_Q:OFF_K], N_Q_HEADS)
            kc = split_heads(pc[..., OFF_K:OFF_V], N_KV_HEADS)
            vc = split_heads(pc[..., OFF_V:OFF_G], N_KV_HEADS)
            oc = context_attention(qc, kc, vc, sink[l])
            yc = merge_branches(pc[..., :OFF_Q], oc, pc[..., OFF_G:OFF_G + D_MODEL], pc[..., OFF_G + D_MODEL:],
                                w_fourier[l], w_attn[l], w_out[l])
        ol = latent_attention(q, k, v, kc, vc, sink[l])
        yl = merge_branches(pl[..., :OFF_Q], ol, pl[..., OFF_G:OFF_G + D_MODEL], pl[..., OFF_G + D_MODEL:],
                            w_fourier[l], w_attn[l], w_out[l])
        xl = xl + ga1 * yl
        hl2 = modulate(rmsnorm(xl, norm2_g[l]), sh2, sc2)
        if l % 2 == 0:
            i = l // 2
            ffn = lambda h: swiglu(h, w_gate_d[i], w_up_d[i], w_down_d[i])
        else:
            i = l // 2
            ffn = lambda h: moe_swiglu(h, w_router[i], w_gate_e[i], w_up_e[i], w_down_e[i])
        xl = xl + ga2 * ffn(hl2)
        if not last:
            xc = xc + cga1 * yc
            hc2 = modulate(rmsnorm(xc, norm2_g[l]), csh2, csc2)
            xc = xc + cga2 * ffn(hc2)
    return rmsnorm(xl, final_g)
```

```python
import os
from contextlib import ExitStack
import numpy as np
import ml_dtypes
import concourse.bass as bass
import concourse.mybir as mybir
from concourse.bass_utils import run_bass_kernel_spmd

F32 = mybir.dt.float32
BF16 = mybir.dt.bfloat16
AF = mybir.ActivationFunctionType
ALU = mybir.AluOpType
AX = mybir.AxisListType
NPBF = ml_dtypes.bfloat16

D = 1024
T = 8192
NT = 2048
CT = 256
NCEXT = 4224
O_U, O_K, O_V, O_Q, O_G = 0, 512, 1024, 1152, 2176
DFF = 2816
DFE = 3584
NDMASEM = 8
STOP = os.environ.get("MK_STOP", "")


class Buf:
    __slots__ = ("name", "w", "r", "phase")

    def __init__(self, name, phase=False):
        self.name = name
        self.w = None
        self.r = {}
        self.phase = phase


class Prog:
    COMPUTE = ("pe", "act", "dve", "pool")

    def __init__(self, nc, sigset=None):
        self.nc = nc
        self.dry = sigset is None
        self.sigset = sigset if sigset is not None else set()
        self.need = set()
        self.n = 0
        self.meta = []
        self.cnt = {e: 0 for e in self.COMPUTE}
        self.sigval = {}
        self.waited = {}
        self.dma_no = {"sp": 0, "pool": 0, "act": 0}
        self.PH = Buf("PHASE")
        self.eng = {"pe": nc.tensor, "act": nc.scalar, "dve": nc.vector, "pool": nc.gpsimd, "sp": nc.sync}
        self.sems = {}
        self.ncc = 0

    def alloc_sems(self, stack):
        for e in self.COMPUTE:
            self.sems[e] = stack.enter_context(self.nc.semaphore("s_" + e))
        for q in ("sp", "pool"):
            for i in range(NDMASEM):
                self.sems[(q, i)] = stack.enter_context(self.nc.semaphore("d_%s%d" % (q, i)))
        for i in range(8):
            self.sems[("cc", i)] = stack.enter_context(self.nc.semaphore("cc%d" % i))

    def _wait(self, eng, semkey, val):
        k = (eng, semkey)
        if self.waited.get(k, 0) >= val:
            return
        self.waited[k] = val
        self.eng[eng].wait_ge(self.sems[semkey], val)

    skip = False
    limit = int(os.environ.get("MK_LIMIT", "0")) or None
    final = False

    def op(self, eng, fn, reads=(), writes=(), dma=False, cc=False):
        if self.skip:
            return None
        if self.limit is not None and self.n >= self.limit and not self.final:
            return None
        if os.environ.get("MK_TRACE") and self.dry:
            import inspect
            fr = inspect.currentframe().f_back
            print("OP", self.n, eng, fr.f_lineno, "dma" if dma else ("cc" if cc else ""))
        idx = self.n
        self.n += 1
        reads = list(reads)
        writes = list(writes)
        if any(b.phase for b in reads) or any(b.phase for b in writes):
            if self.PH not in writes:
                reads.append(self.PH)
        deps = set()
        for b in reads:
            if b.w is not None:
                deps.add(b.w)
        for b in writes:
            if b.w is not None:
                deps.add(b.w)
            deps.update(b.r.values())
        deps.discard(idx)
        async_op = dma or cc
        rkey = ("a", idx) if async_op else eng
        for b in writes:
            b.w = idx
            b.r = {}
        for b in reads:
            if b not in writes:
                b.r[rkey] = idx
        self.meta.append((eng, async_op))
        real = []
        for dpt in deps:
            deng, dasync = self.meta[dpt]
            if (not dasync) and deng == eng and (not async_op) and eng == "pe":
                continue
            real.append(dpt)
        if self.dry:
            for dpt in real:
                self.need.add(dpt)
            return idx
        for dpt in sorted(real):
            semkey, val = self.sigval[dpt]
            self._wait(eng, semkey, val)
        if dma:
            m = self.dma_no[eng]
            self.dma_no[eng] = m + 1
            semkey = (eng, m % NDMASEM)
            if m >= NDMASEM:
                self._wait(eng, semkey, 16 * (m // NDMASEM))
            ins = fn()
            ins.then_inc(self.sems[semkey], 16)
            self.sigval[idx] = (semkey, 16 * (m // NDMASEM + 1))
        elif cc:
            semkey = ("cc", self.ncc)
            self.ncc += 1
            ins = fn()
            ins.then_inc(self.sems[semkey])
            self.sigval[idx] = (semkey, 1)
        else:
            ins = fn()
            if idx in self.sigset:
                self.cnt[eng] += 1
                ins.then_inc(self.sems[eng], 1)
                self.sigval[idx] = (eng, self.cnt[eng])
        return idx

    def barrier(self):
        self.op("dve", lambda: self.nc.vector.engine_nop(), writes=[self.PH])


def build(nc, P):
    declared = set()
    nc._mk_declared = declared

    def din(name, shape, dt=F32):
        declared.add(name)
        return nc.dram_tensor(name, list(shape), dt, kind="ExternalInput")

    x_d = din("x", [NT, D])
    ctx_d = din("ctx", [CT, D])
    cvec_d = din("cvec", [128, 16])
    wmod_d = din("w_mod", [2, D, 6 * D])
    bmod_d = din("bmod", [128, 2 * 96])
    n1g_d = din("n1g", [128, 32])
    n2g_d = din("n2g", [128, 32])
    fing_d = din("fing", [128, 8])
    win_d = din("w_in_ext", [2, D, NCEXT])
    sink_d = din("sinkb", [128, 16])
    wf_d = din("w_fourier", [2, 512, D])
    wa_d = din("w_attn", [2, 512, D])
    wo_d = din("w_out", [2, D, D])
    need_d = STOP in ("", "ffn0", "p1_1", "pa_1", "pb0_1", "mix1", "ffn1")
    need_e = STOP in ("", "ffn1")
    wgd_d = wud_d = wdd_d = wr_d = wge_d = wue_d = wde_d = None
    if need_d:
        wgd_d = din("w_gate_d", [D, DFF])
        wud_d = din("w_up_d", [D, DFF])
        wdd_d = din("w_down_d", [DFF, D])
    if need_e:
        wr_d = din("w_router", [D, 8])
        wge_d = din("w_gate_e", [8, D, DFE])
        wue_d = din("w_up_e", [8, D, DFE])
        wde_d = din("w_down_e", [8, DFE, D])
    ident_d = din("ident", [128, 128])
    ones_d = din("ones1024", [128, 128], BF16)
    cos_d = din("ropecos", [128, NT], BF16)
    sin_d = din("ropesin", [128, NT], BF16)
    masks_d = din("masks", [128, 4 * 512], BF16)
    sel_d = din("sel", [128, 8])
    c128_d = din("c128", [128, 256], BF16)
    tab23_d = din("tab23", [128, 128 * 32], BF16)
    s1_d = nc.dram_tensor("s1_scratch", [2, 128, 64, 512], BF16)
    B_s1 = Buf("s1")
    tabcc_d = din("tabcc", [CT, CT], BF16)
    tabsc_d = din("tabsc", [CT, CT], BF16)
    ccsc_d = din("ccsc", [128, 256], BF16)
    esel_d = din("esel", [8, 8 * 128], BF16)
    out_d = nc.dram_tensor("out", [NT, D], F32, kind="ExternalOutput")

    u_own = [nc.dram_tensor("u_own%d" % k, [1024, 512], BF16) for k in range(2)]
    u_all = [nc.dram_tensor("u_all%d" % k, [4096, 512], BF16) for k in range(2)]
    u_ctx = nc.dram_tensor("u_ctx", [CT, 512], BF16)
    halo_in = nc.dram_tensor("halo_in", [128, 1024], BF16)
    halo_all = nc.dram_tensor("halo_all", [512, 1024], BF16)
    B_uown, B_uall, B_uctx, B_hin, B_hall = Buf("uown"), Buf("uall"), Buf("uctx"), Buf("hin"), Buf("hall")

    with ExitStack() as st:
        block = st.enter_context(nc.Block())
        P.alloc_sems(st)

        sbn = [0]

        def SB(stack, name, shape, dt, phase=True):
            sbn[0] += 1
            return stack.enter_context(nc.sbuf_tensor("sb%d_%s" % (sbn[0], name), shape, dt)), Buf(name, phase)

        ps = [st.enter_context(nc.psum_tensor("ps%d" % i, [128, 512], F32)) for i in range(8)]
        psB = [Buf("ps%d" % i) for i in range(8)]
        psrr = [0]

        pslist = [list(range(8))]

        def nps():
            i = pslist[0][psrr[0] % len(pslist[0])]
            psrr[0] += 1
            return i

        XT, _ = SB(st, "XT", [128, 8, NT], F32, False)
        XB = {(c, g): Buf("XT%d_%d" % (c, g)) for c in range(8) for g in range(4)}
        XcT, _ = SB(st, "XcT", [128, 8, CT], F32, False)
        XcB = {(c, 0): Buf("XcT%d" % c) for c in range(8)}
        mod, _ = SB(st, "mod", [128, 2, 48, 2], F32, False)
        g1m, _ = SB(st, "g1m", [128, 2, 8, 2], F32, False)
        g2m, _ = SB(st, "g2m", [128, 2, 8, 2], F32, False)
        B_modl = [Buf("mod0"), Buf("mod1")]
        B_g1l = [Buf("g1m0"), Buf("g1m1")]
        B_g2l = [Buf("g2m0"), Buf("g2m1")]
        ident, B_ident = SB(st, "ident", [128, 128], F32, False)
        ones, B_ones = SB(st, "ones", [128, 128], BF16, False)
        epsb, B_eps = SB(st, "epsb", [128, 1], F32, False)
        esink, B_esink = SB(st, "esink", [128, 16], F32, False)
        fing, B_fing = SB(st, "fing", [128, 8], F32, False)
        masks, B_masks = SB(st, "masks", [128, 4, 512], BF16, False)
        sel, B_sel = SB(st, "sel", [128, 8], F32, False)
        ccsc, B_ccsc = SB(st, "ccsc", [128, 256], BF16, False)
        c128, B_c128 = SB(st, "c128", [128, 256], BF16, False)

        cv, B_cv = SB(st, "cv", [128, 16], F32, False)
        scb, B_scb = SB(st, "scb", [128, 8, 2], BF16, False)
        bm, B_bm = SB(st, "bm", [128, 2, 96], F32, False)
        ng1, B_ng1 = SB(st, "ng1", [128, 32], F32, False)
        ng2, B_ng2 = SB(st, "ng2", [128, 32], F32, False)
        tmpm, B_tmpm = SB(st, "tmpm", [128, 16], F32, False)
        LAT = dict(name="lat", res=XT, rb=XB, n=NT, s=0, tgs=[(g, g * 512, 512) for g in range(4)])
        CTX = dict(name="ctx", res=XcT, rb=XcB, n=CT, s=1, tgs=[(0, 0, CT)])

        def mm(o, lhsT, rhs, start, stop, reads, pw):
            P.op("pe", lambda: nc.tensor.matmul(o(), lhsT=lhsT(), rhs=rhs(), start=start, stop=stop),
                 reads=reads, writes=[pw])

        def stop_pt(tag):
            if STOP == tag:
                P.skip = True

        @block.sync
        def _(sync):
            def ld(dst, B, src):
                P.op("sp", lambda: nc.sync.dma_start(out=dst(), in_=src()), writes=[B], dma=True)
            ld(lambda: ident[:], B_ident, lambda: ident_d[:, :])
            ld(lambda: ones[:], B_ones, lambda: ones_d[:, :])
            ld(lambda: esink[:], B_esink, lambda: sink_d[:, :])
            ld(lambda: fing[:], B_fing, lambda: fing_d[:, :])
            ld(lambda: masks[:], B_masks, lambda: masks_d.ap().rearrange("p (m n) -> p m n", m=4))
            ld(lambda: sel[:], B_sel, lambda: sel_d[:, :])
            ld(lambda: ccsc[:], B_ccsc, lambda: ccsc_d[:, :])
            ld(lambda: c128[:], B_c128, lambda: c128_d[:, :])
            P.op("dve", lambda: nc.vector.memset(epsb[:], 1e-6), writes=[B_eps])
            P.op("act", lambda: nc.scalar.activation(out=esink[:], in_=esink[:], func=AF.Exp),
                 reads=[B_esink], writes=[B_esink])

            def mods_piece(l, piece, wmb, pi):
                wt, wb = wmb
                P.op("pool", lambda: nc.gpsimd.dma_start(
                    out=wt[:], in_=wmod_d[l, :, piece * 512:(piece + 1) * 512].rearrange("(kc p) n -> p kc n", p=128)),
                    writes=[wb], dma=True)
                for sub in range(4):
                    ch = piece * 4 + sub
                    for kc in range(8):
                        mm(lambda ch=ch: ps[pi][:, ch * 2:ch * 2 + 2],
                           lambda kc=kc, sub=sub: wt[:, kc, sub * 128:(sub + 1) * 128],
                           lambda kc=kc: scb[:, kc, :], kc == 0, kc == 7, [wb, B_scb], psB[pi])

            def mods_fin(l, pi):
                P.op("dve", lambda: nc.vector.tensor_tensor(
                    out=mod[:, l].rearrange("p c s -> p (c s)"), in0=ps[pi][:, 0:96], in1=bm[:, l, :], op=ALU.add),
                    reads=[psB[pi], B_bm], writes=[B_modl[l]])
                for (gm, Bg, ng, Bn, o) in ((g1m, B_g1l, ng1, B_ng1, 8), (g2m, B_g2l, ng2, B_ng2, 32)):
                    P.op("dve", lambda o=o: nc.vector.tensor_scalar(
                        out=tmpm[:], in0=mod[:, l, o:o + 8, :].rearrange("p c s -> p (c s)"), scalar1=1.0, scalar2=None, op0=ALU.add),
                        reads=[B_modl[l]], writes=[B_tmpm])
                    P.op("dve", lambda gm=gm, ng=ng: nc.vector.tensor_tensor(
                        out=gm[:, l].rearrange("p c s -> p (c s)"), in0=tmpm[:], in1=ng[:, l * 16:(l + 1) * 16], op=ALU.mult),
                        reads=[B_tmpm, Bn], writes=[Bg[l]])

            with ExitStack() as ph:
                xs = [SB(ph, "xs%d" % i, [128, D], F32) for i in range(2)]

                def load_T(src, nblk, res, rb):
                    for blk in range(nblk):
                        xt, xb = xs[blk % 2]
                        P.op("sp", lambda xt=xt, blk=blk: nc.sync.dma_start(out=xt[:], in_=src[blk * 128:(blk + 1) * 128, :]),
                             writes=[xb], dma=True)
                        for half in range(2):
                            pi = nps()
                            for c in range(4):
                                cc = half * 4 + c
                                P.op("pe", lambda xt=xt, pi=pi, c=c, cc=cc: nc.tensor.transpose(
                                    out=ps[pi][:, c * 128:(c + 1) * 128], in_=xt[:, cc * 128:(cc + 1) * 128], identity=ident[:]),
                                    reads=[xb, B_ident], writes=[psB[pi]])
                            g = (blk * 128) // 512
                            P.op("dve", lambda pi=pi, half=half, blk=blk: nc.vector.tensor_copy(
                                out=res[:, half * 4:(half + 1) * 4, blk * 128:(blk + 1) * 128],
                                in_=ps[pi][:].rearrange("p (c t) -> p c t", c=4)),
                                reads=[psB[pi]], writes=[rb[(half * 4 + c, g)] for c in range(4)])
                load_T(x_d, 16, XT, XB)
                load_T(ctx_d, 2, XcT, XcB)
                stop_pt("load")

                wm = [SB(ph, "wm%d" % i, [128, 8, 512], BF16) for i in range(2)]
                ld(lambda: cv[:], B_cv, lambda: cvec_d[:, :])
                ld(lambda: bm[:], B_bm, lambda: bmod_d.ap().rearrange("p (l n) -> p l n", l=2))
                ld(lambda: ng1[:], B_ng1, lambda: n1g_d[:, :])
                ld(lambda: ng2[:], B_ng2, lambda: n2g_d[:, :])
                P.op("act", lambda: nc.scalar.activation(out=cv[:], in_=cv[:], func=AF.Silu), reads=[B_cv], writes=[B_cv])
                P.op("dve", lambda: nc.vector.tensor_copy(out=scb[:], in_=cv[:].rearrange("p (k s) -> p k s", s=2)),
                     reads=[B_cv], writes=[B_scb])
                mpi0 = nps()
                for piece in range(12):
                    mods_piece(0, piece, wm[piece % 2], mpi0)
                mods_fin(0, mpi0)
                P.barrier()
                stop_pt("mods")

            def norm_tile(ph_bufs, ts, g, off, n, gm_ap, sh_ap, hT, hB, extra_reads, hout=None, sqb=None):
                _sq, _B_sq, rstd, B_rstd, tt = ph_bufs
                if hout is None:
                    hout = lambda c: hT[:, c, :n]
                sq, B_sq = sqb if sqb is not None else (hT, hB)
                res, rb = ts["res"], ts["rb"]
                for c in range(8):
                    P.op("act", lambda c=c: nc.scalar.activation(out=sq[:, c, :n], in_=res[:, c, off:off + n], func=AF.Square),
                         reads=[rb[(c, g)]], writes=[B_sq])
                pi = nps()
                for c in range(8):
                    mm(lambda: ps[pi][:, :n], lambda: ones[:], lambda c=c: sq[:, c, :n], c == 0, c == 7, [B_ones, B_sq], psB[pi])
                P.op("act", lambda: nc.scalar.activation(out=rstd[:, :n], in_=ps[pi][:, :n], func=AF.Sqrt, bias=epsb[:, 0:1], scale=1.0),
                     reads=[psB[pi], B_eps], writes=[B_rstd])
                P.op("dve", lambda: nc.vector.reciprocal(out=rstd[:, :n], in_=rstd[:, :n]), reads=[B_rstd], writes=[B_rstd])
                for c in range(8):
                    t, tb = tt[c % 2]
                    if sh_ap is None:
                        P.op("dve", lambda c=c: nc.vector.scalar_tensor_tensor(
                            out=hout(c), in0=res[:, c, off:off + n], scalar=gm_ap(c), in1=rstd[:, :n], op0=ALU.mult, op1=ALU.mult),
                            reads=[rb[(c, g)], B_rstd] + extra_reads, writes=[hB])
                    else:
                        P.op("dve", lambda c=c, t=t: nc.vector.scalar_tensor_tensor(
                            out=t[:, :n], in0=res[:, c, off:off + n], scalar=gm_ap(c), in1=rstd[:, :n], op0=ALU.mult, op1=ALU.mult),
                            reads=[rb[(c, g)], B_rstd] + extra_reads, writes=[tb])
                        P.op("act", lambda c=c, t=t: nc.scalar.activation(
                            out=hout(c), in_=t[:, :n], func=AF.Identity, bias=sh_ap(c), scale=1.0),
                            reads=[tb] + extra_reads, writes=[hB])

            def norm_bufs(ph):
                sq, B_sq = None, None
                rstd, B_rstd = SB(ph, "rstd", [128, 512], F32)
                tt = [SB(ph, "nt%d" % i, [128, 512], F32) for i in range(2)]
                return (sq, B_sq, rstd, B_rstd, tt)

            def wload(wt, wb, src):
                P.op("pool", lambda: nc.gpsimd.dma_start(out=wt(), in_=src()), writes=[wb], dma=True)

            for l in range(2):
                last = (l == 1)
                with ExitStack() as lay:
                    OT, B_OT = SB(lay, "OT", [128, 4, NT], BF16)
                    OcT, B_OcT = SB(lay, "OcT", [128, 4, CT], BF16)
                    FT, B_FT = SB(lay, "FT", [128, 4, NT], BF16)
                    FcT, B_FcT = SB(lay, "FcT", [128, 4, CT], BF16)
                    att = ExitStack()
                    KT, B_KT = SB(att, "KT", [128, 2, NT], BF16)
                    Vown, B_V = SB(att, "Vown", [128, 16, 2, 80], BF16)
                    KcT, B_KcT = SB(att, "KcT", [128, 2, CT], BF16)
                    Vc, B_Vc = SB(att, "Vc", [128, 2, 2, 80], BF16)
                    hp, B_hp = SB(att, "hp", [128, 1024], BF16)
                    hn, B_hn = SB(att, "hn", [128, 1024], BF16)
                    P.op("pool", lambda: nc.gpsimd.memset(Vown[:], 1.0), writes=[B_V])
                    P.op("pool", lambda: nc.gpsimd.memset(Vc[:], 1.0), writes=[B_Vc])

                    def mods(ts, base):
                        s = ts["s"]
                        return lambda c: mod[:, l, base + c, s:s + 1]

                    with ExitStack() as ph:
                        nb = norm_bufs(ph)
                        hTs = [SB(ph, "hT%d" % i, [128, 8, 512], BF16) for i in range(2)]
                        hq = [0]
                        wu, B_wu = SB(ph, "wu", [128, 8, 512], BF16)
                        wk, B_wk = SB(ph, "wk", [128, 8, 512], BF16)
                        wv, B_wv = SB(ph, "wv", [128, 8, 128], BF16)
                        rc, B_rc = SB(ph, "rc", [128, NT], BF16)
                        rs, B_rs = SB(ph, "rs", [128, NT], BF16)
                        ust = [SB(ph, "ust%d" % i, [128, 512], BF16) for i in range(2)]
                        r1 = [SB(ph, "r1_%d" % i, [128, 512], F32) for i in range(2)]
                        r2 = [SB(ph, "r2_%d" % i, [128, 512], F32) for i in range(2)]
                        wsrc = lambda o, n: (lambda: win_d[l, :, o:o + n].rearrange("(kc p) n -> p kc n", p=128))
                        wload(lambda: wu[:], B_wu, wsrc(O_U, 512))
                        wload(lambda: wk[:], B_wk, wsrc(O_K, 512))
                        wload(lambda: wv[:], B_wv, wsrc(O_V, 128))
                        ld(lambda: rc[:], B_rc, lambda: cos_d[:, :])
                        ld(lambda: rs[:], B_rs, lambda: sin_d[:, :])
                        uq = [0]
                        for ts in ([CTX, LAT]):
                            isl = ts is LAT
                            for (g, off, n) in ts["tgs"]:
                                hT, B_hT = hTs[hq[0] % 2]
                                hq[0] += 1
                                norm_tile(nb, ts, g, off, n, lambda c, ts=ts: g1m[:, l, c, ts["s"]:ts["s"] + 1], mods(ts, 0), hT, B_hT, [B_g1l[l], B_modl[l]])
                                for b in range(n // 128):
                                    gb = off // 128 + b
                                    pi = nps()
                                    for kc in range(8):
                                        mm(lambda pi=pi: ps[pi][:, :], lambda kc=kc, b=b: hT[:, kc, b * 128:(b + 1) * 128],
                                           lambda kc=kc: wu[:, kc, :], kc == 0, kc == 7, [B_hT, B_wu], psB[pi])
                                    ut, ub = ust[uq[0] % 2]
                                    uq[0] += 1
                                    P.op("dve", lambda pi=pi, ut=ut: nc.vector.tensor_copy(out=ut[:], in_=ps[pi][:]), reads=[psB[pi]], writes=[ub])
                                    udst, uB = (u_own[gb // 8], B_uown) if isl else (u_ctx, B_uctx)
                                    gr = gb % 8 if isl else gb
                                    P.op("sp", lambda ut=ut, udst=udst, gr=gr: nc.sync.dma_start(out=udst[gr * 128:(gr + 1) * 128, :], in_=ut[:]),
                                         reads=[ub], writes=[uB], dma=True)
                                    pi = nps()
                                    for kc in range(8):
                                        mm(lambda pi=pi: ps[pi][:, 0:128], lambda kc=kc, b=b: hT[:, kc, b * 128:(b + 1) * 128],
                                           lambda kc=kc: wv[:, kc, :], kc == 0, kc == 7, [B_hT, B_wv], psB[pi])
                                    vdst, vB = (Vown, B_V) if isl else (Vc, B_Vc)
                                    P.op("dve", lambda pi=pi, vdst=vdst, gb=gb: nc.vector.tensor_copy(
                                        out=vdst[:, gb, :, 0:64], in_=ps[pi][:, 0:128].rearrange("p (h d) -> p h d", h=2)),
                                        reads=[psB[pi]], writes=[vB])
                                for h in range(2):
                                    pa = nps()
                                    for kc in range(8):
                                        mm(lambda pa=pa: ps[pa][0:64, :n], lambda kc=kc, h=h: wk[:, kc, h * 128:h * 128 + 64],
                                           lambda kc=kc: hT[:, kc, :n], kc == 0, kc == 7, [B_hT, B_wk], psB[pa])
                                    if not isl:
                                        P.op("dve", lambda pa=pa, h=h: nc.vector.tensor_copy(out=KcT[0:64, h, :], in_=ps[pa][0:64, :n]),
                                             reads=[psB[pa]], writes=[B_KcT])
                                        continue
                                    pb = nps()
                                    for kc in range(8):
                                        mm(lambda pb=pb: ps[pb][0:64, :n], lambda kc=kc, h=h: wk[:, kc, (2 + h) * 128:(2 + h) * 128 + 64],
                                           lambda kc=kc: hT[:, kc, :n], kc == 0, kc == 7, [B_hT, B_wk], psB[pb])
                                    t1, b1 = r1[h]
                                    t2, b2 = r2[h]
                                    P.op("dve", lambda pa=pa, t1=t1, off=off: nc.vector.tensor_tensor(out=t1[0:64, :], in0=ps[pa][0:64, :], in1=rc[0:64, off:off + 512], op=ALU.mult),
                                         reads=[psB[pa], B_rc], writes=[b1])
                                    P.op("dve", lambda pb=pb, t2=t2, off=off: nc.vector.tensor_tensor(out=t2[0:64, :], in0=ps[pb][0:64, :], in1=rs[0:64, off:off + 512], op=ALU.mult),
                                         reads=[psB[pb], B_rs], writes=[b2])
                                    P.op("pool", lambda t1=t1, t2=t2, h=h, off=off: nc.gpsimd.tensor_tensor(out=KT[0:64, h, off:off + 512], in0=t1[0:64, :], in1=t2[0:64, :], op=ALU.add),
                                         reads=[b1, b2], writes=[B_KT])
                        stop_pt("p1b")
                        P.op("sp", lambda: nc.sync.dma_start(out=halo_in[:, 0:256].rearrange("p (h t) -> p h t", h=2), in_=KT[:, :, 0:128]),
                             reads=[B_KT], writes=[B_hin], dma=True)
                        P.op("sp", lambda: nc.sync.dma_start(out=halo_in[:, 256:512].rearrange("p (h t) -> p h t", h=2), in_=KT[:, :, NT - 128:NT]),
                             reads=[B_KT], writes=[B_hin], dma=True)
                        P.op("sp", lambda: nc.sync.dma_start(out=halo_in[:, 512:672], in_=Vown[:, 0].rearrange("p h e -> p (h e)")),
                             reads=[B_V], writes=[B_hin], dma=True)
                        P.op("sp", lambda: nc.sync.dma_start(out=halo_in[:, 672:832], in_=Vown[:, 15].rearrange("p h e -> p (h e)")),
                             reads=[B_V], writes=[B_hin], dma=True)
                        stop_pt("p1c")
                        P.op("pool", lambda: nc.gpsimd.collective_compute("AllGather", ALU.bypass, replica_groups=[[0, 1, 2, 3], [4, 5, 6, 7]],
                                                                          ins=[halo_in.ap().opt()], outs=[halo_all.ap().opt()]),
                             reads=[B_hin], writes=[B_hall], cc=True)
                        for k2 in range(2):
                            P.op("pool", lambda k2=k2: nc.gpsimd.collective_compute("AllGather", ALU.bypass, replica_groups=[[0, 1, 2, 3], [4, 5, 6, 7]],
                                                                                    ins=[u_own[k2].ap().opt()], outs=[u_all[k2].ap().opt()]),
                                 reads=[B_uown], writes=[B_uall], cc=True)
                        stop_pt("p1d")
                        hall, B_hl = SB(ph, "hall", [128, 4, 1024], BF16)
                        P.op("sp", lambda: nc.sync.dma_start(out=hall[:], in_=halo_all.ap().rearrange("(r p) n -> p r n", p=128)),
                             reads=[B_hall], writes=[B_hl], dma=True)
                        for (dst, Bd, so) in ((hp, B_hp, 0), (hn, B_hn, 4)):
                            P.op("dve", lambda dst=dst, so=so: nc.vector.tensor_scalar(out=dst[:, 0:832], in0=hall[:, 0, 0:832], scalar1=sel[:, so:so + 1], scalar2=None, op0=ALU.mult),
                                 reads=[B_hl, B_sel], writes=[Bd])
                            for r in range(1, 4):
                                P.op("dve", lambda dst=dst, so=so, r=r: nc.vector.scalar_tensor_tensor(
                                    out=dst[:, 0:832], in0=hall[:, r, 0:832], scalar=sel[:, so + r:so + r + 1], in1=dst[:, 0:832], op0=ALU.mult, op1=ALU.add),
                                    reads=[B_hl, B_sel], writes=[Bd])
                        P.barrier()
                        stop_pt("p1_%d" % l)

                    with ExitStack() as ph:
                        nb = norm_bufs(ph)
                        hT, B_hT = SB(ph, "hT", [128, 8, 512], BF16)
                        wq, B_wq = SB(ph, "wq", [128, 8, 1024], BF16)
                        rc, B_rc = SB(ph, "rc", [128, NT], BF16)
                        rs, B_rs = SB(ph, "rs", [128, NT], BF16)
                        QT, B_QT = SB(ph, "QT", [128, 8, 512], BF16)
                        r1 = [SB(ph, "r1_%d" % i, [128, 512], F32) for i in range(2)]
                        r2 = [SB(ph, "r2_%d" % i, [128, 512], F32) for i in range(2)]
                        pt = [SB(ph, "pt%d" % i, [128, 512], BF16) for i in range(6)]
                        pslist[0] = [0, 1, 2, 3]
                        qcount = [0]
                        Ons = [SB(ph, "On%d" % i, [128, 512], F32) for i in range(2)]
                        pendT = []
                        den, B_den = SB(ph, "den", [128, 8], F32)
                        wload(lambda: wq[:], B_wq, lambda: win_d[l, :, O_Q:O_Q + 1024].rearrange("(kc p) n -> p kc n", p=128))
                        ld(lambda: rc[:], B_rc, lambda: cos_d[:, :])
                        ld(lambda: rs[:], B_rs, lambda: sin_d[:, :])
                        eq = [0]
                        for ts in ([LAT] if last else [CTX, LAT]):
                            isl = ts is LAT
                            for (g, off, n) in ts["tgs"]:
                                norm_tile(nb, ts, g, off, n, lambda c, ts=ts: g1m[:, l, c, ts["s"]:ts["s"] + 1], mods(ts, 0), hT, B_hT, [B_g1l[l], B_modl[l]])
                                for hd in range(8):
                                    pa = nps()
                                    for kc in range(8):
                                        mm(lambda pa=pa: ps[pa][0:64, :n], lambda kc=kc, hd=hd: wq[:, kc, hd * 64:(hd + 1) * 64],
                                           lambda kc=kc: hT[:, kc, :n], kc == 0, kc == 7, [B_hT, B_wq], psB[pa])
                                    if not isl:
                                        P.op("dve", lambda pa=pa, hd=hd: nc.vector.tensor_copy(out=QT[0:64, hd, :n], in_=ps[pa][0:64, :n]),
                                             reads=[psB[pa]], writes=[B_QT])
                                        continue
                                    pb = nps()
                                    for kc in range(8):
                                        mm(lambda pb=pb: ps[pb][0:64, :n], lambda kc=kc, hd=hd: wq[:, kc, 512 + hd * 64:512 + (hd + 1) * 64],
                                           lambda kc=kc: hT[:, kc, :n], kc == 0, kc == 7, [B_hT, B_wq], psB[pb])
                                    t1, b1 = r1[hd % 2]
                                    t2, b2 = r2[hd % 2]
                                    P.op("dve", lambda pa=pa, t1=t1, off=off: nc.vector.tensor_tensor(out=t1[0:64, :], in0=ps[pa][0:64, :], in1=rc[0:64, off:off + 512], op=ALU.mult),
                                         reads=[psB[pa], B_rc], writes=[b1])
                                    P.op("dve", lambda pb=pb, t2=t2, off=off: nc.vector.tensor_tensor(out=t2[0:64, :], in0=ps[pb][0:64, :], in1=rs[0:64, off:off + 512], op=ALU.mult),
                                         reads=[psB[pb], B_rs], writes=[b2])
                                    P.op("pool", lambda t1=t1, t2=t2, hd=hd: nc.gpsimd.tensor_tensor(out=QT[0:64, hd, :], in0=t1[0:64, :], in1=t2[0:64, :], op=ALU.add),
                                         reads=[b1, b2], writes=[B_QT])
                                for qb in range(n // 128):
                                    gb = off // 128 + qb
                                    po2 = [4 + (qcount[0] % 2) * 2 + h for h in range(2)]
                                    On, B_On = Ons[qcount[0] % 2]
                                    qcount[0] += 1
                                    keysets = []
                                    for h in range(2):
                                        keys = []
                                        if isl:
                                            if gb == 0:
                                                keys.append((lambda h=h: hp[0:64, 256 + h * 128:256 + (h + 1) * 128],
                                                             lambda h=h: hp[:, 672 + h * 80:672 + h * 80 + 65], 2, [B_hp]))
                                            else:
                                                keys.append((lambda h=h, gb=gb: KT[0:64, h, (gb - 1) * 128:gb * 128],
                                                             lambda h=h, gb=gb: Vown[:, gb - 1, h, 0:65], 0, [B_KT, B_V]))
                                            keys.append((lambda h=h, gb=gb: KT[0:64, h, gb * 128:(gb + 1) * 128],
                                                         lambda h=h, gb=gb: Vown[:, gb, h, 0:65], None, [B_KT, B_V]))
                                            if gb == 15:
                                                keys.append((lambda h=h: hn[0:64, h * 128:(h + 1) * 128],
                                                             lambda h=h: hn[:, 512 + h * 80:512 + h * 80 + 65], 3, [B_hn]))
                                            else:
                                                keys.append((lambda h=h, gb=gb: KT[0:64, h, (gb + 1) * 128:(gb + 2) * 128],
                                                             lambda h=h, gb=gb: Vown[:, gb + 1, h, 0:65], 1, [B_KT, B_V]))
                                        for cb in range(2):
                                            keys.append((lambda h=h, cb=cb: KcT[0:64, h, cb * 128:(cb + 1) * 128],
                                                         lambda h=h, cb=cb: Vc[:, cb, h, 0:65], None, [B_KcT, B_Vc]))
                                        keysets.append(keys)
                                    nk = len(keysets[0])
                                    jobs = [(h, ki) for ki in range(nk) for h in range(2)]
                                    LAG = 3
                                    pend = []
                                    for s_ in range(len(jobs) + LAG):
                                        if s_ < len(jobs):
                                            h, ki = jobs[s_]
                                            kap, vap, mk, kr = keysets[h][ki]
                                            pi = nps()
                                            mm(lambda pi=pi: ps[pi][:, :], lambda kap=kap: kap(),
                                               lambda h=h, qb=qb: QT[0:64, 4 * h:4 * h + 4, qb * 128:(qb + 1) * 128],
                                               True, True, [B_QT] + kr, psB[pi])
                                            ptt, pb_ = pt[eq[0] % len(pt)]
                                            eq[0] += 1
                                            P.op("act", lambda pi=pi, ptt=ptt: nc.scalar.activation(out=ptt[:], in_=ps[pi][:], func=AF.Exp, scale=0.125),
                                                 reads=[psB[pi]], writes=[pb_])
                                            if mk is not None:
                                                P.op("pool", lambda ptt=ptt, mk=mk: nc.gpsimd.tensor_tensor(out=ptt[:], in0=ptt[:], in1=masks[:, mk, :], op=ALU.mult),
                                                     reads=[pb_, B_masks], writes=[pb_])
                                            pend.append((h, ki, ptt, pb_, vap, kr))
                                        if s_ == LAG and pendT:
                                            pendT.pop(0)()
                                        if s_ >= LAG:
                                            h, ki, ptt, pb_, vap, kr = pend[s_ - LAG]
                                            po = po2[h]
                                            for gi in range(4):
                                                col = gi * 128
                                                mm(lambda po=po, gi=gi: ps[po][:, gi * 80:gi * 80 + 65], lambda ptt=ptt, col=col: ptt[:, col:col + 128],
                                                   lambda vap=vap: vap(), ki == 0, ki == nk - 1, [pb_] + kr, psB[po])
                                    for h in range(2):
                                        po = po2[h]
                                        P.op("dve", lambda po=po, h=h: nc.vector.tensor_tensor(
                                            out=den[:, 4 * h:4 * h + 4], in0=ps[po][:, 0:320].rearrange("p (g e) -> p g e", e=80)[:, :, 64],
                                            in1=esink[:, l * 8 + 4 * h:l * 8 + 4 * h + 4], op=ALU.add),
                                            reads=[psB[po], B_esink], writes=[B_den])
                                    P.op("dve", lambda: nc.vector.reciprocal(out=den[:], in_=den[:]), reads=[B_den], writes=[B_den])
                                    for h in range(2):
                                        po = po2[h]
                                        for gi in range(4):
                                            P.op("dve", lambda po=po, gi=gi, h=h: nc.vector.tensor_scalar(
                                                out=On[:, (4 * h + gi) * 64:(4 * h + gi + 1) * 64], in0=ps[po][:, gi * 80:gi * 80 + 64],
                                                scalar1=den[:, 4 * h + gi:4 * h + gi + 1], scalar2=None, op0=ALU.mult),
                                                reads=[psB[po], B_den], writes=[B_On])
                                    def emit_T(On=On, B_On=B_On, gb=gb, isl=isl):
                                        pi = nps()
                                        for c4 in range(4):
                                            P.op("pe", lambda pi=pi, c4=c4: nc.tensor.transpose(out=ps[pi][:, c4 * 128:(c4 + 1) * 128], in_=On[:, c4 * 128:(c4 + 1) * 128], identity=ident[:]),
                                                 reads=[B_On, B_ident], writes=[psB[pi]])
                                        odst, oB = (OT, B_OT) if isl else (OcT, B_OcT)
                                        P.op("dve", lambda pi=pi, odst=odst: nc.vector.tensor_copy(
                                            out=odst[:, :, gb * 128:(gb + 1) * 128], in_=ps[pi][:].rearrange("p (c t) -> p c t", c=4)),
                                            reads=[psB[pi]], writes=[oB])
                                    pendT.append(emit_T)
                                while pendT:
                                    pendT.pop(0)()
                        pslist[0] = list(range(8))
                        P.barrier()
                        stop_pt("pa_%d" % l)
                    att.close()

                    with ExitStack() as ph:
                        ub = [SB(ph, "ub%d" % i, [128, 512], BF16) for i in range(3)]
                        tcb = [SB(ph, "tcb%d" % i, [128, 512], BF16) for i in range(3)]
                        tsb = [SB(ph, "tsb%d" % i, [128, 512], BF16) for i in range(3)]
                        yri, B_yri = SB(ph, "yri", [128, 8, 512], BF16)
                        dq = [0]
                        for ts in ([] if last else [CTX]):
                            isl = ts is LAT
                            uB = B_uall if isl else B_uctx
                            tc_d, ts_d = (tabcc_d, tabsc_d)
                            nblk = 64 if isl else 2
                            for (g, off, n) in ts["tgs"]:
                                acc = [nps() for _ in range(8)]
                                for nbk in range(nblk):
                                    i3 = dq[0] % 3
                                    dq[0] += 1
                                    (ut, uBb), (ct, cB), (st_, sB_) = ub[i3], tcb[i3], tsb[i3]
                                    if not isl:
                                        usrc, urow = u_ctx, nbk * 128
                                        P.op("sp", lambda ut=ut, usrc=usrc, urow=urow: nc.sync.dma_start(out=ut[:], in_=usrc[urow:urow + 128, :]),
                                             reads=[uB], writes=[uBb], dma=True)
                                        uap = lambda gg, ut=ut: ut[:, gg * 128:(gg + 1) * 128]

                                    P.op("sp", lambda ct=ct, tc_d=tc_d, nbk=nbk, off=off, n=n: nc.sync.dma_start(out=ct[:, :n], in_=tc_d[nbk * 128:(nbk + 1) * 128, off:off + n]),
                                         writes=[cB], dma=True)
                                    P.op("sp", lambda st_=st_, ts_d=ts_d, nbk=nbk, off=off, n=n: nc.sync.dma_start(out=st_[:, :n], in_=ts_d[nbk * 128:(nbk + 1) * 128, off:off + n]),
                                         writes=[sB_], dma=True)
                                    for gg in range(4):
                                        mm(lambda gg=gg: ps[acc[gg]][:, :n], lambda uap=uap, gg=gg: uap(gg), lambda ct=ct: ct[:, :n],
                                           nbk == 0, nbk == nblk - 1, [uBb, cB], psB[acc[gg]])
                                        mm(lambda gg=gg: ps[acc[4 + gg]][:, :n], lambda uap=uap, gg=gg: uap(gg), lambda st_=st_: st_[:, :n],
                                           nbk == 0, nbk == nblk - 1, [uBb, sB_], psB[acc[4 + gg]])
                                for j in range(8):
                                    P.op("dve", lambda j=j: nc.vector.tensor_copy(out=yri[:, j, :n], in_=ps[acc[j]][:, :n]), reads=[psB[acc[j]]], writes=[B_yri])
                                fdst, fB = (FT, B_FT) if isl else (FcT, B_FcT)
                                for gg in range(4):
                                    pi = nps()
                                    mm(lambda pi=pi: ps[pi][:, :n], lambda: ccsc[:, 0:128], lambda gg=gg: yri[:, gg, :n], True, False, [B_ccsc, B_yri], psB[pi])
                                    mm(lambda pi=pi: ps[pi][:, :n], lambda: ccsc[:, 128:256], lambda gg=gg: yri[:, 4 + gg, :n], False, True, [B_ccsc, B_yri], psB[pi])
                                    P.op("dve", lambda pi=pi, fdst=fdst, gg=gg, off=off, n=n: nc.vector.tensor_copy(out=fdst[:, gg, off:off + n], in_=ps[pi][:, :n]),
                                         reads=[psB[pi]], writes=[fB])
                        P.barrier()
                        stop_pt("pb0c_%d" % l)
                    with ExitStack() as ph:
                        Tb_ = [SB(ph, "ctT%d" % i, [128, 8, 512], BF16) for i in range(2)]
                        st1 = [SB(ph, "st1_%d" % i, [128, 2, 8, 512], BF16) for i in range(2)]
                        def ct_loads(n2c):
                            Tt, TB = Tb_[n2c % 2]
                            for r_ in range(4):
                                for k2_ in range(2):
                                    P.op("sp", lambda Tt=Tt, r_=r_, k2_=k2_, n2c=n2c: nc.sync.dma_start(
                                        out=Tt[r_ * 32 + k2_ * 16:r_ * 32 + k2_ * 16 + 16, :, :],
                                        in_=u_all[k2_][r_ * 1024:(r_ + 1) * 1024, :].rearrange("(a n) c -> a n c", n=64)[:, n2c * 8:(n2c + 1) * 8, :]),
                                        reads=[B_uall], writes=[TB], dma=True)
                        ct_loads(0)
                        for n2c in range(8):
                            Tt, TB = Tb_[n2c % 2]
                            s1t, s1B = st1[n2c % 2]
                            if n2c + 1 < 8:
                                ct_loads(n2c + 1)
                            for n2i in range(8):
                                pr, pi_ = nps(), nps()
                                mm(lambda pr=pr: ps[pr][:, :], lambda: c128[:, 0:128], lambda Tt=Tt, n2i=n2i: Tt[:, n2i, :], True, True, [B_c128, TB], psB[pr])
                                mm(lambda pi_=pi_: ps[pi_][:, :], lambda: c128[:, 128:256], lambda Tt=Tt, n2i=n2i: Tt[:, n2i, :], True, True, [B_c128, TB], psB[pi_])
                                P.op("act", lambda pr=pr, s1t=s1t, n2i=n2i: nc.scalar.copy(out=s1t[:, 0, n2i, :], in_=ps[pr][:, :]), reads=[psB[pr]], writes=[s1B])
                                P.op("dve", lambda pi_=pi_, s1t=s1t, n2i=n2i: nc.vector.tensor_copy(out=s1t[:, 1, n2i, :], in_=ps[pi_][:, :]), reads=[psB[pi_]], writes=[s1B])
                            for ri in range(2):
                                P.op("sp", lambda s1t=s1t, ri=ri, n2c=n2c: nc.sync.dma_start(out=s1_d[ri, :, n2c * 8:(n2c + 1) * 8, :], in_=s1t[:, ri, :, :]),
                                     reads=[s1B], writes=[B_s1], dma=True)
                        P.barrier()
                        stop_pt("pb0s1_%d" % l)
                    with ExitStack() as ph:
                        Yb, B_Yb = SB(ph, "ctY", [128, 4, 2, NT], BF16)
                        Rb_ = [SB(ph, "ctR%d" % i, [128, 8, 512], BF16) for i in range(2)]
                        t23, B_t23 = SB(ph, "t23", [128, 128, 32], BF16)
                        ld(lambda: t23[:], B_t23, lambda: tab23_d.ap().rearrange("p (k c) -> p k c", c=32))
                        acc = None
                        for k1c in range(16):
                            Rt, RB = Rb_[k1c % 2]
                            for ri in range(2):
                                P.op("sp", lambda Rt=Rt, ri=ri, k1c=k1c: nc.sync.dma_start(
                                    out=Rt[ri * 64:(ri + 1) * 64, :, :], in_=s1_d[ri, k1c * 8:(k1c + 1) * 8, :, :].rearrange("k n c -> n k c")),
                                    reads=[B_s1], writes=[RB], dma=True)
                            if k1c % 2 == 0:
                                acc = [nps() for _ in range(4)]
                            for k1i in range(8):
                                k1 = k1c * 8 + k1i
                                for gg in range(4):
                                    mm(lambda gg=gg, k1=k1: ps[acc[gg]][:, (k1 % 16) * 32:(k1 % 16) * 32 + 32],
                                       lambda Rt=Rt, k1i=k1i, gg=gg: Rt[:, k1i, gg * 128:(gg + 1) * 128],
                                       lambda k1=k1: t23[:, k1, :], True, True, [RB, B_t23], psB[acc[gg]])
                            if k1c % 2 == 1:
                                kbase = (k1c - 1) * 8
                                for gg in range(4):
                                    for ro in range(2):
                                        src = lambda gg=gg, ro=ro: ps[acc[gg]][:, :].rearrange("p (a r b) -> p a r b", r=2, b=16)[:, :, ro, :]
                                        dst = lambda gg=gg, ro=ro, kbase=kbase: Yb[:, gg, ro, :].rearrange("p (b a) -> p a b", a=128)[:, kbase:kbase + 16, :]
                                        if gg % 2 == 0:
                                            P.op("act", lambda src=src, dst=dst: nc.scalar.copy(out=dst(), in_=src()), reads=[psB[acc[gg]]], writes=[B_Yb])
                                        else:
                                            P.op("dve", lambda src=src, dst=dst: nc.vector.tensor_copy(out=dst(), in_=src()), reads=[psB[acc[gg]]], writes=[B_Yb])
                        for tg_ in range(4):
                            for gg in range(4):
                                pi = nps()
                                mm(lambda pi=pi: ps[pi][:, :], lambda: ccsc[:, 0:128], lambda gg=gg, tg_=tg_: Yb[:, gg, 0, tg_ * 512:(tg_ + 1) * 512], True, False, [B_ccsc, B_Yb], psB[pi])
                                mm(lambda pi=pi: ps[pi][:, :], lambda: ccsc[:, 128:256], lambda gg=gg, tg_=tg_: Yb[:, gg, 1, tg_ * 512:(tg_ + 1) * 512], False, True, [B_ccsc, B_Yb], psB[pi])
                                P.op("dve", lambda pi=pi, gg=gg, tg_=tg_: nc.vector.tensor_copy(out=FT[:, gg, tg_ * 512:(tg_ + 1) * 512], in_=ps[pi][:, :]),
                                     reads=[psB[pi]], writes=[B_FT])
                        P.barrier()
                        stop_pt("pb0_%d" % l)

                    with ExitStack() as ph:
                        nb = norm_bufs(ph)
                        hT, B_hT = SB(ph, "hT", [128, 8, 256], BF16)
                        wg2, B_wg2 = SB(ph, "wg2", [128, 8, 2048], BF16)
                        wf, B_wf = SB(ph, "wf", [128, 4, D], BF16)
                        wa, B_wa = SB(ph, "wa", [128, 4, D], BF16)
                        wo, B_wo = SB(ph, "wo", [128, 8, D], BF16)
                        yT, B_yT = SB(ph, "yT", [128, 8, 256], BF16)
                        sg = [SB(ph, "sg%d" % i, [128, 256], F32) for i in range(4)]
                        tm = [SB(ph, "tm%d" % i, [128, 256], F32) for i in range(4)]
                        wload(lambda: wf[:], B_wf, lambda: wf_d[l].rearrange("(kc p) n -> p kc n", p=128))
                        wload(lambda: wa[:], B_wa, lambda: wa_d[l].rearrange("(kc p) n -> p kc n", p=128))
                        wload(lambda: wg2[:], B_wg2, lambda: win_d[l, :, O_G:O_G + 2048].rearrange("(kc p) n -> p kc n", p=128))
                        wload(lambda: wo[:], B_wo, lambda: wo_d[l].rearrange("(kc p) n -> p kc n", p=128))
                        for ts in ([LAT] if last else [CTX, LAT]):
                            isl = ts is LAT
                            fsrc, fB = (FT, B_FT) if isl else (FcT, B_FcT)
                            osrc, oB = (OT, B_OT) if isl else (OcT, B_OcT)
                            s = ts["s"]
                            for (g, off, n) in [(o_ // 512, o_, 256) for o_ in range(0, ts["n"], 256)]:
                                norm_tile(nb, ts, g, off, n, lambda c, s=s: g1m[:, l, c, s:s + 1], mods(ts, 0), hT, B_hT, [B_g1l[l], B_modl[l]])
                                for dc in range(8):
                                    pF, pA, pGf, pGa = nps(), nps(), nps(), nps()
                                    for gg in range(4):
                                        mm(lambda pF=pF: ps[pF][:, :n], lambda gg=gg, dc=dc: wf[:, gg, dc * 128:(dc + 1) * 128],
                                           lambda gg=gg: fsrc[:, gg, off:off + n], gg == 0, gg == 3, [B_wf, fB], psB[pF])
                                    for gg in range(4):
                                        mm(lambda pA=pA: ps[pA][:, :n], lambda gg=gg, dc=dc: wa[:, gg, dc * 128:(dc + 1) * 128],
                                           lambda gg=gg: osrc[:, gg, off:off + n], gg == 0, gg == 3, [B_wa, oB], psB[pA])
                                    for kc in range(8):
                                        mm(lambda pGf=pGf: ps[pGf][:, :n], lambda kc=kc, dc=dc: wg2[:, kc, dc * 128:(dc + 1) * 128],
                                           lambda kc=kc: hT[:, kc, :n], kc == 0, kc == 7, [B_wg2, B_hT], psB[pGf])
                                    for kc in range(8):
                                        mm(lambda pGa=pGa: ps[pGa][:, :n], lambda kc=kc, dc=dc: wg2[:, kc, D + dc * 128:D + (dc + 1) * 128],
                                           lambda kc=kc: hT[:, kc, :n], kc == 0, kc == 7, [B_wg2, B_hT], psB[pGa])
                                    i0, i1 = (dc % 2) * 2, (dc % 2) * 2 + 1
                                    P.op("act", lambda pGf=pGf, i0=i0: nc.scalar.activation(out=sg[i0][0][:, :n], in_=ps[pGf][:, :n], func=AF.Sigmoid),
                                         reads=[psB[pGf]], writes=[sg[i0][1]])
                                    P.op("act", lambda pGa=pGa, i1=i1: nc.scalar.activation(out=sg[i1][0][:, :n], in_=ps[pGa][:, :n], func=AF.Sigmoid),
                                         reads=[psB[pGa]], writes=[sg[i1][1]])
                                    P.op("dve", lambda pF=pF, i0=i0: nc.vector.tensor_tensor(out=tm[i0][0][:, :n], in0=ps[pF][:, :n], in1=sg[i0][0][:, :n], op=ALU.mult),
                                         reads=[psB[pF], sg[i0][1]], writes=[tm[i0][1]])
                                    P.op("dve", lambda pA=pA, i1=i1: nc.vector.tensor_tensor(out=tm[i1][0][:, :n], in0=ps[pA][:, :n], in1=sg[i1][0][:, :n], op=ALU.mult),
                                         reads=[psB[pA], sg[i1][1]], writes=[tm[i1][1]])
                                    P.op("pool", lambda i0=i0, i1=i1, dc=dc: nc.gpsimd.tensor_tensor(out=yT[:, dc, :n], in0=tm[i0][0][:, :n], in1=tm[i1][0][:, :n], op=ALU.add),
                                         reads=[tm[i0][1], tm[i1][1]], writes=[B_yT])
                                res, rb = ts["res"], ts["rb"]
                                for dc in range(8):
                                    pZ = nps()
                                    for kc in range(8):
                                        mm(lambda pZ=pZ: ps[pZ][:, :n], lambda kc=kc, dc=dc: wo[:, kc, dc * 128:(dc + 1) * 128],
                                           lambda kc=kc: yT[:, kc, :n], kc == 0, kc == 7, [B_wo, B_yT], psB[pZ])
                                    P.op("dve", lambda pZ=pZ, dc=dc, res=res, s=s: nc.vector.scalar_tensor_tensor(
                                        out=res[:, dc, off:off + n], in0=ps[pZ][:, :n], scalar=mod[:, l, 16 + dc, s:s + 1], in1=res[:, dc, off:off + n],
                                        op0=ALU.mult, op1=ALU.add), reads=[psB[pZ], B_modl[l], rb[(dc, g)]], writes=[rb[(dc, g)]])
                        P.barrier()

                stop_pt("mix%d" % l)
                with ExitStack() as ph:
                    nb = norm_bufs(ph)
                    NTOT = NT + (0 if last else CT)
                    h2, B_h2 = SB(ph, "h2", [128, 8, NTOT], BF16)
                    sets = [LAT] if last else [LAT, CTX]
                    tgl = []
                    for ts in sets:
                        for (g, off, n) in ts["tgs"]:
                            h2off = off if ts is LAT else NT
                            tgl.append((ts, g, off, n, h2off))
                    hB2 = {i: Buf("h2_%d" % i, True) for i in range(len(tgl))}
                    for ti, (ts, g, off, n, h2off) in enumerate(tgl):
                        s = ts["s"]

                        class _V:
                            def __init__(self, o):
                                self.o = o

                            def __getitem__(self, k):
                                p, c, sl = k
                                return h2[p, c, self.o + (sl.start or 0):self.o + sl.stop]
                        norm_tile(nb, ts, g, off, n, lambda c, s=s: g2m[:, l, c, s:s + 1], mods(ts, 24), None, hB2[ti], [B_g2l[l], B_modl[l]],
                                  hout=lambda c, h2off=h2off, n=n: h2[:, c, h2off:h2off + n], sqb=(_V(h2off), hB2[ti]))
                    if last:
                        wr, B_wr = SB(ph, "wr", [128, 8, 8], BF16)
                        comb, B_comb = SB(ph, "comb", [128, 16, 8], F32)
                        combT, B_combT = SB(ph, "combT", [8, NT], BF16)
                        esel, B_esel = SB(ph, "esel", [8, 8, 128], BF16)
                        lg, B_lg = SB(ph, "lg", [128, 8], F32)
                        m1, B_m1 = SB(ph, "m1", [128, 4], F32)
                        e1, B_e1 = SB(ph, "e1", [128, 8], F32)
                        l2, B_l2 = SB(ph, "l2", [128, 8], F32)
                        wload(lambda: wr[:], B_wr, lambda: wr_d.ap().rearrange("(kc p) n -> p kc n", p=128))
                        ld(lambda: esel[:], B_esel, lambda: esel_d.ap().rearrange("p (e n) -> p e n", e=8))
                        for blk in range(16):
                            pi = nps()
                            for kc in range(8):
                                mm(lambda pi=pi: ps[pi][:, 0:8], lambda kc=kc, blk=blk: h2[:, kc, blk * 128:(blk + 1) * 128],
                                   lambda kc=kc: wr[:, kc, :], kc == 0, kc == 7, [hB2[blk // 4], B_wr], psB[pi])
                            V = nc.vector
                            P.op("dve", lambda pi=pi: V.tensor_copy(out=lg[:], in_=ps[pi][:, 0:8]), reads=[psB[pi]], writes=[B_lg])
                            P.op("dve", lambda: V.reduce_max(out=m1[:, 0:1], in_=lg[:], axis=AX.X), reads=[B_lg], writes=[B_m1])
                            P.op("dve", lambda: V.tensor_scalar(out=e1[:], in0=lg[:], scalar1=m1[:, 0:1], scalar2=None, op0=ALU.is_equal), reads=[B_lg, B_m1], writes=[B_e1])
                            P.op("dve", lambda: V.scalar_tensor_tensor(out=l2[:], in0=e1[:], scalar=-1e30, in1=lg[:], op0=ALU.mult, op1=ALU.add), reads=[B_e1, B_lg], writes=[B_l2])
                            P.op("dve", lambda: V.reduce_max(out=m1[:, 1:2], in_=l2[:], axis=AX.X), reads=[B_l2, B_m1], writes=[B_m1])
                            P.op("dve", lambda: V.tensor_scalar(out=e1[:], in0=lg[:], scalar1=m1[:, 1:2], scalar2=None, op0=ALU.is_ge), reads=[B_lg, B_m1, B_l2], writes=[B_e1])
                            P.op("dve", lambda: V.tensor_scalar(out=l2[:], in0=lg[:], scalar1=m1[:, 0:1], scalar2=None, op0=ALU.subtract), reads=[B_lg, B_m1, B_e1], writes=[B_l2])
                            P.op("act", lambda: nc.scalar.activation(out=l2[:], in_=l2[:], func=AF.Exp), reads=[B_l2], writes=[B_l2])
                            P.op("dve", lambda: V.tensor_tensor(out=l2[:], in0=l2[:], in1=e1[:], op=ALU.mult), reads=[B_l2, B_e1], writes=[B_l2])
                            P.op("dve", lambda: V.reduce_sum(out=m1[:, 2:3], in_=l2[:], axis=AX.X), reads=[B_l2, B_m1], writes=[B_m1])
                            P.op("dve", lambda: V.reciprocal(out=m1[:, 3:4], in_=m1[:, 2:3]), reads=[B_m1], writes=[B_m1])
                            P.op("dve", lambda blk=blk: V.tensor_scalar(out=comb[:, blk, :], in0=l2[:], scalar1=m1[:, 3:4], scalar2=None, op0=ALU.mult),
                                 reads=[B_l2, B_m1], writes=[B_comb])
                            pj = nps()
                            P.op("pe", lambda pj=pj, blk=blk: nc.tensor.transpose(out=ps[pj][0:8, 0:128], in_=comb[:, blk, :], identity=ident[:]),
                                 reads=[B_comb, B_ident], writes=[psB[pj]])
                            P.op("dve", lambda pj=pj, blk=blk: V.tensor_copy(out=combT[:, blk * 128:(blk + 1) * 128], in_=ps[pj][0:8, 0:128]),
                                 reads=[psB[pj]], writes=[B_combT])
                        units = []
                        for e in range(8):
                            for pc in range(7):
                                units.append((e, 4, (lambda e=e, pc=pc: wge_d[e, :, pc * 512:(pc + 1) * 512]),
                                              (lambda e=e, pc=pc: wue_d[e, :, pc * 512:(pc + 1) * 512]),
                                              (lambda e=e, pc=pc: wde_d[e, pc * 512:(pc + 1) * 512, :])))
                    else:
                        units = []
                        for pc in range(6):
                            nf_ = 4 if pc < 5 else 2
                            units.append((None, nf_, (lambda pc=pc, nf_=nf_: wgd_d[:, pc * 512:pc * 512 + nf_ * 128]),
                                          (lambda pc=pc, nf_=nf_: wud_d[:, pc * 512:pc * 512 + nf_ * 128]),
                                          (lambda pc=pc, nf_=nf_: wdd_d[pc * 512:pc * 512 + nf_ * 128, :])))
                    wgu = [SB(ph, "wgu%d" % i, [128, 8, 2, 512], BF16) for i in range(2)]
                    wdn = [SB(ph, "wdn%d" % i, [128, 4, D], BF16) for i in range(2)]
                    aT = [SB(ph, "aT%d" % i, [128, 4, 512], BF16) for i in range(2)]

                    def uload(ui):
                        e, nf, gsrc, usrc_, dsrc = units[ui]
                        wt, wb = wgu[ui % 2]
                        dt_, db = wdn[ui % 2]
                        wload(lambda: wt[:, :, 0, 0:nf * 128], wb, lambda: gsrc().rearrange("(kc p) n -> p kc n", p=128))
                        wload(lambda: wt[:, :, 1, 0:nf * 128], wb, lambda: usrc_().rearrange("(kc p) n -> p kc n", p=128))
                        wload(lambda: dt_[:, 0:nf, :], db, lambda: dsrc().rearrange("(kc p) n -> p kc n", p=128))
                    uload(0)
                    sgl = [SB(ph, "fsg%d" % i, [128, 512], F32) for i in range(2)]
                    ftm = [SB(ph, "ftm%d" % i, [128, 512], F32) for i in range(2)]
                    cbc = [SB(ph, "cbc%d" % i, [128, 512], F32) for i in range(2)]
                    aq = [0]
                    fq = [0]
                    mstep = [0]
                    if not last:
                        wm1 = SB(ph, "wm1", [128, 8, 512], BF16)
                        pslist[0] = [0, 1, 2, 3, 4, 5, 6]
                        mpi1 = 7
                    for ui, (e, nf, gsrc, usrc_, dsrc) in enumerate(units):
                        wt, wb = wgu[ui % 2]
                        dt_, db = wdn[ui % 2]
                        if ui + 1 < len(units):
                            uload(ui + 1)
                        pend = None

                        def down(ti, at, ab):
                            ts, g, off, n, h2off = tgl[ti]
                            res, rb, s = ts["res"], ts["rb"], ts["s"]
                            for dc in range(8):
                                pY = nps()
                                for f in range(nf):
                                    mm(lambda pY=pY: ps[pY][:, :n], lambda f=f, dc=dc: dt_[:, f, dc * 128:(dc + 1) * 128],
                                       lambda f=f: at[:, f, :n], f == 0, f == nf - 1, [db, ab], psB[pY])
                                P.op("dve", lambda pY=pY, dc=dc: nc.vector.scalar_tensor_tensor(
                                    out=res[:, dc, off:off + n], in0=ps[pY][:, :n], scalar=mod[:, l, 40 + dc, s:s + 1], in1=res[:, dc, off:off + n],
                                    op0=ALU.mult, op1=ALU.add), reads=[psB[pY], B_modl[l], rb[(dc, g)]], writes=[rb[(dc, g)]])

                        for ti, (ts, g, off, n, h2off) in enumerate(tgl):
                            at, ab = aT[aq[0] % 2]
                            aq[0] += 1
                            if e is not None:
                                pc_ = nps()
                                ct_, cb_ = cbc[ti % 2]
                                mm(lambda pc_=pc_: ps[pc_][:, :n], lambda e=e: esel[:, e, :], lambda off=off, n=n: combT[:, off:off + n], True, True, [B_esel, B_combT], psB[pc_])
                                P.op("dve", lambda pc_=pc_, ct_=ct_: nc.vector.tensor_copy(out=ct_[:, :n], in_=ps[pc_][:, :n]), reads=[psB[pc_]], writes=[cb_])
                            for f in range(nf):
                                pG, pU = nps(), nps()
                                for kc in range(8):
                                    mm(lambda pG=pG: ps[pG][:, :n], lambda kc=kc, f=f: wt[:, kc, 0, f * 128:(f + 1) * 128],
                                       lambda kc=kc: h2[:, kc, h2off:h2off + n], kc == 0, kc == 7, [wb, hB2[ti]], psB[pG])
                                for kc in range(8):
                                    mm(lambda pU=pU: ps[pU][:, :n], lambda kc=kc, f=f: wt[:, kc, 1, f * 128:(f + 1) * 128],
                                       lambda kc=kc: h2[:, kc, h2off:h2off + n], kc == 0, kc == 7, [wb, hB2[ti]], psB[pU])
                                st2, sb2 = sgl[fq[0] % 2]
                                ft2, fb2 = ftm[fq[0] % 2]
                                fq[0] += 1
                                P.op("act", lambda pG=pG, st2=st2: nc.scalar.activation(out=st2[:, :n], in_=ps[pG][:, :n], func=AF.Silu),
                                     reads=[psB[pG]], writes=[sb2])
                                if e is None:
                                    P.op("dve", lambda pU=pU, st2=st2, at=at, f=f: nc.vector.tensor_tensor(out=at[:, f, :n], in0=ps[pU][:, :n], in1=st2[:, :n], op=ALU.mult),
                                         reads=[psB[pU], sb2], writes=[ab])
                                else:
                                    P.op("dve", lambda pU=pU, st2=st2, ft2=ft2: nc.vector.tensor_tensor(out=ft2[:, :n], in0=ps[pU][:, :n], in1=st2[:, :n], op=ALU.mult),
                                         reads=[psB[pU], sb2], writes=[fb2])
                                    P.op("pool", lambda ft2=ft2, ct_=ct_, at=at, f=f: nc.gpsimd.tensor_tensor(out=at[:, f, :n], in0=ft2[:, :n], in1=ct_[:, :n], op=ALU.mult),
                                         reads=[fb2, cb_], writes=[ab])
                            if pend is not None:
                                down(*pend)
                            pend = (ti, at, ab)
                            if not last and mstep[0] < 12:
                                mods_piece(1, mstep[0], wm1, mpi1)
                                mstep[0] += 1
                        down(*pend)
                    if not last:
                        while mstep[0] < 12:
                            mods_piece(1, mstep[0], wm1, mpi1)
                            mstep[0] += 1
                        mods_fin(1, mpi1)
                        pslist[0] = list(range(8))
                    P.barrier()
                stop_pt("ffn%d" % l)

            P.skip = False
            P.final = True
            with ExitStack() as ph:
                nb = norm_bufs(ph)
                oT, B_oT = SB(ph, "oT", [128, 8, 512], F32)
                sqo = SB(ph, "sqo", [128, 8, 512], BF16)
                ost = [SB(ph, "ost%d" % i, [128, D], F32) for i in range(2)]
                B_out = Buf("out")
                for (g, off, n) in LAT["tgs"]:
                    norm_tile(nb, LAT, g, off, n, lambda c: fing[:, c:c + 1], None, oT, B_oT, [B_fing], sqb=sqo)
                    for b in range(4):
                        gb = g * 4 + b
                        o_t, o_b = ost[gb % 2]
                        for half in range(2):
                            pi = nps()
                            for c in range(4):
                                cc = half * 4 + c
                                P.op("pe", lambda pi=pi, c=c, cc=cc, b=b: nc.tensor.transpose(
                                    out=ps[pi][:, c * 128:(c + 1) * 128], in_=oT[:, cc, b * 128:(b + 1) * 128], identity=ident[:]),
                                    reads=[B_oT, B_ident], writes=[psB[pi]])
                            P.op("dve", lambda pi=pi, o_t=o_t, half=half: nc.vector.tensor_copy(out=o_t[:, half * 512:(half + 1) * 512], in_=ps[pi][:]),
                                 reads=[psB[pi]], writes=[o_b])
                        P.op("sp", lambda o_t=o_t, gb=gb: nc.sync.dma_start(out=out_d[gb * 128:(gb + 1) * 128, :], in_=o_t[:]),
                             reads=[o_b], writes=[B_out], dma=True)
                if not P.dry:
                    for q in ("sp",):
                        m = P.dma_no[q]
                        for i in range(NDMASEM):
                            cnt = (m - i + NDMASEM - 1) // NDMASEM if m > i else 0
                            if cnt > 0:
                                P._wait("sp", (q, i), 16 * cnt)
    return nc


_CACHE = {}


def _consts():
    if "c" in _CACHE:
        return _CACHE["c"]
    c = {}
    c["ident"] = np.eye(128, dtype=np.float32)
    _a = 2 * np.pi * (np.outer(np.arange(128), np.arange(128)) % 128) / 128
    c["c128"] = np.concatenate([np.cos(_a), -np.sin(_a)], axis=1).astype(NPBF)
    c["ones1024"] = np.full((128, 128), 1.0 / 1024, dtype=NPBF)
    d = np.arange(64)
    partner = np.where((d % 32) < 16, d + 16, d - 16)
    c["partner"] = partner
    sign = np.where((d % 32) < 16, -1.0, 1.0)
    inv = 10000.0 ** (-np.arange(16, dtype=np.float64) / 16)
    c["rope"] = (sign, inv)
    kq = np.arange(128)
    mprev = (kq[:, None] >= kq[None, :]).astype(np.float32)
    mnext = (kq[:, None] <= kq[None, :]).astype(np.float32)
    c["mprev"] = np.tile(mprev, (1, 4))
    c["mnext"] = np.tile(mnext, (1, 4))
    ch = np.arange(128)
    ang = 2 * np.pi * ((np.outer(ch, ch)) % 128) / 128
    c["ccsc"] = np.concatenate([np.cos(ang), np.sin(ang)], axis=1) / np.sqrt(128.0)
    n = np.arange(CT)
    ang = 2 * np.pi * ((np.outer(n, n)) % CT) / CT
    c["tabcc"] = (np.cos(ang) / np.sqrt(CT)).astype(NPBF)
    c["tabsc"] = (-np.sin(ang) / np.sqrt(CT)).astype(NPBF)
    es = np.zeros((8, 8, 128), np.float32)
    for e in range(8):
        es[e, e, :] = 1.0
    c["esel"] = es.reshape(8, 1024).astype(NPBF)
    for j in range(4):
        n2 = np.arange(64, dtype=np.int64)
        k1 = np.arange(128, dtype=np.int64)
        k2 = np.arange(16, dtype=np.int64) + 16 * j
        kk = k1[:, None] + 128 * k2[None, :]
        ph_ = 2 * np.pi * ((n2[:, None, None] * kk[None, :, :]) % T).astype(np.float64) / T
        cph, sph = np.cos(ph_) / np.sqrt(T), np.sin(ph_) / np.sqrt(T)
        tb = np.zeros((128, 128, 32), np.float64)
        tb[:64, :, :16] = cph
        tb[64:, :, :16] = sph
        tb[:64, :, 16:] = -sph
        tb[64:, :, 16:] = cph
        c["tab23_%d" % j] = tb.reshape(128, 128 * 32).astype(NPBF)
        t = np.arange(NT) + NT * j
        rows = (t // 64).astype(np.float64)
        cols = (t % 64).astype(np.float64)
        dd = np.arange(128) % 64
        fi = dd % 16
        pos = np.where((dd < 32)[:, None], rows[None, :], cols[None, :])
        a = pos * inv[fi][:, None]
        c["cos%d" % j] = np.cos(a).astype(NPBF)
        c["sin%d" % j] = (np.sin(a) * sign[dd][:, None]).astype(NPBF)
    _CACHE["c"] = c
    return c


def _fm(v, nch):
    return np.ascontiguousarray(np.asarray(v, np.float32).reshape(nch, 128).T)


def kernel(x, c, ctx, c_ctx, w_mod, b_mod, norm1_g, norm2_g, w_in, sink, w_fourier, w_attn, w_out,
           w_gate_d, w_up_d, w_down_d, w_router, w_gate_e, w_up_e, w_down_e, final_g):
    K = _consts()
    f = lambda a: np.ascontiguousarray(np.asarray(a, dtype=np.float32))
    x, c, ctx, c_ctx = f(x), f(c), f(ctx), f(c_ctx)
    w_in = f(w_in)
    partner = K["partner"]
    qcols = np.arange(512)
    qpcols = (qcols // 64) * 64 + partner[qcols % 64]
    kcols = []
    for h in range(2):
        kcols += list(512 + 512 + h * 64 + np.arange(64)) * 2
    kpcols = []
    for h in range(2):
        kpcols += list(512 + 512 + h * 64 + partner) * 2
    cols = np.concatenate([np.arange(512), np.array(kcols), np.array(kpcols), 1152 + np.arange(128),
                           512 + qcols, 512 + qpcols, 1280 + np.arange(2048)]).astype(np.int64)
    assert cols.shape[0] == NCEXT
    w_in_ext = np.ascontiguousarray(w_in[:, :, cols])
    bmod = np.stack([np.repeat(_fm(b_mod[l], 48)[:, :, None], 2, axis=2).reshape(128, 96) for l in range(2)], axis=1).reshape(128, 192)
    n1g = np.stack([np.repeat(_fm(norm1_g[l], 8)[:, :, None], 2, axis=2).reshape(128, 16) for l in range(2)], axis=1).reshape(128, 32)
    n2g = np.stack([np.repeat(_fm(norm2_g[l], 8)[:, :, None], 2, axis=2).reshape(128, 16) for l in range(2)], axis=1).reshape(128, 32)
    fing = _fm(final_g, 8)
    sinkb = np.ascontiguousarray(np.broadcast_to(f(sink).reshape(1, 16), (128, 16)))
    shared = dict(w_mod=f(w_mod), bmod=np.ascontiguousarray(bmod), n1g=np.ascontiguousarray(n1g), n2g=np.ascontiguousarray(n2g),
                  fing=fing, w_in_ext=w_in_ext, sinkb=sinkb, w_fourier=f(w_fourier), w_attn=f(w_attn), w_out=f(w_out),
                  w_gate_d=f(w_gate_d)[0], w_up_d=f(w_up_d)[0], w_down_d=f(w_down_d)[0], w_router=f(w_router)[0],
                  w_gate_e=f(w_gate_e)[0], w_up_e=f(w_up_e)[0], w_down_e=f(w_down_e)[0],
                  ident=K["ident"], ones1024=K["ones1024"], tabcc=K["tabcc"], tabsc=K["tabsc"],
                  ccsc=K["ccsc"].astype(NPBF), esel=K["esel"])
    in_maps = []
    zeros = np.zeros((128, 512), np.float32)
    for i in range(8):
        b, j = i // 4, i % 4
        m = dict(shared)
        m["x"] = np.ascontiguousarray(x[b, j * NT:(j + 1) * NT])
        m["ctx"] = np.ascontiguousarray(ctx[b])
        cv = np.stack([_fm(c[b], 8), _fm(c_ctx, 8)], axis=2).reshape(128, 16)
        m["cvec"] = np.ascontiguousarray(cv)
        m["ropecos"] = K["cos%d" % j]
        m["ropesin"] = K["sin%d" % j]
        m["masks"] = np.concatenate([K["mprev"], K["mnext"], K["mprev"] if j > 0 else zeros, K["mnext"] if j < 3 else zeros], axis=1).astype(NPBF)
        sl = np.zeros((128, 8), np.float32)
        if j > 0:
            sl[:, j - 1] = 1.0
        if j < 3:
            sl[:, 4 + j + 1] = 1.0
        m["sel"] = sl
        m["tab23"] = K["tab23_%d" % j]
        m["c128"] = K["c128"]
        in_maps.append(m)

    if "nc" not in _CACHE:
        nc0 = bass.Bass("TRN2", target_bir_lowering=False)
        P0 = Prog(nc0)
        build(nc0, P0)
        nc = bass.Bass("TRN2", target_bir_lowering=False)
        P1 = Prog(nc, sigset=P0.need)
        build(nc, P1)
        _CACHE["nc"] = nc
    nc = _CACHE["nc"]
    in_maps = [{k: v for k, v in m.items() if k in nc._mk_declared} for m in in_maps]
    res = run_bass_kernel_spmd(nc, in_maps, core_ids=list(range(8)))
    out = np.zeros((2, T, D), np.float32)
    for i in range(8):
        b, j = i // 4, i % 4
        out[b, j * NT:(j + 1) * NT] = np.asarray(res.results[i]["out"], dtype=np.float32)
    return out
```

```python
import os
from contextlib import ExitStack
import numpy as np
import ml_dtypes
import concourse.bass as bass
import concourse.mybir as mybir
from concourse.bass_utils import run_bass_kernel_spmd

F32 = mybir.dt.float32
BF16 = mybir.dt.bfloat16
AF = mybir.ActivationFunctionType
ALU = mybir.AluOpType
AX = mybir.AxisListType
NPBF = ml_dtypes.bfloat16

D = 1024
T = 8192
NT = 2048
CT = 256
NCEXT = 4224
O_U, O_K, O_V, O_Q, O_G = 0, 512, 1024, 1152, 2176
DFF = 2816
DFE = 3584
NDMASEM = 8
STOP = os.environ.get("MK_STOP", "")


class Buf:
    __slots__ = ("name", "w", "r", "phase")

    def __init__(self, name, phase=False):
        self.name = name
        self.w = None
        self.r = {}
        self.phase = phase


class Prog:
    COMPUTE = ("pe", "act", "dve", "pool")

    def __init__(self, nc, sigset=None):
        self.nc = nc
        self.dry = sigset is None
        self.sigset = sigset if sigset is not None else set()
        self.need = set()
        self.n = 0
        self.meta = []
        self.cnt = {e: 0 for e in self.COMPUTE}
        self.sigval = {}
        self.waited = {}
        self.dma_no = {"sp": 0, "pool": 0, "act": 0}
        self.PH = Buf("PHASE")
        self.eng = {"pe": nc.tensor, "act": nc.scalar, "dve": nc.vector, "pool": nc.gpsimd, "sp": nc.sync}
        self.sems = {}
        self.ncc = 0

    def alloc_sems(self, stack):
        for e in self.COMPUTE:
            self.sems[e] = stack.enter_context(self.nc.semaphore("s_" + e))
        for q in ("sp", "pool"):
            for i in range(NDMASEM):
                self.sems[(q, i)] = stack.enter_context(self.nc.semaphore("d_%s%d" % (q, i)))
        for i in range(8):
            self.sems[("cc", i)] = stack.enter_context(self.nc.semaphore("cc%d" % i))

    def _wait(self, eng, semkey, val):
        k = (eng, semkey)
        if self.waited.get(k, 0) >= val:
            return
        self.waited[k] = val
        self.eng[eng].wait_ge(self.sems[semkey], val)

    skip = False
    limit = int(os.environ.get("MK_LIMIT", "0")) or None
    final = False

    def op(self, eng, fn, reads=(), writes=(), dma=False, cc=False):
        if self.skip:
            return None
        if self.limit is not None and self.n >= self.limit and not self.final:
            return None
        if os.environ.get("MK_TRACE") and self.dry:
            import inspect
            fr = inspect.currentframe().f_back
            print("OP", self.n, eng, fr.f_lineno, "dma" if dma else ("cc" if cc else ""))
        idx = self.n
        self.n += 1
        reads = list(reads)
        writes = list(writes)
        if any(b.phase for b in reads) or any(b.phase for b in writes):
            if self.PH not in writes:
                reads.append(self.PH)
        deps = set()
        for b in reads:
            if b.w is not None:
                deps.add(b.w)
        for b in writes:
            if b.w is not None:
                deps.add(b.w)
            deps.update(b.r.values())
        deps.discard(idx)
        async_op = dma or cc
        rkey = ("a", idx) if async_op else eng
        for b in writes:
            b.w = idx
            b.r = {}
        for b in reads:
            if b not in writes:
                b.r[rkey] = idx
        self.meta.append((eng, async_op))
        real = []
        for dpt in deps:
            deng, dasync = self.meta[dpt]
            if (not dasync) and deng == eng and (not async_op) and eng == "pe":
                continue
            real.append(dpt)
        if self.dry:
            for dpt in real:
                self.need.add(dpt)
            return idx
        for dpt in sorted(real):
            semkey, val = self.sigval[dpt]
            self._wait(eng, semkey, val)
        if dma:
            m = self.dma_no[eng]
            self.dma_no[eng] = m + 1
            semkey = (eng, m % NDMASEM)
            if m >= NDMASEM:
                self._wait(eng, semkey, 16 * (m // NDMASEM))
            ins = fn()
            ins.then_inc(self.sems[semkey], 16)
            self.sigval[idx] = (semkey, 16 * (m // NDMASEM + 1))
        elif cc:
            semkey = ("cc", self.ncc)
            self.ncc += 1
            ins = fn()
            ins.then_inc(self.sems[semkey])
            self.sigval[idx] = (semkey, 1)
        else:
            ins = fn()
            if idx in self.sigset:
                self.cnt[eng] += 1
                ins.then_inc(self.sems[eng], 1)
                self.sigval[idx] = (eng, self.cnt[eng])
        return idx

    def barrier(self):
        self.op("dve", lambda: self.nc.vector.engine_nop(), writes=[self.PH])


def build(nc, P):
    declared = set()
    nc._mk_declared = declared

    def din(name, shape, dt=F32):
        declared.add(name)
        return nc.dram_tensor(name, list(shape), dt, kind="ExternalInput")

    x_d = din("x", [NT, D])
    ctx_d = din("ctx", [CT, D])
    cvec_d = din("cvec", [128, 16])
    wmod_d = din("w_mod", [2, D, 6 * D])
    bmod_d = din("bmod", [128, 2 * 96])
    n1g_d = din("n1g", [128, 32])
    n2g_d = din("n2g", [128, 32])
    fing_d = din("fing", [128, 8])
    win_d = din("w_in_ext", [2, D, NCEXT])
    sink_d = din("sinkb", [128, 16])
    wf_d = din("w_fourier", [2, 512, D])
    wa_d = din("w_attn", [2, 512, D])
    wo_d = din("w_out", [2, D, D])
    need_d = STOP in ("", "ffn0", "p1_1", "pa_1", "pb0_1", "mix1", "ffn1")
    need_e = STOP in ("", "ffn1")
    wgd_d = wud_d = wdd_d = wr_d = wge_d = wue_d = wde_d = None
    if need_d:
        wgd_d = din("w_gate_d", [D, DFF])
        wud_d = din("w_up_d", [D, DFF])
        wdd_d = din("w_down_d", [DFF, D])
    if need_e:
        wr_d = din("w_router", [D, 8])
        wge_d = din("w_gate_e", [8, D, DFE])
        wue_d = din("w_up_e", [8, D, DFE])
        wde_d = din("w_down_e", [8, DFE, D])
    ident_d = din("ident", [128, 128])
    ones_d = din("ones1024", [128, 128], BF16)
    cos_d = din("ropecos", [128, NT], BF16)
    sin_d = din("ropesin", [128, NT], BF16)
    masks_d = din("masks", [128, 4 * 512], BF16)
    sel_d = din("sel", [128, 8])
    c128_d = din("c128", [128, 256], BF16)
    tab23_d = din("tab23", [128, 128 * 32], BF16)
    s1_d = nc.dram_tensor("s1_scratch", [2, 128, 64, 512], BF16)
    B_s1 = Buf("s1")
    tabcc_d = din("tabcc", [CT, CT], BF16)
    tabsc_d = din("tabsc", [CT, CT], BF16)
    ccsc_d = din("ccsc", [128, 256], BF16)
    esel_d = din("esel", [8, 8 * 128], BF16)
    out_d = nc.dram_tensor("out", [NT, D], F32, kind="ExternalOutput")

    u_own = [nc.dram_tensor("u_own%d" % k, [1024, 512], BF16) for k in range(2)]
    u_all = [nc.dram_tensor("u_all%d" % k, [4096, 512], BF16) for k in range(2)]
    u_ctx = nc.dram_tensor("u_ctx", [CT, 512], BF16)
    halo_in = nc.dram_tensor("halo_in", [128, 1024], BF16)
    halo_all = nc.dram_tensor("halo_all", [512, 1024], BF16)
    B_uown, B_uall, B_uctx, B_hin, B_hall = Buf("uown"), Buf("uall"), Buf("uctx"), Buf("hin"), Buf("hall")

    with ExitStack() as st:
        block = st.enter_context(nc.Block())
        P.alloc_sems(st)

        sbn = [0]

        def SB(stack, name, shape, dt, phase=True):
            sbn[0] += 1
            return stack.enter_context(nc.sbuf_tensor("sb%d_%s" % (sbn[0], name), shape, dt)), Buf(name, phase)

        ps = [st.enter_context(nc.psum_tensor("ps%d" % i, [128, 512], F32)) for i in range(8)]
        psB = [Buf("ps%d" % i) for i in range(8)]
        psrr = [0]

        pslist = [list(range(8))]

        def nps():
            i = pslist[0][psrr[0] % len(pslist[0])]
            psrr[0] += 1
            return i

        XT, _ = SB(st, "XT", [128, 8, NT], F32, False)
        XB = {(c, g): Buf("XT%d_%d" % (c, g)) for c in range(8) for g in range(4)}
        XcT, _ = SB(st, "XcT", [128, 8, CT], F32, False)
        XcB = {(c, 0): Buf("XcT%d" % c) for c in range(8)}
        mod, _ = SB(st, "mod", [128, 2, 48, 2], F32, False)
        g1m, _ = SB(st, "g1m", [128, 2, 8, 2], F32, False)
        g2m, _ = SB(st, "g2m", [128, 2, 8, 2], F32, False)
        B_modl = [Buf("mod0"), Buf("mod1")]
        B_g1l = [Buf("g1m0"), Buf("g1m1")]
        B_g2l = [Buf("g2m0"), Buf("g2m1")]
        ident, B_ident = SB(st, "ident", [128, 128], F32, False)
        ones, B_ones = SB(st, "ones", [128, 128], BF16, False)
        epsb, B_eps = SB(st, "epsb", [128, 1], F32, False)
        esink, B_esink = SB(st, "esink", [128, 16], F32, False)
        fing, B_fing = SB(st, "fing", [128, 8], F32, False)
        masks, B_masks = SB(st, "masks", [128, 4, 512], BF16, False)
        sel, B_sel = SB(st, "sel", [128, 8], F32, False)
        ccsc, B_ccsc = SB(st, "ccsc", [128, 256], BF16, False)
        c128, B_c128 = SB(st, "c128", [128, 256], BF16, False)

        cv, B_cv = SB(st, "cv", [128, 16], F32, False)
        scb, B_scb = SB(st, "scb", [128, 8, 2], BF16, False)
        bm, B_bm = SB(st, "bm", [128, 2, 96], F32, False)
        ng1, B_ng1 = SB(st, "ng1", [128, 32], F32, False)
        ng2, B_ng2 = SB(st, "ng2", [128, 32], F32, False)
        tmpm, B_tmpm = SB(st, "tmpm", [128, 16], F32, False)
        LAT = dict(name="lat", res=XT, rb=XB, n=NT, s=0, tgs=[(g, g * 512, 512) for g in range(4)])
        CTX = dict(name="ctx", res=XcT, rb=XcB, n=CT, s=1, tgs=[(0, 0, CT)])

        def mm(o, lhsT, rhs, start, stop, reads, pw):
            P.op("pe", lambda: nc.tensor.matmul(o(), lhsT=lhsT(), rhs=rhs(), start=start, stop=stop),
                 reads=reads, writes=[pw])

        def stop_pt(tag):
            if STOP == tag:
                P.skip = True

        @block.sync
        def _(sync):
            def ld(dst, B, src):
                P.op("sp", lambda: nc.sync.dma_start(out=dst(), in_=src()), writes=[B], dma=True)
            ld(lambda: ident[:], B_ident, lambda: ident_d[:, :])
            ld(lambda: ones[:], B_ones, lambda: ones_d[:, :])
            ld(lambda: esink[:], B_esink, lambda: sink_d[:, :])
            ld(lambda: fing[:], B_fing, lambda: fing_d[:, :])
            ld(lambda: masks[:], B_masks, lambda: masks_d.ap().rearrange("p (m n) -> p m n", m=4))
            ld(lambda: sel[:], B_sel, lambda: sel_d[:, :])
            ld(lambda: ccsc[:], B_ccsc, lambda: ccsc_d[:, :])
            ld(lambda: c128[:], B_c128, lambda: c128_d[:, :])
            P.op("dve", lambda: nc.vector.memset(epsb[:], 1e-6), writes=[B_eps])
            P.op("act", lambda: nc.scalar.activation(out=esink[:], in_=esink[:], func=AF.Exp),
                 reads=[B_esink], writes=[B_esink])

            def mods_piece(l, piece, wmb, pi):
                wt, wb = wmb
                P.op("pool", lambda: nc.gpsimd.dma_start(
                    out=wt[:], in_=wmod_d[l, :, piece * 512:(piece + 1) * 512].rearrange("(kc p) n -> p kc n", p=128)),
                    writes=[wb], dma=True)
                for sub in range(4):
                    ch = piece * 4 + sub
                    for kc in range(8):
                        mm(lambda ch=ch: ps[pi][:, ch * 2:ch * 2 + 2],
                           lambda kc=kc, sub=sub: wt[:, kc, sub * 128:(sub + 1) * 128],
                           lambda kc=kc: scb[:, kc, :], kc == 0, kc == 7, [wb, B_scb], psB[pi])

            def mods_fin(l, pi):
                P.op("dve", lambda: nc.vector.tensor_tensor(
                    out=mod[:, l].rearrange("p c s -> p (c s)"), in0=ps[pi][:, 0:96], in1=bm[:, l, :], op=ALU.add),
                    reads=[psB[pi], B_bm], writes=[B_modl[l]])
                for (gm, Bg, ng, Bn, o) in ((g1m, B_g1l, ng1, B_ng1, 8), (g2m, B_g2l, ng2, B_ng2, 32)):
                    P.op("dve", lambda o=o: nc.vector.tensor_scalar(
                        out=tmpm[:], in0=mod[:, l, o:o + 8, :].rearrange("p c s -> p (c s)"), scalar1=1.0, scalar2=None, op0=ALU.add),
                        reads=[B_modl[l]], writes=[B_tmpm])
                    P.op("dve", lambda gm=gm, ng=ng: nc.vector.tensor_tensor(
                        out=gm[:, l].rearrange("p c s -> p (c s)"), in0=tmpm[:], in1=ng[:, l * 16:(l + 1) * 16], op=ALU.mult),
                        reads=[B_tmpm, Bn], writes=[Bg[l]])

            with ExitStack() as ph:
                xs = [SB(ph, "xs%d" % i, [128, D], F32) for i in range(2)]

                def load_T(src, nblk, res, rb):
                    for blk in range(nblk):
                        xt, xb = xs[blk % 2]
                        P.op("sp", lambda xt=xt, blk=blk: nc.sync.dma_start(out=xt[:], in_=src[blk * 128:(blk + 1) * 128, :]),
                             writes=[xb], dma=True)
                        for half in range(2):
                            pi = nps()
                            for c in range(4):
                                cc = half * 4 + c
                                P.op("pe", lambda xt=xt, pi=pi, c=c, cc=cc: nc.tensor.transpose(
                                    out=ps[pi][:, c * 128:(c + 1) * 128], in_=xt[:, cc * 128:(cc + 1) * 128], identity=ident[:]),
                                    reads=[xb, B_ident], writes=[psB[pi]])
                            g = (blk * 128) // 512
                            P.op("dve", lambda pi=pi, half=half, blk=blk: nc.vector.tensor_copy(
                                out=res[:, half * 4:(half + 1) * 4, blk * 128:(blk + 1) * 128],
                                in_=ps[pi][:].rearrange("p (c t) -> p c t", c=4)),
                                reads=[psB[pi]], writes=[rb[(half * 4 + c, g)] for c in range(4)])
                load_T(x_d, 16, XT, XB)
                load_T(ctx_d, 2, XcT, XcB)
                stop_pt("load")

                wm = [SB(ph, "wm%d" % i, [128, 8, 512], BF16) for i in range(2)]
                ld(lambda: cv[:], B_cv, lambda: cvec_d[:, :])
                ld(lambda: bm[:], B_bm, lambda: bmod_d.ap().rearrange("p (l n) -> p l n", l=2))
                ld(lambda: ng1[:], B_ng1, lambda: n1g_d[:, :])
                ld(lambda: ng2[:], B_ng2, lambda: n2g_d[:, :])
                P.op("act", lambda: nc.scalar.activation(out=cv[:], in_=cv[:], func=AF.Silu), reads=[B_cv], writes=[B_cv])
                P.op("dve", lambda: nc.vector.tensor_copy(out=scb[:], in_=cv[:].rearrange("p (k s) -> p k s", s=2)),
                     reads=[B_cv], writes=[B_scb])
                mpi0 = nps()
                for piece in range(12):
                    mods_piece(0, piece, wm[piece % 2], mpi0)
                mods_fin(0, mpi0)
                P.barrier()
                stop_pt("mods")

            def norm_tile(ph_bufs, ts, g, off, n, gm_ap, sh_ap, hT, hB, extra_reads, hout=None, sqb=None):
                _sq, _B_sq, rstd, B_rstd, tt = ph_bufs
                if hout is None:
                    hout = lambda c: hT[:, c, :n]
                sq, B_sq = sqb if sqb is not None else (hT, hB)
                res, rb = ts["res"], ts["rb"]
                for c in range(8):
                    P.op("act", lambda c=c: nc.scalar.activation(out=sq[:, c, :n], in_=res[:, c, off:off + n], func=AF.Square),
                         reads=[rb[(c, g)]], writes=[B_sq])
                pi = nps()
                for c in range(8):
                    mm(lambda: ps[pi][:, :n], lambda: ones[:], lambda c=c: sq[:, c, :n], c == 0, c == 7, [B_ones, B_sq], psB[pi])
                P.op("act", lambda: nc.scalar.activation(out=rstd[:, :n], in_=ps[pi][:, :n], func=AF.Sqrt, bias=epsb[:, 0:1], scale=1.0),
                     reads=[psB[pi], B_eps], writes=[B_rstd])
                P.op("dve", lambda: nc.vector.reciprocal(out=rstd[:, :n], in_=rstd[:, :n]), reads=[B_rstd], writes=[B_rstd])
                for c in range(8):
                    t, tb = tt[c % 2]
                    if sh_ap is None:
                        P.op("dve", lambda c=c: nc.vector.scalar_tensor_tensor(
                            out=hout(c), in0=res[:, c, off:off + n], scalar=gm_ap(c), in1=rstd[:, :n], op0=ALU.mult, op1=ALU.mult),
                            reads=[rb[(c, g)], B_rstd] + extra_reads, writes=[hB])
                    else:
                        P.op("dve", lambda c=c, t=t: nc.vector.scalar_tensor_tensor(
                            out=t[:, :n], in0=res[:, c, off:off + n], scalar=gm_ap(c), in1=rstd[:, :n], op0=ALU.mult, op1=ALU.mult),
                            reads=[rb[(c, g)], B_rstd] + extra_reads, writes=[tb])
                        P.op("act", lambda c=c, t=t: nc.scalar.activation(
                            out=hout(c), in_=t[:, :n], func=AF.Identity, bias=sh_ap(c), scale=1.0),
                            reads=[tb] + extra_reads, writes=[hB])

            def norm_bufs(ph):
                sq, B_sq = None, None
                rstd, B_rstd = SB(ph, "rstd", [128, 512], F32)
                tt = [SB(ph, "nt%d" % i, [128, 512], F32) for i in range(2)]
                return (sq, B_sq, rstd, B_rstd, tt)

            def wload(wt, wb, src):
                P.op("pool", lambda: nc.gpsimd.dma_start(out=wt(), in_=src()), writes=[wb], dma=True)

            for l in range(2):
                last = (l == 1)
                with ExitStack() as lay:
                    OT, B_OT = SB(lay, "OT", [128, 4, NT], BF16)
                    OcT, B_OcT = SB(lay, "OcT", [128, 4, CT], BF16)
                    FT, B_FT = SB(lay, "FT", [128, 4, NT], BF16)
                    FcT, B_FcT = SB(lay, "FcT", [128, 4, CT], BF16)
                    att = ExitStack()
                    KT, B_KT = SB(att, "KT", [128, 2, NT], BF16)
                    Vown, B_V = SB(att, "Vown", [128, 16, 2, 80], BF16)
                    KcT, B_KcT = SB(att, "KcT", [128, 2, CT], BF16)
                    Vc, B_Vc = SB(att, "Vc", [128, 2, 2, 80], BF16)
                    hp, B_hp = SB(att, "hp", [128, 1024], BF16)
                    hn, B_hn = SB(att, "hn", [128, 1024], BF16)
                    P.op("pool", lambda: nc.gpsimd.memset(Vown[:], 1.0), writes=[B_V])
                    P.op("pool", lambda: nc.gpsimd.memset(Vc[:], 1.0), writes=[B_Vc])

                    def mods(ts, base):
                        s = ts["s"]
                        return lambda c: mod[:, l, base + c, s:s + 1]

                    with ExitStack() as ph:
                        nb = norm_bufs(ph)
                        hTs = [SB(ph, "hT%d" % i, [128, 8, 512], BF16) for i in range(2)]
                        hq = [0]
                        wu, B_wu = SB(ph, "wu", [128, 8, 512], BF16)
                        wk, B_wk = SB(ph, "wk", [128, 8, 512], BF16)
                        wv, B_wv = SB(ph, "wv", [128, 8, 128], BF16)
                        rc, B_rc = SB(ph, "rc", [128, NT], BF16)
                        rs, B_rs = SB(ph, "rs", [128, NT], BF16)
                        ust = [SB(ph, "ust%d" % i, [128, 512], BF16) for i in range(2)]
                        r1 = [SB(ph, "r1_%d" % i, [128, 512], F32) for i in range(2)]
                        r2 = [SB(ph, "r2_%d" % i, [128, 512], F32) for i in range(2)]
                        wsrc = lambda o, n: (lambda: win_d[l, :, o:o + n].rearrange("(kc p) n -> p kc n", p=128))
                        wload(lambda: wu[:], B_wu, wsrc(O_U, 512))
                        wload(lambda: wk[:], B_wk, wsrc(O_K, 512))
                        wload(lambda: wv[:], B_wv, wsrc(O_V, 128))
                        ld(lambda: rc[:], B_rc, lambda: cos_d[:, :])
                        ld(lambda: rs[:], B_rs, lambda: sin_d[:, :])
                        uq = [0]
                        for ts in ([CTX, LAT]):
                            isl = ts is LAT
                            for (g, off, n) in ts["tgs"]:
                                hT, B_hT = hTs[hq[0] % 2]
                                hq[0] += 1
                                norm_tile(nb, ts, g, off, n, lambda c, ts=ts: g1m[:, l, c, ts["s"]:ts["s"] + 1], mods(ts, 0), hT, B_hT, [B_g1l[l], B_modl[l]])
                                for b in range(n // 128):
                                    gb = off // 128 + b
                                    pi = nps()
                                    for kc in range(8):
                                        mm(lambda pi=pi: ps[pi][:, :], lambda kc=kc, b=b: hT[:, kc, b * 128:(b + 1) * 128],
                                           lambda kc=kc: wu[:, kc, :], kc == 0, kc == 7, [B_hT, B_wu], psB[pi])
                                    ut, ub = ust[uq[0] % 2]
                                    uq[0] += 1
                                    P.op("dve", lambda pi=pi, ut=ut: nc.vector.tensor_copy(out=ut[:], in_=ps[pi][:]), reads=[psB[pi]], writes=[ub])
                                    udst, uB = (u_own[gb // 8], B_uown) if isl else (u_ctx, B_uctx)
                                    gr = gb % 8 if isl else gb
                                    P.op("sp", lambda ut=ut, udst=udst, gr=gr: nc.sync.dma_start(out=udst[gr * 128:(gr + 1) * 128, :], in_=ut[:]),
                                         reads=[ub], writes=[uB], dma=True)
                                    pi = nps()
                                    for kc in range(8):
                                        mm(lambda pi=pi: ps[pi][:, 0:128], lambda kc=kc, b=b: hT[:, kc, b * 128:(b + 1) * 128],
                                           lambda kc=kc: wv[:, kc, :], kc == 0, kc == 7, [B_hT, B_wv], psB[pi])
                                    vdst, vB = (Vown, B_V) if isl else (Vc, B_Vc)
                                    P.op("dve", lambda pi=pi, vdst=vdst, gb=gb: nc.vector.tensor_copy(
                                        out=vdst[:, gb, :, 0:64], in_=ps[pi][:, 0:128].rearrange("p (h d) -> p h d", h=2)),
                                        reads=[psB[pi]], writes=[vB])
                                for h in range(2):
                                    pa = nps()
                                    for kc in range(8):
                                        mm(lambda pa=pa: ps[pa][0:64, :n], lambda kc=kc, h=h: wk[:, kc, h * 128:h * 128 + 64],
                                           lambda kc=kc: hT[:, kc, :n], kc == 0, kc == 7, [B_hT, B_wk], psB[pa])
                                    if not isl:
                                        P.op("dve", lambda pa=pa, h=h: nc.vector.tensor_copy(out=KcT[0:64, h, :], in_=ps[pa][0:64, :n]),
                                             reads=[psB[pa]], writes=[B_KcT])
                                        continue
                                    pb = nps()
                                    for kc in range(8):
                                        mm(lambda pb=pb: ps[pb][0:64, :n], lambda kc=kc, h=h: wk[:, kc, (2 + h) * 128:(2 + h) * 128 + 64],
                                           lambda kc=kc: hT[:, kc, :n], kc == 0, kc == 7, [B_hT, B_wk], psB[pb])
                                    t1, b1 = r1[h]
                                    t2, b2 = r2[h]
                                    P.op("dve", lambda pa=pa, t1=t1, off=off: nc.vector.tensor_tensor(out=t1[0:64, :], in0=ps[pa][0:64, :], in1=rc[0:64, off:off + 512], op=ALU.mult),
                                         reads=[psB[pa], B_rc], writes=[b1])
                                    P.op("dve", lambda pb=pb, t2=t2, off=off: nc.vector.tensor_tensor(out=t2[0:64, :], in0=ps[pb][0:64, :], in1=rs[0:64, off:off + 512], op=ALU.mult),
                                         reads=[psB[pb], B_rs], writes=[b2])
                                    P.op("pool", lambda t1=t1, t2=t2, h=h, off=off: nc.gpsimd.tensor_tensor(out=KT[0:64, h, off:off + 512], in0=t1[0:64, :], in1=t2[0:64, :], op=ALU.add),
                                         reads=[b1, b2], writes=[B_KT])
                        stop_pt("p1b")
                        P.op("sp", lambda: nc.sync.dma_start(out=halo_in[:, 0:256].rearrange("p (h t) -> p h t", h=2), in_=KT[:, :, 0:128]),
                             reads=[B_KT], writes=[B_hin], dma=True)
                        P.op("sp", lambda: nc.sync.dma_start(out=halo_in[:, 256:512].rearrange("p (h t) -> p h t", h=2), in_=KT[:, :, NT - 128:NT]),
                             reads=[B_KT], writes=[B_hin], dma=True)
                        P.op("sp", lambda: nc.sync.dma_start(out=halo_in[:, 512:672], in_=Vown[:, 0].rearrange("p h e -> p (h e)")),
                             reads=[B_V], writes=[B_hin], dma=True)
                        P.op("sp", lambda: nc.sync.dma_start(out=halo_in[:, 672:832], in_=Vown[:, 15].rearrange("p h e -> p (h e)")),
                             reads=[B_V], writes=[B_hin], dma=True)
                        stop_pt("p1c")
                        P.op("pool", lambda: nc.gpsimd.collective_compute("AllGather", ALU.bypass, replica_groups=[[0, 1, 2, 3], [4, 5, 6, 7]],
                                                                          ins=[halo_in.ap().opt()], outs=[halo_all.ap().opt()]),
                             reads=[B_hin], writes=[B_hall], cc=True)
                        for k2 in range(2):
                            P.op("pool", lambda k2=k2: nc.gpsimd.collective_compute("AllGather", ALU.bypass, replica_groups=[[0, 1, 2, 3], [4, 5, 6, 7]],
                                                                                    ins=[u_own[k2].ap().opt()], outs=[u_all[k2].ap().opt()]),
                                 reads=[B_uown], writes=[B_uall], cc=True)
                        stop_pt("p1d")
                        hall, B_hl = SB(ph, "hall", [128, 4, 1024], BF16)
                        P.op("sp", lambda: nc.sync.dma_start(out=hall[:], in_=halo_all.ap().rearrange("(r p) n -> p r n", p=128)),
                             reads=[B_hall], writes=[B_hl], dma=True)
                        for (dst, Bd, so) in ((hp, B_hp, 0), (hn, B_hn, 4)):
                            P.op("dve", lambda dst=dst, so=so: nc.vector.tensor_scalar(out=dst[:, 0:832], in0=hall[:, 0, 0:832], scalar1=sel[:, so:so + 1], scalar2=None, op0=ALU.mult),
                                 reads=[B_hl, B_sel], writes=[Bd])
                            for r in range(1, 4):
                                P.op("dve", lambda dst=dst, so=so, r=r: nc.vector.scalar_tensor_tensor(
                                    out=dst[:, 0:832], in0=hall[:, r, 0:832], scalar=sel[:, so + r:so + r + 1], in1=dst[:, 0:832], op0=ALU.mult, op1=ALU.add),
                                    reads=[B_hl, B_sel], writes=[Bd])
                        P.barrier()
                        stop_pt("p1_%d" % l)

                    with ExitStack() as ph:
                        nb = norm_bufs(ph)
                        hT, B_hT = SB(ph, "hT", [128, 8, 512], BF16)
                        wq, B_wq = SB(ph, "wq", [128, 8, 1024], BF16)
                        rc, B_rc = SB(ph, "rc", [128, NT], BF16)
                        rs, B_rs = SB(ph, "rs", [128, NT], BF16)
                        QT, B_QT = SB(ph, "QT", [128, 8, 512], BF16)
                        r1 = [SB(ph, "r1_%d" % i, [128, 512], F32) for i in range(2)]
                        r2 = [SB(ph, "r2_%d" % i, [128, 512], F32) for i in range(2)]
                        pt = [SB(ph, "pt%d" % i, [128, 512], BF16) for i in range(6)]
                        pslist[0] = [0, 1, 2, 3]
                        qcount = [0]
                        Ons = [SB(ph, "On%d" % i, [128, 512], F32) for i in range(2)]
                        pendT = []
                        den, B_den = SB(ph, "den", [128, 8], F32)
                        wload(lambda: wq[:], B_wq, lambda: win_d[l, :, O_Q:O_Q + 1024].rearrange("(kc p) n -> p kc n", p=128))
                        ld(lambda: rc[:], B_rc, lambda: cos_d[:, :])
                        ld(lambda: rs[:], B_rs, lambda: sin_d[:, :])
                        eq = [0]
                        for ts in ([LAT] if last else [CTX, LAT]):
                            isl = ts is LAT
                            for (g, off, n) in ts["tgs"]:
                                norm_tile(nb, ts, g, off, n, lambda c, ts=ts: g1m[:, l, c, ts["s"]:ts["s"] + 1], mods(ts, 0), hT, B_hT, [B_g1l[l], B_modl[l]])
                                for hd in range(8):
                                    pa = nps()
                                    for kc in range(8):
                                        mm(lambda pa=pa: ps[pa][0:64, :n], lambda kc=kc, hd=hd: wq[:, kc, hd * 64:(hd + 1) * 64],
                                           lambda kc=kc: hT[:, kc, :n], kc == 0, kc == 7, [B_hT, B_wq], psB[pa])
                                    if not isl:
                                        P.op("dve", lambda pa=pa, hd=hd: nc.vector.tensor_copy(out=QT[0:64, hd, :n], in_=ps[pa][0:64, :n]),
                                             reads=[psB[pa]], writes=[B_QT])
                                        continue
                                    pb = nps()
                                    for kc in range(8):
                                        mm(lambda pb=pb: ps[pb][0:64, :n], lambda kc=kc, hd=hd: wq[:, kc, 512 + hd * 64:512 + (hd + 1) * 64],
                                           lambda kc=kc: hT[:, kc, :n], kc == 0, kc == 7, [B_hT, B_wq], psB[pb])
                                    t1, b1 = r1[hd % 2]
                                    t2, b2 = r2[hd % 2]
                                    P.op("dve", lambda pa=pa, t1=t1, off=off: nc.vector.tensor_tensor(out=t1[0:64, :], in0=ps[pa][0:64, :], in1=rc[0:64, off:off + 512], op=ALU.mult),
                                         reads=[psB[pa], B_rc], writes=[b1])
                                    P.op("dve", lambda pb=pb, t2=t2, off=off: nc.vector.tensor_tensor(out=t2[0:64, :], in0=ps[pb][0:64, :], in1=rs[0:64, off:off + 512], op=ALU.mult),
                                         reads=[psB[pb], B_rs], writes=[b2])
                                    P.op("pool", lambda t1=t1, t2=t2, hd=hd: nc.gpsimd.tensor_tensor(out=QT[0:64, hd, :], in0=t1[0:64, :], in1=t2[0:64, :], op=ALU.add),
                                         reads=[b1, b2], writes=[B_QT])
                                for qb in range(n // 128):
                                    gb = off // 128 + qb
                                    po2 = [4 + (qcount[0] % 2) * 2 + h for h in range(2)]
                                    On, B_On = Ons[qcount[0] % 2]
                                    qcount[0] += 1
                                    keysets = []
                                    for h in range(2):
                                        keys = []
                                        if isl:
                                            if gb == 0:
                                                keys.append((lambda h=h: hp[0:64, 256 + h * 128:256 + (h + 1) * 128],
                                                             lambda h=h: hp[:, 672 + h * 80:672 + h * 80 + 65], 2, [B_hp]))
                                            else:
                                                keys.append((lambda h=h, gb=gb: KT[0:64, h, (gb - 1) * 128:gb * 128],
                                                             lambda h=h, gb=gb: Vown[:, gb - 1, h, 0:65], 0, [B_KT, B_V]))
                                            keys.append((lambda h=h, gb=gb: KT[0:64, h, gb * 128:(gb + 1) * 128],
                                                         lambda h=h, gb=gb: Vown[:, gb, h, 0:65], None, [B_KT, B_V]))
                                            if gb == 15:
                                                keys.append((lambda h=h: hn[0:64, h * 128:(h + 1) * 128],
                                                             lambda h=h: hn[:, 512 + h * 80:512 + h * 80 + 65], 3, [B_hn]))
                                            else:
                                                keys.append((lambda h=h, gb=gb: KT[0:64, h, (gb + 1) * 128:(gb + 2) * 128],
                                                             lambda h=h, gb=gb: Vown[:, gb + 1, h, 0:65], 1, [B_KT, B_V]))
                                        for cb in range(2):
                                            keys.append((lambda h=h, cb=cb: KcT[0:64, h, cb * 128:(cb + 1) * 128],
                                                         lambda h=h, cb=cb: Vc[:, cb, h, 0:65], None, [B_KcT, B_Vc]))
                                        keysets.append(keys)
                                    nk = len(keysets[0])
                                    jobs = [(h, ki) for ki in range(nk) for h in range(2)]
                                    LAG = 3
                                    pend = []
                                    for s_ in range(len(jobs) + LAG):
                                        if s_ < len(jobs):
                                            h, ki = jobs[s_]
                                            kap, vap, mk, kr = keysets[h][ki]
                                            pi = nps()
                                            mm(lambda pi=pi: ps[pi][:, :], lambda kap=kap: kap(),
                                               lambda h=h, qb=qb: QT[0:64, 4 * h:4 * h + 4, qb * 128:(qb + 1) * 128],
                                               True, True, [B_QT] + kr, psB[pi])
                                            ptt, pb_ = pt[eq[0] % len(pt)]
                                            eq[0] += 1
                                            P.op("act", lambda pi=pi, ptt=ptt: nc.scalar.activation(out=ptt[:], in_=ps[pi][:], func=AF.Exp, scale=0.125),
                                                 reads=[psB[pi]], writes=[pb_])
                                            if mk is not None:
                                                P.op("pool", lambda ptt=ptt, mk=mk: nc.gpsimd.tensor_tensor(out=ptt[:], in0=ptt[:], in1=masks[:, mk, :], op=ALU.mult),
                                                     reads=[pb_, B_masks], writes=[pb_])
                                            pend.append((h, ki, ptt, pb_, vap, kr))
                                        if s_ == min(8, len(jobs) - 1) and pendT:
                                            pendT.pop(0)()
                                        if s_ >= LAG:
                                            h, ki, ptt, pb_, vap, kr = pend[s_ - LAG]
                                            po = po2[h]
                                            for gi in range(4):
                                                col = gi * 128
                                                mm(lambda po=po, gi=gi: ps[po][:, gi * 80:gi * 80 + 65], lambda ptt=ptt, col=col: ptt[:, col:col + 128],
                                                   lambda vap=vap: vap(), ki == 0, ki == nk - 1, [pb_] + kr, psB[po])
                                    for h in range(2):
                                        po = po2[h]
                                        P.op("dve", lambda po=po, h=h: nc.vector.tensor_tensor(
                                            out=den[:, 4 * h:4 * h + 4], in0=ps[po][:, 0:320].rearrange("p (g e) -> p g e", e=80)[:, :, 64],
                                            in1=esink[:, l * 8 + 4 * h:l * 8 + 4 * h + 4], op=ALU.add),
                                            reads=[psB[po], B_esink], writes=[B_den])
                                    P.op("dve", lambda: nc.vector.reciprocal(out=den[:], in_=den[:]), reads=[B_den], writes=[B_den])
                                    for h in range(2):
                                        po = po2[h]
                                        for gi in range(4):
                                            P.op("dve", lambda po=po, gi=gi, h=h: nc.vector.tensor_scalar(
                                                out=On[:, (4 * h + gi) * 64:(4 * h + gi + 1) * 64], in0=ps[po][:, gi * 80:gi * 80 + 64],
                                                scalar1=den[:, 4 * h + gi:4 * h + gi + 1], scalar2=None, op0=ALU.mult),
                                                reads=[psB[po], B_den], writes=[B_On])
                                    def emit_T(On=On, B_On=B_On, gb=gb, isl=isl):
                                        pi = nps()
                                        for c4 in range(4):
                                            P.op("pe", lambda pi=pi, c4=c4: nc.tensor.transpose(out=ps[pi][:, c4 * 128:(c4 + 1) * 128], in_=On[:, c4 * 128:(c4 + 1) * 128], identity=ident[:]),
                                                 reads=[B_On, B_ident], writes=[psB[pi]])
                                        odst, oB = (OT, B_OT) if isl else (OcT, B_OcT)
                                        P.op("dve", lambda pi=pi, odst=odst: nc.vector.tensor_copy(
                                            out=odst[:, :, gb * 128:(gb + 1) * 128], in_=ps[pi][:].rearrange("p (c t) -> p c t", c=4)),
                                            reads=[psB[pi]], writes=[oB])
                                    pendT.append(emit_T)
                                while pendT:
                                    pendT.pop(0)()
                        pslist[0] = list(range(8))
                        P.barrier()
                        stop_pt("pa_%d" % l)
                    att.close()

                    with ExitStack() as ph:
                        ub = [SB(ph, "ub%d" % i, [128, 512], BF16) for i in range(3)]
                        tcb = [SB(ph, "tcb%d" % i, [128, 512], BF16) for i in range(3)]
                        tsb = [SB(ph, "tsb%d" % i, [128, 512], BF16) for i in range(3)]
                        yri, B_yri = SB(ph, "yri", [128, 8, 512], BF16)
                        dq = [0]
                        for ts in ([] if last else [CTX]):
                            isl = ts is LAT
                            uB = B_uall if isl else B_uctx
                            tc_d, ts_d = (tabcc_d, tabsc_d)
                            nblk = 64 if isl else 2
                            for (g, off, n) in ts["tgs"]:
                                acc = [nps() for _ in range(8)]
                                for nbk in range(nblk):
                                    i3 = dq[0] % 3
                                    dq[0] += 1
                                    (ut, uBb), (ct, cB), (st_, sB_) = ub[i3], tcb[i3], tsb[i3]
                                    if not isl:
                                        usrc, urow = u_ctx, nbk * 128
                                        P.op("sp", lambda ut=ut, usrc=usrc, urow=urow: nc.sync.dma_start(out=ut[:], in_=usrc[urow:urow + 128, :]),
                                             reads=[uB], writes=[uBb], dma=True)
                                        uap = lambda gg, ut=ut: ut[:, gg * 128:(gg + 1) * 128]

                                    P.op("sp", lambda ct=ct, tc_d=tc_d, nbk=nbk, off=off, n=n: nc.sync.dma_start(out=ct[:, :n], in_=tc_d[nbk * 128:(nbk + 1) * 128, off:off + n]),
                                         writes=[cB], dma=True)
                                    P.op("sp", lambda st_=st_, ts_d=ts_d, nbk=nbk, off=off, n=n: nc.sync.dma_start(out=st_[:, :n], in_=ts_d[nbk * 128:(nbk + 1) * 128, off:off + n]),
                                         writes=[sB_], dma=True)
                                    for gg in range(4):
                                        mm(lambda gg=gg: ps[acc[gg]][:, :n], lambda uap=uap, gg=gg: uap(gg), lambda ct=ct: ct[:, :n],
                                           nbk == 0, nbk == nblk - 1, [uBb, cB], psB[acc[gg]])
                                        mm(lambda gg=gg: ps[acc[4 + gg]][:, :n], lambda uap=uap, gg=gg: uap(gg), lambda st_=st_: st_[:, :n],
                                           nbk == 0, nbk == nblk - 1, [uBb, sB_], psB[acc[4 + gg]])
                                for j in range(8):
                                    P.op("dve", lambda j=j: nc.vector.tensor_copy(out=yri[:, j, :n], in_=ps[acc[j]][:, :n]), reads=[psB[acc[j]]], writes=[B_yri])
                                fdst, fB = (FT, B_FT) if isl else (FcT, B_FcT)
                                for gg in range(4):
                                    pi = nps()
                                    mm(lambda pi=pi: ps[pi][:, :n], lambda: ccsc[:, 0:128], lambda gg=gg: yri[:, gg, :n], True, False, [B_ccsc, B_yri], psB[pi])
                                    mm(lambda pi=pi: ps[pi][:, :n], lambda: ccsc[:, 128:256], lambda gg=gg: yri[:, 4 + gg, :n], False, True, [B_ccsc, B_yri], psB[pi])
                                    P.op("dve", lambda pi=pi, fdst=fdst, gg=gg, off=off, n=n: nc.vector.tensor_copy(out=fdst[:, gg, off:off + n], in_=ps[pi][:, :n]),
                                         reads=[psB[pi]], writes=[fB])
                        P.barrier()
                        stop_pt("pb0c_%d" % l)
                    with ExitStack() as ph:
                        Tb_ = [SB(ph, "ctT%d" % i, [128, 8, 512], BF16) for i in range(2)]
                        st1 = [SB(ph, "st1_%d" % i, [128, 2, 8, 512], BF16) for i in range(2)]
                        def ct_loads(n2c):
                            Tt, TB = Tb_[n2c % 2]
                            for r_ in range(4):
                                for k2_ in range(2):
                                    P.op("sp", lambda Tt=Tt, r_=r_, k2_=k2_, n2c=n2c: nc.sync.dma_start(
                                        out=Tt[r_ * 32 + k2_ * 16:r_ * 32 + k2_ * 16 + 16, :, :],
                                        in_=u_all[k2_][r_ * 1024:(r_ + 1) * 1024, :].rearrange("(a n) c -> a n c", n=64)[:, n2c * 8:(n2c + 1) * 8, :]),
                                        reads=[B_uall], writes=[TB], dma=True)
                        ct_loads(0)
                        for n2c in range(8):
                            Tt, TB = Tb_[n2c % 2]
                            s1t, s1B = st1[n2c % 2]
                            if n2c + 1 < 8:
                                ct_loads(n2c + 1)
                            for n2i in range(8):
                                pr, pi_ = nps(), nps()
                                mm(lambda pr=pr: ps[pr][:, :], lambda: c128[:, 0:128], lambda Tt=Tt, n2i=n2i: Tt[:, n2i, :], True, True, [B_c128, TB], psB[pr])
                                mm(lambda pi_=pi_: ps[pi_][:, :], lambda: c128[:, 128:256], lambda Tt=Tt, n2i=n2i: Tt[:, n2i, :], True, True, [B_c128, TB], psB[pi_])
                                P.op("act", lambda pr=pr, s1t=s1t, n2i=n2i: nc.scalar.copy(out=s1t[:, 0, n2i, :], in_=ps[pr][:, :]), reads=[psB[pr]], writes=[s1B])
                                P.op("dve", lambda pi_=pi_, s1t=s1t, n2i=n2i: nc.vector.tensor_copy(out=s1t[:, 1, n2i, :], in_=ps[pi_][:, :]), reads=[psB[pi_]], writes=[s1B])
                            for ri in range(2):
                                P.op("sp", lambda s1t=s1t, ri=ri, n2c=n2c: nc.sync.dma_start(out=s1_d[ri, :, n2c * 8:(n2c + 1) * 8, :], in_=s1t[:, ri, :, :]),
                                     reads=[s1B], writes=[B_s1], dma=True)
                        P.barrier()
                        stop_pt("pb0s1_%d" % l)
                    with ExitStack() as ph:
                        Yb, B_Yb = SB(ph, "ctY", [128, 4, 2, NT], BF16)
                        Rb_ = [SB(ph, "ctR%d" % i, [128, 8, 512], BF16) for i in range(2)]
                        t23, B_t23 = SB(ph, "t23", [128, 128, 32], BF16)
                        ld(lambda: t23[:], B_t23, lambda: tab23_d.ap().rearrange("p (k c) -> p k c", c=32))
                        acc = None
                        for k1c in range(16):
                            Rt, RB = Rb_[k1c % 2]
                            for ri in range(2):
                                P.op("sp", lambda Rt=Rt, ri=ri, k1c=k1c: nc.sync.dma_start(
                                    out=Rt[ri * 64:(ri + 1) * 64, :, :], in_=s1_d[ri, k1c * 8:(k1c + 1) * 8, :, :].rearrange("k n c -> n k c")),
                                    reads=[B_s1], writes=[RB], dma=True)
                            if k1c % 2 == 0:
                                acc = [nps() for _ in range(4)]
                            for k1i in range(8):
                                k1 = k1c * 8 + k1i
                                for gg in range(4):
                                    mm(lambda gg=gg, k1=k1: ps[acc[gg]][:, (k1 % 16) * 32:(k1 % 16) * 32 + 32],
                                       lambda Rt=Rt, k1i=k1i, gg=gg: Rt[:, k1i, gg * 128:(gg + 1) * 128],
                                       lambda k1=k1: t23[:, k1, :], True, True, [RB, B_t23], psB[acc[gg]])
                            if k1c % 2 == 1:
                                kbase = (k1c - 1) * 8
                                for gg in range(4):
                                    for ro in range(2):
                                        src = lambda gg=gg, ro=ro: ps[acc[gg]][:, :].rearrange("p (a r b) -> p a r b", r=2, b=16)[:, :, ro, :]
                                        dst = lambda gg=gg, ro=ro, kbase=kbase: Yb[:, gg, ro, :].rearrange("p (b a) -> p a b", a=128)[:, kbase:kbase + 16, :]
                                        if gg % 2 == 0:
                                            P.op("act", lambda src=src, dst=dst: nc.scalar.copy(out=dst(), in_=src()), reads=[psB[acc[gg]]], writes=[B_Yb])
                                        else:
                                            P.op("dve", lambda src=src, dst=dst: nc.vector.tensor_copy(out=dst(), in_=src()), reads=[psB[acc[gg]]], writes=[B_Yb])
                        for tg_ in range(4):
                            for gg in range(4):
                                pi = nps()
                                mm(lambda pi=pi: ps[pi][:, :], lambda: ccsc[:, 0:128], lambda gg=gg, tg_=tg_: Yb[:, gg, 0, tg_ * 512:(tg_ + 1) * 512], True, False, [B_ccsc, B_Yb], psB[pi])
                                mm(lambda pi=pi: ps[pi][:, :], lambda: ccsc[:, 128:256], lambda gg=gg, tg_=tg_: Yb[:, gg, 1, tg_ * 512:(tg_ + 1) * 512], False, True, [B_ccsc, B_Yb], psB[pi])
                                P.op("dve", lambda pi=pi, gg=gg, tg_=tg_: nc.vector.tensor_copy(out=FT[:, gg, tg_ * 512:(tg_ + 1) * 512], in_=ps[pi][:, :]),
                                     reads=[psB[pi]], writes=[B_FT])
                        P.barrier()
                        stop_pt("pb0_%d" % l)

                    with ExitStack() as ph:
                        nb = norm_bufs(ph)
                        hT, B_hT = SB(ph, "hT", [128, 8, 256], BF16)
                        wg2, B_wg2 = SB(ph, "wg2", [128, 8, 2048], BF16)
                        wf, B_wf = SB(ph, "wf", [128, 4, D], BF16)
                        wa, B_wa = SB(ph, "wa", [128, 4, D], BF16)
                        wo, B_wo = SB(ph, "wo", [128, 8, D], BF16)
                        yT, B_yT = SB(ph, "yT", [128, 8, 256], BF16)
                        sg = [SB(ph, "sg%d" % i, [128, 256], F32) for i in range(4)]
                        tm = [SB(ph, "tm%d" % i, [128, 256], F32) for i in range(4)]
                        wload(lambda: wf[:], B_wf, lambda: wf_d[l].rearrange("(kc p) n -> p kc n", p=128))
                        wload(lambda: wa[:], B_wa, lambda: wa_d[l].rearrange("(kc p) n -> p kc n", p=128))
                        wload(lambda: wg2[:], B_wg2, lambda: win_d[l, :, O_G:O_G + 2048].rearrange("(kc p) n -> p kc n", p=128))
                        wload(lambda: wo[:], B_wo, lambda: wo_d[l].rearrange("(kc p) n -> p kc n", p=128))
                        for ts in ([LAT] if last else [CTX, LAT]):
                            isl = ts is LAT
                            fsrc, fB = (FT, B_FT) if isl else (FcT, B_FcT)
                            osrc, oB = (OT, B_OT) if isl else (OcT, B_OcT)
                            s = ts["s"]
                            for (g, off, n) in [(o_ // 512, o_, 256) for o_ in range(0, ts["n"], 256)]:
                                norm_tile(nb, ts, g, off, n, lambda c, s=s: g1m[:, l, c, s:s + 1], mods(ts, 0), hT, B_hT, [B_g1l[l], B_modl[l]])
                                for dc in range(8):
                                    pF, pA, pGf, pGa = nps(), nps(), nps(), nps()
                                    for gg in range(4):
                                        mm(lambda pF=pF: ps[pF][:, :n], lambda gg=gg, dc=dc: wf[:, gg, dc * 128:(dc + 1) * 128],
                                           lambda gg=gg: fsrc[:, gg, off:off + n], gg == 0, gg == 3, [B_wf, fB], psB[pF])
                                    for gg in range(4):
                                        mm(lambda pA=pA: ps[pA][:, :n], lambda gg=gg, dc=dc: wa[:, gg, dc * 128:(dc + 1) * 128],
                                           lambda gg=gg: osrc[:, gg, off:off + n], gg == 0, gg == 3, [B_wa, oB], psB[pA])
                                    for kc in range(8):
                                        mm(lambda pGf=pGf: ps[pGf][:, :n], lambda kc=kc, dc=dc: wg2[:, kc, dc * 128:(dc + 1) * 128],
                                           lambda kc=kc: hT[:, kc, :n], kc == 0, kc == 7, [B_wg2, B_hT], psB[pGf])
                                    for kc in range(8):
                                        mm(lambda pGa=pGa: ps[pGa][:, :n], lambda kc=kc, dc=dc: wg2[:, kc, D + dc * 128:D + (dc + 1) * 128],
                                           lambda kc=kc: hT[:, kc, :n], kc == 0, kc == 7, [B_wg2, B_hT], psB[pGa])
                                    i0, i1 = (dc % 2) * 2, (dc % 2) * 2 + 1
                                    P.op("act", lambda pGf=pGf, i0=i0: nc.scalar.activation(out=sg[i0][0][:, :n], in_=ps[pGf][:, :n], func=AF.Sigmoid),
                                         reads=[psB[pGf]], writes=[sg[i0][1]])
                                    P.op("act", lambda pGa=pGa, i1=i1: nc.scalar.activation(out=sg[i1][0][:, :n], in_=ps[pGa][:, :n], func=AF.Sigmoid),
                                         reads=[psB[pGa]], writes=[sg[i1][1]])
                                    P.op("dve", lambda pF=pF, i0=i0: nc.vector.tensor_tensor(out=tm[i0][0][:, :n], in0=ps[pF][:, :n], in1=sg[i0][0][:, :n], op=ALU.mult),
                                         reads=[psB[pF], sg[i0][1]], writes=[tm[i0][1]])
                                    P.op("dve", lambda pA=pA, i1=i1: nc.vector.tensor_tensor(out=tm[i1][0][:, :n], in0=ps[pA][:, :n], in1=sg[i1][0][:, :n], op=ALU.mult),
                                         reads=[psB[pA], sg[i1][1]], writes=[tm[i1][1]])
                                    P.op("pool", lambda i0=i0, i1=i1, dc=dc: nc.gpsimd.tensor_tensor(out=yT[:, dc, :n], in0=tm[i0][0][:, :n], in1=tm[i1][0][:, :n], op=ALU.add),
                                         reads=[tm[i0][1], tm[i1][1]], writes=[B_yT])
                                res, rb = ts["res"], ts["rb"]
                                for dc in range(8):
                                    pZ = nps()
                                    for kc in range(8):
                                        mm(lambda pZ=pZ: ps[pZ][:, :n], lambda kc=kc, dc=dc: wo[:, kc, dc * 128:(dc + 1) * 128],
                                           lambda kc=kc: yT[:, kc, :n], kc == 0, kc == 7, [B_wo, B_yT], psB[pZ])
                                    P.op("dve", lambda pZ=pZ, dc=dc, res=res, s=s: nc.vector.scalar_tensor_tensor(
                                        out=res[:, dc, off:off + n], in0=ps[pZ][:, :n], scalar=mod[:, l, 16 + dc, s:s + 1], in1=res[:, dc, off:off + n],
                                        op0=ALU.mult, op1=ALU.add), reads=[psB[pZ], B_modl[l], rb[(dc, g)]], writes=[rb[(dc, g)]])
                        P.barrier()

                stop_pt("mix%d" % l)
                with ExitStack() as ph:
                    nb = norm_bufs(ph)
                    NTOT = NT + (0 if last else CT)
                    h2, B_h2 = SB(ph, "h2", [128, 8, NTOT], BF16)
                    sets = [LAT] if last else [LAT, CTX]
                    tgl = []
                    for ts in sets:
                        for (g, off, n) in ts["tgs"]:
                            h2off = off if ts is LAT else NT
                            tgl.append((ts, g, off, n, h2off))
                    hB2 = {i: Buf("h2_%d" % i, True) for i in range(len(tgl))}
                    for ti, (ts, g, off, n, h2off) in enumerate(tgl):
                        s = ts["s"]

                        class _V:
                            def __init__(self, o):
                                self.o = o

                            def __getitem__(self, k):
                                p, c, sl = k
                                return h2[p, c, self.o + (sl.start or 0):self.o + sl.stop]
                        norm_tile(nb, ts, g, off, n, lambda c, s=s: g2m[:, l, c, s:s + 1], mods(ts, 24), None, hB2[ti], [B_g2l[l], B_modl[l]],
                                  hout=lambda c, h2off=h2off, n=n: h2[:, c, h2off:h2off + n], sqb=(_V(h2off), hB2[ti]))
                    if last:
                        wr, B_wr = SB(ph, "wr", [128, 8, 8], BF16)
                        comb, B_comb = SB(ph, "comb", [128, 16, 8], F32)
                        combT, B_combT = SB(ph, "combT", [8, NT], BF16)
                        esel, B_esel = SB(ph, "esel", [8, 8, 128], BF16)
                        lg, B_lg = SB(ph, "lg", [128, 8], F32)
                        m1, B_m1 = SB(ph, "m1", [128, 4], F32)
                        e1, B_e1 = SB(ph, "e1", [128, 8], F32)
                        l2, B_l2 = SB(ph, "l2", [128, 8], F32)
                        wload(lambda: wr[:], B_wr, lambda: wr_d.ap().rearrange("(kc p) n -> p kc n", p=128))
                        ld(lambda: esel[:], B_esel, lambda: esel_d.ap().rearrange("p (e n) -> p e n", e=8))
                        for blk in range(16):
                            pi = nps()
                            for kc in range(8):
                                mm(lambda pi=pi: ps[pi][:, 0:8], lambda kc=kc, blk=blk: h2[:, kc, blk * 128:(blk + 1) * 128],
                                   lambda kc=kc: wr[:, kc, :], kc == 0, kc == 7, [hB2[blk // 4], B_wr], psB[pi])
                            V = nc.vector
                            P.op("dve", lambda pi=pi: V.tensor_copy(out=lg[:], in_=ps[pi][:, 0:8]), reads=[psB[pi]], writes=[B_lg])
                            P.op("dve", lambda: V.reduce_max(out=m1[:, 0:1], in_=lg[:], axis=AX.X), reads=[B_lg], writes=[B_m1])
                            P.op("dve", lambda: V.tensor_scalar(out=e1[:], in0=lg[:], scalar1=m1[:, 0:1], scalar2=None, op0=ALU.is_equal), reads=[B_lg, B_m1], writes=[B_e1])
                            P.op("dve", lambda: V.scalar_tensor_tensor(out=l2[:], in0=e1[:], scalar=-1e30, in1=lg[:], op0=ALU.mult, op1=ALU.add), reads=[B_e1, B_lg], writes=[B_l2])
                            P.op("dve", lambda: V.reduce_max(out=m1[:, 1:2], in_=l2[:], axis=AX.X), reads=[B_l2, B_m1], writes=[B_m1])
                            P.op("dve", lambda: V.tensor_scalar(out=e1[:], in0=lg[:], scalar1=m1[:, 1:2], scalar2=None, op0=ALU.is_ge), reads=[B_lg, B_m1, B_l2], writes=[B_e1])
                            P.op("dve", lambda: V.tensor_scalar(out=l2[:], in0=lg[:], scalar1=m1[:, 0:1], scalar2=None, op0=ALU.subtract), reads=[B_lg, B_m1, B_e1], writes=[B_l2])
                            P.op("act", lambda: nc.scalar.activation(out=l2[:], in_=l2[:], func=AF.Exp), reads=[B_l2], writes=[B_l2])
                            P.op("dve", lambda: V.tensor_tensor(out=l2[:], in0=l2[:], in1=e1[:], op=ALU.mult), reads=[B_l2, B_e1], writes=[B_l2])
                            P.op("dve", lambda: V.reduce_sum(out=m1[:, 2:3], in_=l2[:], axis=AX.X), reads=[B_l2, B_m1], writes=[B_m1])
                            P.op("dve", lambda: V.reciprocal(out=m1[:, 3:4], in_=m1[:, 2:3]), reads=[B_m1], writes=[B_m1])
                            P.op("dve", lambda blk=blk: V.tensor_scalar(out=comb[:, blk, :], in0=l2[:], scalar1=m1[:, 3:4], scalar2=None, op0=ALU.mult),
                                 reads=[B_l2, B_m1], writes=[B_comb])
                            pj = nps()
                            P.op("pe", lambda pj=pj, blk=blk: nc.tensor.transpose(out=ps[pj][0:8, 0:128], in_=comb[:, blk, :], identity=ident[:]),
                                 reads=[B_comb, B_ident], writes=[psB[pj]])
                            P.op("dve", lambda pj=pj, blk=blk: V.tensor_copy(out=combT[:, blk * 128:(blk + 1) * 128], in_=ps[pj][0:8, 0:128]),
                                 reads=[psB[pj]], writes=[B_combT])
                        units = []
                        for e in range(8):
                            for pc in range(7):
                                units.append((e, 4, (lambda e=e, pc=pc: wge_d[e, :, pc * 512:(pc + 1) * 512]),
                                              (lambda e=e, pc=pc: wue_d[e, :, pc * 512:(pc + 1) * 512]),
                                              (lambda e=e, pc=pc: wde_d[e, pc * 512:(pc + 1) * 512, :])))
                    else:
                        units = []
                        for pc in range(6):
                            nf_ = 4 if pc < 5 else 2
                            units.append((None, nf_, (lambda pc=pc, nf_=nf_: wgd_d[:, pc * 512:pc * 512 + nf_ * 128]),
                                          (lambda pc=pc, nf_=nf_: wud_d[:, pc * 512:pc * 512 + nf_ * 128]),
                                          (lambda pc=pc, nf_=nf_: wdd_d[pc * 512:pc * 512 + nf_ * 128, :])))
                    wgu = [SB(ph, "wgu%d" % i, [128, 8, 2, 512], BF16) for i in range(2)]
                    wdn = [SB(ph, "wdn%d" % i, [128, 4, D], BF16) for i in range(2)]
                    aT = [SB(ph, "aT%d" % i, [128, 4, 512], BF16) for i in range(2)]

                    def uload(ui):
                        e, nf, gsrc, usrc_, dsrc = units[ui]
                        wt, wb = wgu[ui % 2]
                        dt_, db = wdn[ui % 2]
                        wload(lambda: wt[:, :, 0, 0:nf * 128], wb, lambda: gsrc().rearrange("(kc p) n -> p kc n", p=128))
                        wload(lambda: wt[:, :, 1, 0:nf * 128], wb, lambda: usrc_().rearrange("(kc p) n -> p kc n", p=128))
                        wload(lambda: dt_[:, 0:nf, :], db, lambda: dsrc().rearrange("(kc p) n -> p kc n", p=128))
                    uload(0)
                    sgl = [SB(ph, "fsg%d" % i, [128, 512], F32) for i in range(2)]
                    ftm = [SB(ph, "ftm%d" % i, [128, 512], F32) for i in range(2)]
                    cbc = [SB(ph, "cbc%d" % i, [128, 512], F32) for i in range(2)]
                    aq = [0]
                    fq = [0]
                    mstep = [0]
                    if not last:
                        wm1 = SB(ph, "wm1", [128, 8, 512], BF16)
                        pslist[0] = [0, 1, 2, 3, 4, 5, 6]
                        mpi1 = 7
                    for ui, (e, nf, gsrc, usrc_, dsrc) in enumerate(units):
                        wt, wb = wgu[ui % 2]
                        dt_, db = wdn[ui % 2]
                        if ui + 1 < len(units):
                            uload(ui + 1)
                        pend = None

                        def down(ti, at, ab):
                            ts, g, off, n, h2off = tgl[ti]
                            res, rb, s = ts["res"], ts["rb"], ts["s"]
                            for dc in range(8):
                                pY = nps()
                                for f in range(nf):
                                    mm(lambda pY=pY: ps[pY][:, :n], lambda f=f, dc=dc: dt_[:, f, dc * 128:(dc + 1) * 128],
                                       lambda f=f: at[:, f, :n], f == 0, f == nf - 1, [db, ab], psB[pY])
                                P.op("dve", lambda pY=pY, dc=dc: nc.vector.scalar_tensor_tensor(
                                    out=res[:, dc, off:off + n], in0=ps[pY][:, :n], scalar=mod[:, l, 40 + dc, s:s + 1], in1=res[:, dc, off:off + n],
                                    op0=ALU.mult, op1=ALU.add), reads=[psB[pY], B_modl[l], rb[(dc, g)]], writes=[rb[(dc, g)]])

                        for ti, (ts, g, off, n, h2off) in enumerate(tgl):
                            at, ab = aT[aq[0] % 2]
                            aq[0] += 1
                            if e is not None:
                                pc_ = nps()
                                ct_, cb_ = cbc[ti % 2]
                                mm(lambda pc_=pc_: ps[pc_][:, :n], lambda e=e: esel[:, e, :], lambda off=off, n=n: combT[:, off:off + n], True, True, [B_esel, B_combT], psB[pc_])
                                P.op("dve", lambda pc_=pc_, ct_=ct_: nc.vector.tensor_copy(out=ct_[:, :n], in_=ps[pc_][:, :n]), reads=[psB[pc_]], writes=[cb_])
                            for f in range(nf):
                                pG, pU = nps(), nps()
                                for kc in range(8):
                                    mm(lambda pG=pG: ps[pG][:, :n], lambda kc=kc, f=f: wt[:, kc, 0, f * 128:(f + 1) * 128],
                                       lambda kc=kc: h2[:, kc, h2off:h2off + n], kc == 0, kc == 7, [wb, hB2[ti]], psB[pG])
                                for kc in range(8):
                                    mm(lambda pU=pU: ps[pU][:, :n], lambda kc=kc, f=f: wt[:, kc, 1, f * 128:(f + 1) * 128],
                                       lambda kc=kc: h2[:, kc, h2off:h2off + n], kc == 0, kc == 7, [wb, hB2[ti]], psB[pU])
                                st2, sb2 = sgl[fq[0] % 2]
                                ft2, fb2 = ftm[fq[0] % 2]
                                fq[0] += 1
                                P.op("act", lambda pG=pG, st2=st2: nc.scalar.activation(out=st2[:, :n], in_=ps[pG][:, :n], func=AF.Silu),
                                     reads=[psB[pG]], writes=[sb2])
                                if e is None:
                                    P.op("dve", lambda pU=pU, st2=st2, at=at, f=f: nc.vector.tensor_tensor(out=at[:, f, :n], in0=ps[pU][:, :n], in1=st2[:, :n], op=ALU.mult),
                                         reads=[psB[pU], sb2], writes=[ab])
                                else:
                                    P.op("dve", lambda pU=pU, st2=st2, ft2=ft2: nc.vector.tensor_tensor(out=ft2[:, :n], in0=ps[pU][:, :n], in1=st2[:, :n], op=ALU.mult),
                                         reads=[psB[pU], sb2], writes=[fb2])
                                    P.op("pool", lambda ft2=ft2, ct_=ct_, at=at, f=f: nc.gpsimd.tensor_tensor(out=at[:, f, :n], in0=ft2[:, :n], in1=ct_[:, :n], op=ALU.mult),
                                         reads=[fb2, cb_], writes=[ab])
                            if pend is not None:
                                down(*pend)
                            pend = (ti, at, ab)
                            if not last and mstep[0] < 12:
                                mods_piece(1, mstep[0], wm1, mpi1)
                                mstep[0] += 1
                        down(*pend)
                    if not last:
                        while mstep[0] < 12:
                            mods_piece(1, mstep[0], wm1, mpi1)
                            mstep[0] += 1
                        mods_fin(1, mpi1)
                        pslist[0] = list(range(8))
                    P.barrier()
                stop_pt("ffn%d" % l)

            P.skip = False
            P.final = True
            with ExitStack() as ph:
                nb = norm_bufs(ph)
                oT, B_oT = SB(ph, "oT", [128, 8, 512], F32)
                sqo = SB(ph, "sqo", [128, 8, 512], BF16)
                ost = [SB(ph, "ost%d" % i, [128, D], F32) for i in range(2)]
                B_out = Buf("out")
                for (g, off, n) in LAT["tgs"]:
                    norm_tile(nb, LAT, g, off, n, lambda c: fing[:, c:c + 1], None, oT, B_oT, [B_fing], sqb=sqo)
                    for b in range(4):
                        gb = g * 4 + b
                        o_t, o_b = ost[gb % 2]
                        for half in range(2):
                            pi = nps()
                            for c in range(4):
                                cc = half * 4 + c
                                P.op("pe", lambda pi=pi, c=c, cc=cc, b=b: nc.tensor.transpose(
                                    out=ps[pi][:, c * 128:(c + 1) * 128], in_=oT[:, cc, b * 128:(b + 1) * 128], identity=ident[:]),
                                    reads=[B_oT, B_ident], writes=[psB[pi]])
                            P.op("dve", lambda pi=pi, o_t=o_t, half=half: nc.vector.tensor_copy(out=o_t[:, half * 512:(half + 1) * 512], in_=ps[pi][:]),
                                 reads=[psB[pi]], writes=[o_b])
                        P.op("sp", lambda o_t=o_t, gb=gb: nc.sync.dma_start(out=out_d[gb * 128:(gb + 1) * 128, :], in_=o_t[:]),
                             reads=[o_b], writes=[B_out], dma=True)
                if not P.dry:
                    for q in ("sp",):
                        m = P.dma_no[q]
                        for i in range(NDMASEM):
                            cnt = (m - i + NDMASEM - 1) // NDMASEM if m > i else 0
                            if cnt > 0:
                                P._wait("sp", (q, i), 16 * cnt)
    return nc


_CACHE = {}


def _consts():
    if "c" in _CACHE:
        return _CACHE["c"]
    c = {}
    c["ident"] = np.eye(128, dtype=np.float32)
    _a = 2 * np.pi * (np.outer(np.arange(128), np.arange(128)) % 128) / 128
    c["c128"] = np.concatenate([np.cos(_a), -np.sin(_a)], axis=1).astype(NPBF)
    c["ones1024"] = np.full((128, 128), 1.0 / 1024, dtype=NPBF)
    d = np.arange(64)
    partner = np.where((d % 32) < 16, d + 16, d - 16)
    c["partner"] = partner
    sign = np.where((d % 32) < 16, -1.0, 1.0)
    inv = 10000.0 ** (-np.arange(16, dtype=np.float64) / 16)
    c["rope"] = (sign, inv)
    kq = np.arange(128)
    mprev = (kq[:, None] >= kq[None, :]).astype(np.float32)
    mnext = (kq[:, None] <= kq[None, :]).astype(np.float32)
    c["mprev"] = np.tile(mprev, (1, 4))
    c["mnext"] = np.tile(mnext, (1, 4))
    ch = np.arange(128)
    ang = 2 * np.pi * ((np.outer(ch, ch)) % 128) / 128
    c["ccsc"] = np.concatenate([np.cos(ang), np.sin(ang)], axis=1) / np.sqrt(128.0)
    n = np.arange(CT)
    ang = 2 * np.pi * ((np.outer(n, n)) % CT) / CT
    c["tabcc"] = (np.cos(ang) / np.sqrt(CT)).astype(NPBF)
    c["tabsc"] = (-np.sin(ang) / np.sqrt(CT)).astype(NPBF)
    es = np.zeros((8, 8, 128), np.float32)
    for e in range(8):
        es[e, e, :] = 1.0
    c["esel"] = es.reshape(8, 1024).astype(NPBF)
    for j in range(4):
        n2 = np.arange(64, dtype=np.int64)
        k1 = np.arange(128, dtype=np.int64)
        k2 = np.arange(16, dtype=np.int64) + 16 * j
        kk = k1[:, None] + 128 * k2[None, :]
        ph_ = 2 * np.pi * ((n2[:, None, None] * kk[None, :, :]) % T).astype(np.float64) / T
        cph, sph = np.cos(ph_) / np.sqrt(T), np.sin(ph_) / np.sqrt(T)
        tb = np.zeros((128, 128, 32), np.float64)
        tb[:64, :, :16] = cph
        tb[64:, :, :16] = sph
        tb[:64, :, 16:] = -sph
        tb[64:, :, 16:] = cph
        c["tab23_%d" % j] = tb.reshape(128, 128 * 32).astype(NPBF)
        t = np.arange(NT) + NT * j
        rows = (t // 64).astype(np.float64)
        cols = (t % 64).astype(np.float64)
        dd = np.arange(128) % 64
        fi = dd % 16
        pos = np.where((dd < 32)[:, None], rows[None, :], cols[None, :])
        a = pos * inv[fi][:, None]
        c["cos%d" % j] = np.cos(a).astype(NPBF)
        c["sin%d" % j] = (np.sin(a) * sign[dd][:, None]).astype(NPBF)
    _CACHE["c"] = c
    return c


def _fm(v, nch):
    return np.ascontiguousarray(np.asarray(v, np.float32).reshape(nch, 128).T)


def kernel(x, c, ctx, c_ctx, w_mod, b_mod, norm1_g, norm2_g, w_in, sink, w_fourier, w_attn, w_out,
           w_gate_d, w_up_d, w_down_d, w_router, w_gate_e, w_up_e, w_down_e, final_g):
    K = _consts()
    f = lambda a: np.ascontiguousarray(np.asarray(a, dtype=np.float32))
    x, c, ctx, c_ctx = f(x), f(c), f(ctx), f(c_ctx)
    w_in = f(w_in)
    partner = K["partner"]
    qcols = np.arange(512)
    qpcols = (qcols // 64) * 64 + partner[qcols % 64]
    kcols = []
    for h in range(2):
        kcols += list(512 + 512 + h * 64 + np.arange(64)) * 2
    kpcols = []
    for h in range(2):
        kpcols += list(512 + 512 + h * 64 + partner) * 2
    cols = np.concatenate([np.arange(512), np.array(kcols), np.array(kpcols), 1152 + np.arange(128),
                           512 + qcols, 512 + qpcols, 1280 + np.arange(2048)]).astype(np.int64)
    assert cols.shape[0] == NCEXT
    w_in_ext = np.ascontiguousarray(w_in[:, :, cols])
    bmod = np.stack([np.repeat(_fm(b_mod[l], 48)[:, :, None], 2, axis=2).reshape(128, 96) for l in range(2)], axis=1).reshape(128, 192)
    n1g = np.stack([np.repeat(_fm(norm1_g[l], 8)[:, :, None], 2, axis=2).reshape(128, 16) for l in range(2)], axis=1).reshape(128, 32)
    n2g = np.stack([np.repeat(_fm(norm2_g[l], 8)[:, :, None], 2, axis=2).reshape(128, 16) for l in range(2)], axis=1).reshape(128, 32)
    fing = _fm(final_g, 8)
    sinkb = np.ascontiguousarray(np.broadcast_to(f(sink).reshape(1, 16), (128, 16)))
    shared = dict(w_mod=f(w_mod), bmod=np.ascontiguousarray(bmod), n1g=np.ascontiguousarray(n1g), n2g=np.ascontiguousarray(n2g),
                  fing=fing, w_in_ext=w_in_ext, sinkb=sinkb, w_fourier=f(w_fourier), w_attn=f(w_attn), w_out=f(w_out),
                  w_gate_d=f(w_gate_d)[0], w_up_d=f(w_up_d)[0], w_down_d=f(w_down_d)[0], w_router=f(w_router)[0],
                  w_gate_e=f(w_gate_e)[0], w_up_e=f(w_up_e)[0], w_down_e=f(w_down_e)[0],
                  ident=K["ident"], ones1024=K["ones1024"], tabcc=K["tabcc"], tabsc=K["tabsc"],
                  ccsc=K["ccsc"].astype(NPBF), esel=K["esel"])
    in_maps = []
    zeros = np.zeros((128, 512), np.float32)
    for i in range(8):
        b, j = i // 4, i % 4
        m = dict(shared)
        m["x"] = np.ascontiguousarray(x[b, j * NT:(j + 1) * NT])
        m["ctx"] = np.ascontiguousarray(ctx[b])
        cv = np.stack([_fm(c[b], 8), _fm(c_ctx, 8)], axis=2).reshape(128, 16)
        m["cvec"] = np.ascontiguousarray(cv)
        m["ropecos"] = K["cos%d" % j]
        m["ropesin"] = K["sin%d" % j]
        m["masks"] = np.concatenate([K["mprev"], K["mnext"], K["mprev"] if j > 0 else zeros, K["mnext"] if j < 3 else zeros], axis=1).astype(NPBF)
        sl = np.zeros((128, 8), np.float32)
        if j > 0:
            sl[:, j - 1] = 1.0
        if j < 3:
            sl[:, 4 + j + 1] = 1.0
        m["sel"] = sl
        m["tab23"] = K["tab23_%d" % j]
        m["c128"] = K["c128"]
        in_maps.append(m)

    if "nc" not in _CACHE:
        nc0 = bass.Bass("TRN2", target_bir_lowering=False)
        P0 = Prog(nc0)
        build(nc0, P0)
        nc = bass.Bass("TRN2", target_bir_lowering=False)
        P1 = Prog(nc, sigset=P0.need)
        build(nc, P1)
        _CACHE["nc"] = nc
    nc = _CACHE["nc"]
    in_maps = [{k: v for k, v in m.items() if k in nc._mk_declared} for m in in_maps]
    res = run_bass_kernel_spmd(nc, in_maps, core_ids=list(range(8)))
    out = np.zeros((2, T, D), np.float32)
    for i in range(8):
        b, j = i // 4, i % 4
        out[b, j * NT:(j + 1) * NT] = np.asarray(res.results[i]["out"], dtype=np.float32)
    return out
```

```python
import os
from contextlib import ExitStack
import numpy as np
import ml_dtypes
import concourse.bass as bass
import concourse.mybir as mybir
from concourse.bass_utils import run_bass_kernel_spmd

F32 = mybir.dt.float32
BF16 = mybir.dt.bfloat16
AF = mybir.ActivationFunctionType
ALU = mybir.AluOpType
AX = mybir.AxisListType
NPBF = ml_dtypes.bfloat16

D = 1024
T = 8192
NT = 2048
CT = 256
NCEXT = 4224
O_U, O_K, O_V, O_Q, O_G = 0, 512, 1024, 1152, 2176
DFF = 2816
DFE = 3584
NDMASEM = 8
STOP = os.environ.get("MK_STOP", "")


class Buf:
    __slots__ = ("name", "w", "r", "phase")

    def __init__(self, name, phase=False):
        self.name = name
        self.w = None
        self.r = {}
        self.phase = phase


class Prog:
    COMPUTE = ("pe", "act", "dve", "pool")

    def __init__(self, nc, sigset=None):
        self.nc = nc
        self.dry = sigset is None
        self.sigset = sigset if sigset is not None else set()
        self.need = set()
        self.n = 0
        self.meta = []
        self.cnt = {e: 0 for e in self.COMPUTE}
        self.sigval = {}
        self.waited = {}
        self.dma_no = {"sp": 0, "pool": 0, "act": 0}
        self.PH = Buf("PHASE")
        self.eng = {"pe": nc.tensor, "act": nc.scalar, "dve": nc.vector, "pool": nc.gpsimd, "sp": nc.sync}
        self.sems = {}
        self.ncc = 0

    def alloc_sems(self, stack):
        for e in self.COMPUTE:
            self.sems[e] = stack.enter_context(self.nc.semaphore("s_" + e))
        for q in ("sp", "pool"):
            for i in range(NDMASEM):
                self.sems[(q, i)] = stack.enter_context(self.nc.semaphore("d_%s%d" % (q, i)))
        for i in range(8):
            self.sems[("cc", i)] = stack.enter_context(self.nc.semaphore("cc%d" % i))

    def _wait(self, eng, semkey, val):
        k = (eng, semkey)
        if self.waited.get(k, 0) >= val:
            return
        self.waited[k] = val
        self.eng[eng].wait_ge(self.sems[semkey], val)

    skip = False
    limit = int(os.environ.get("MK_LIMIT", "0")) or None
    final = False

    def op(self, eng, fn, reads=(), writes=(), dma=False, cc=False):
        if self.skip:
            return None
        if self.limit is not None and self.n >= self.limit and not self.final:
            return None
        if os.environ.get("MK_TRACE") and self.dry:
            import inspect
            fr = inspect.currentframe().f_back
            print("OP", self.n, eng, fr.f_lineno, "dma" if dma else ("cc" if cc else ""))
        idx = self.n
        self.n += 1
        reads = list(reads)
        writes = list(writes)
        if any(b.phase for b in reads) or any(b.phase for b in writes):
            if self.PH not in writes:
                reads.append(self.PH)
        deps = set()
        for b in reads:
            if b.w is not None:
                deps.add(b.w)
        for b in writes:
            if b.w is not None:
                deps.add(b.w)
            deps.update(b.r.values())
        deps.discard(idx)
        async_op = dma or cc
        rkey = ("a", idx) if async_op else eng
        for b in writes:
            b.w = idx
            b.r = {}
        for b in reads:
            if b not in writes:
                b.r[rkey] = idx
        self.meta.append((eng, async_op))
        real = []
        for dpt in deps:
            deng, dasync = self.meta[dpt]
            if (not dasync) and deng == eng and (not async_op) and eng == "pe":
                continue
            real.append(dpt)
        if self.dry:
            for dpt in real:
                self.need.add(dpt)
            return idx
        for dpt in sorted(real):
            semkey, val = self.sigval[dpt]
            self._wait(eng, semkey, val)
        if dma:
            m = self.dma_no[eng]
            self.dma_no[eng] = m + 1
            semkey = (eng, m % NDMASEM)
            if m >= NDMASEM:
                self._wait(eng, semkey, 16 * (m // NDMASEM))
            ins = fn()
            ins.then_inc(self.sems[semkey], 16)
            self.sigval[idx] = (semkey, 16 * (m // NDMASEM + 1))
        elif cc:
            semkey = ("cc", self.ncc)
            self.ncc += 1
            ins = fn()
            ins.then_inc(self.sems[semkey])
            self.sigval[idx] = (semkey, 1)
        else:
            ins = fn()
            if idx in self.sigset:
                self.cnt[eng] += 1
                ins.then_inc(self.sems[eng], 1)
                self.sigval[idx] = (eng, self.cnt[eng])
        return idx

    def barrier(self):
        self.op("dve", lambda: self.nc.vector.engine_nop(), writes=[self.PH])


def build(nc, P):
    declared = set()
    nc._mk_declared = declared

    def din(name, shape, dt=F32):
        declared.add(name)
        return nc.dram_tensor(name, list(shape), dt, kind="ExternalInput")

    x_d = din("x", [NT, D])
    ctx_d = din("ctx", [CT, D])
    cvec_d = din("cvec", [128, 16])
    wmod_d = din("w_mod", [2, D, 6 * D])
    bmod_d = din("bmod", [128, 2 * 96])
    n1g_d = din("n1g", [128, 32])
    n2g_d = din("n2g", [128, 32])
    fing_d = din("fing", [128, 8])
    win_d = din("w_in_ext", [2, D, NCEXT])
    sink_d = din("sinkb", [128, 16])
    wf_d = din("w_fourier", [2, 512, D])
    wa_d = din("w_attn", [2, 512, D])
    wo_d = din("w_out", [2, D, D])
    need_d = STOP in ("", "ffn0", "p1_1", "pa_1", "pb0_1", "mix1", "ffn1")
    need_e = STOP in ("", "ffn1")
    wgd_d = wud_d = wdd_d = wr_d = wge_d = wue_d = wde_d = None
    if need_d:
        wgd_d = din("w_gate_d", [D, DFF])
        wud_d = din("w_up_d", [D, DFF])
        wdd_d = din("w_down_d", [DFF, D])
    if need_e:
        wr_d = din("w_router", [D, 8])
        wge_d = din("w_gate_e", [8, D, DFE])
        wue_d = din("w_up_e", [8, D, DFE])
        wde_d = din("w_down_e", [8, DFE, D])
    ident_d = din("ident", [128, 128])
    ones_d = din("ones1024", [128, 128], BF16)
    cos_d = din("ropecos", [128, NT], BF16)
    sin_d = din("ropesin", [128, NT], BF16)
    masks_d = din("masks", [128, 4 * 512], BF16)
    sel_d = din("sel", [128, 8])
    c128_d = din("c128", [128, 256], BF16)
    tab23_d = din("tab23", [128, 128 * 32], BF16)
    s1_d = nc.dram_tensor("s1_scratch", [2, 128, 64, 512], BF16)
    B_s1 = Buf("s1")
    tabcc_d = din("tabcc", [CT, CT], BF16)
    tabsc_d = din("tabsc", [CT, CT], BF16)
    ccsc_d = din("ccsc", [128, 256], BF16)
    esel_d = din("esel", [8, 8 * 128], BF16)
    out_d = nc.dram_tensor("out", [NT, D], F32, kind="ExternalOutput")

    u_own = [nc.dram_tensor("u_own%d" % k, [1024, 512], BF16) for k in range(2)]
    u_all = [nc.dram_tensor("u_all%d" % k, [4096, 512], BF16) for k in range(2)]
    u_ctx = nc.dram_tensor("u_ctx", [CT, 512], BF16)
    halo_in = nc.dram_tensor("halo_in", [128, 1024], BF16)
    halo_all = nc.dram_tensor("halo_all", [512, 1024], BF16)
    B_uown, B_uall, B_uctx, B_hin, B_hall = Buf("uown"), Buf("uall"), Buf("uctx"), Buf("hin"), Buf("hall")

    with ExitStack() as st:
        block = st.enter_context(nc.Block())
        P.alloc_sems(st)

        sbn = [0]

        def SB(stack, name, shape, dt, phase=True):
            sbn[0] += 1
            return stack.enter_context(nc.sbuf_tensor("sb%d_%s" % (sbn[0], name), shape, dt)), Buf(name, phase)

        ps = [st.enter_context(nc.psum_tensor("ps%d" % i, [128, 512], F32)) for i in range(8)]
        psB = [Buf("ps%d" % i) for i in range(8)]
        psrr = [0]

        pslist = [list(range(8))]

        def nps():
            i = pslist[0][psrr[0] % len(pslist[0])]
            psrr[0] += 1
            return i

        XT, _ = SB(st, "XT", [128, 8, NT], F32, False)
        XB = {(c, g): Buf("XT%d_%d" % (c, g)) for c in range(8) for g in range(4)}
        XcT, _ = SB(st, "XcT", [128, 8, CT], F32, False)
        XcB = {(c, 0): Buf("XcT%d" % c) for c in range(8)}
        mod, _ = SB(st, "mod", [128, 2, 48, 2], F32, False)
        g1m, _ = SB(st, "g1m", [128, 2, 8, 2], F32, False)
        g2m, _ = SB(st, "g2m", [128, 2, 8, 2], F32, False)
        B_modl = [Buf("mod0"), Buf("mod1")]
        B_g1l = [Buf("g1m0"), Buf("g1m1")]
        B_g2l = [Buf("g2m0"), Buf("g2m1")]
        ident, B_ident = SB(st, "ident", [128, 128], F32, False)
        ones, B_ones = SB(st, "ones", [128, 128], BF16, False)
        epsb, B_eps = SB(st, "epsb", [128, 1], F32, False)
        esink, B_esink = SB(st, "esink", [128, 16], F32, False)
        fing, B_fing = SB(st, "fing", [128, 8], F32, False)
        masks, B_masks = SB(st, "masks", [128, 4, 512], BF16, False)
        sel, B_sel = SB(st, "sel", [128, 8], F32, False)
        ccsc, B_ccsc = SB(st, "ccsc", [128, 256], BF16, False)
        c128, B_c128 = SB(st, "c128", [128, 256], BF16, False)

        cv, B_cv = SB(st, "cv", [128, 16], F32, False)
        scb, B_scb = SB(st, "scb", [128, 8, 2], BF16, False)
        bm, B_bm = SB(st, "bm", [128, 2, 96], F32, False)
        ng1, B_ng1 = SB(st, "ng1", [128, 32], F32, False)
        ng2, B_ng2 = SB(st, "ng2", [128, 32], F32, False)
        tmpm, B_tmpm = SB(st, "tmpm", [128, 16], F32, False)
        LAT = dict(name="lat", res=XT, rb=XB, n=NT, s=0, tgs=[(g, g * 512, 512) for g in range(4)])
        CTX = dict(name="ctx", res=XcT, rb=XcB, n=CT, s=1, tgs=[(0, 0, CT)])

        def mm(o, lhsT, rhs, start, stop, reads, pw):
            P.op("pe", lambda: nc.tensor.matmul(o(), lhsT=lhsT(), rhs=rhs(), start=start, stop=stop),
                 reads=reads, writes=[pw])

        def stop_pt(tag):
            if STOP == tag:
                P.skip = True

        @block.sync
        def _(sync):
            def ld(dst, B, src):
                P.op("sp", lambda: nc.sync.dma_start(out=dst(), in_=src()), writes=[B], dma=True)
            ld(lambda: ident[:], B_ident, lambda: ident_d[:, :])
            ld(lambda: ones[:], B_ones, lambda: ones_d[:, :])
            ld(lambda: esink[:], B_esink, lambda: sink_d[:, :])
            ld(lambda: fing[:], B_fing, lambda: fing_d[:, :])
            ld(lambda: masks[:], B_masks, lambda: masks_d.ap().rearrange("p (m n) -> p m n", m=4))
            ld(lambda: sel[:], B_sel, lambda: sel_d[:, :])
            ld(lambda: ccsc[:], B_ccsc, lambda: ccsc_d[:, :])
            ld(lambda: c128[:], B_c128, lambda: c128_d[:, :])
            P.op("dve", lambda: nc.vector.memset(epsb[:], 1e-6), writes=[B_eps])
            P.op("act", lambda: nc.scalar.activation(out=esink[:], in_=esink[:], func=AF.Exp),
                 reads=[B_esink], writes=[B_esink])

            def mods_piece(l, piece, wmb, pi):
                wt, wb = wmb
                P.op("pool", lambda: nc.gpsimd.dma_start(
                    out=wt[:], in_=wmod_d[l, :, piece * 512:(piece + 1) * 512].rearrange("(kc p) n -> p kc n", p=128)),
                    writes=[wb], dma=True)
                for sub in range(4):
                    ch = piece * 4 + sub
                    for kc in range(8):
                        mm(lambda ch=ch: ps[pi][:, ch * 2:ch * 2 + 2],
                           lambda kc=kc, sub=sub: wt[:, kc, sub * 128:(sub + 1) * 128],
                           lambda kc=kc: scb[:, kc, :], kc == 0, kc == 7, [wb, B_scb], psB[pi])

            def mods_fin(l, pi):
                P.op("dve", lambda: nc.vector.tensor_tensor(
                    out=mod[:, l].rearrange("p c s -> p (c s)"), in0=ps[pi][:, 0:96], in1=bm[:, l, :], op=ALU.add),
                    reads=[psB[pi], B_bm], writes=[B_modl[l]])
                for (gm, Bg, ng, Bn, o) in ((g1m, B_g1l, ng1, B_ng1, 8), (g2m, B_g2l, ng2, B_ng2, 32)):
                    P.op("dve", lambda o=o: nc.vector.tensor_scalar(
                        out=tmpm[:], in0=mod[:, l, o:o + 8, :].rearrange("p c s -> p (c s)"), scalar1=1.0, scalar2=None, op0=ALU.add),
                        reads=[B_modl[l]], writes=[B_tmpm])
                    P.op("dve", lambda gm=gm, ng=ng: nc.vector.tensor_tensor(
                        out=gm[:, l].rearrange("p c s -> p (c s)"), in0=tmpm[:], in1=ng[:, l * 16:(l + 1) * 16], op=ALU.mult),
                        reads=[B_tmpm, Bn], writes=[Bg[l]])

            with ExitStack() as ph:
                xs = [SB(ph, "xs%d" % i, [128, D], F32) for i in range(2)]

                def load_T(src, nblk, res, rb):
                    for blk in range(nblk):
                        xt, xb = xs[blk % 2]
                        P.op("sp", lambda xt=xt, blk=blk: nc.sync.dma_start(out=xt[:], in_=src[blk * 128:(blk + 1) * 128, :]),
                             writes=[xb], dma=True)
                        for half in range(2):
                            pi = nps()
                            for c in range(4):
                                cc = half * 4 + c
                                P.op("pe", lambda xt=xt, pi=pi, c=c, cc=cc: nc.tensor.transpose(
                                    out=ps[pi][:, c * 128:(c + 1) * 128], in_=xt[:, cc * 128:(cc + 1) * 128], identity=ident[:]),
                                    reads=[xb, B_ident], writes=[psB[pi]])
                            g = (blk * 128) // 512
                            P.op("dve", lambda pi=pi, half=half, blk=blk: nc.vector.tensor_copy(
                                out=res[:, half * 4:(half + 1) * 4, blk * 128:(blk + 1) * 128],
                                in_=ps[pi][:].rearrange("p (c t) -> p c t", c=4)),
                                reads=[psB[pi]], writes=[rb[(half * 4 + c, g)] for c in range(4)])
                load_T(x_d, 16, XT, XB)
                load_T(ctx_d, 2, XcT, XcB)
                stop_pt("load")

                wm = [SB(ph, "wm%d" % i, [128, 8, 512], BF16) for i in range(2)]
                ld(lambda: cv[:], B_cv, lambda: cvec_d[:, :])
                ld(lambda: bm[:], B_bm, lambda: bmod_d.ap().rearrange("p (l n) -> p l n", l=2))
                ld(lambda: ng1[:], B_ng1, lambda: n1g_d[:, :])
                ld(lambda: ng2[:], B_ng2, lambda: n2g_d[:, :])
                P.op("act", lambda: nc.scalar.activation(out=cv[:], in_=cv[:], func=AF.Silu), reads=[B_cv], writes=[B_cv])
                P.op("dve", lambda: nc.vector.tensor_copy(out=scb[:], in_=cv[:].rearrange("p (k s) -> p k s", s=2)),
                     reads=[B_cv], writes=[B_scb])
                mpi0 = nps()
                for piece in range(12):
                    mods_piece(0, piece, wm[piece % 2], mpi0)
                mods_fin(0, mpi0)
                P.barrier()
                stop_pt("mods")

            def norm_tile(ph_bufs, ts, g, off, n, gm_ap, sh_ap, hT, hB, extra_reads, hout=None, sqb=None):
                _sq, _B_sq, rstd, B_rstd, tt = ph_bufs
                if hout is None:
                    hout = lambda c: hT[:, c, :n]
                sq, B_sq = sqb if sqb is not None else (hT, hB)
                res, rb = ts["res"], ts["rb"]
                for c in range(8):
                    P.op("act", lambda c=c: nc.scalar.activation(out=sq[:, c, :n], in_=res[:, c, off:off + n], func=AF.Square),
                         reads=[rb[(c, g)]], writes=[B_sq])
                pi = nps()
                for c in range(8):
                    mm(lambda: ps[pi][:, :n], lambda: ones[:], lambda c=c: sq[:, c, :n], c == 0, c == 7, [B_ones, B_sq], psB[pi])
                P.op("act", lambda: nc.scalar.activation(out=rstd[:, :n], in_=ps[pi][:, :n], func=AF.Sqrt, bias=epsb[:, 0:1], scale=1.0),
                     reads=[psB[pi], B_eps], writes=[B_rstd])
                P.op("dve", lambda: nc.vector.reciprocal(out=rstd[:, :n], in_=rstd[:, :n]), reads=[B_rstd], writes=[B_rstd])
                for c in range(8):
                    t, tb = tt[c % 2]
                    if sh_ap is None:
                        P.op("dve", lambda c=c: nc.vector.scalar_tensor_tensor(
                            out=hout(c), in0=res[:, c, off:off + n], scalar=gm_ap(c), in1=rstd[:, :n], op0=ALU.mult, op1=ALU.mult),
                            reads=[rb[(c, g)], B_rstd] + extra_reads, writes=[hB])
                    else:
                        P.op("dve", lambda c=c, t=t: nc.vector.scalar_tensor_tensor(
                            out=t[:, :n], in0=res[:, c, off:off + n], scalar=gm_ap(c), in1=rstd[:, :n], op0=ALU.mult, op1=ALU.mult),
                            reads=[rb[(c, g)], B_rstd] + extra_reads, writes=[tb])
                        P.op("act", lambda c=c, t=t: nc.scalar.activation(
                            out=hout(c), in_=t[:, :n], func=AF.Identity, bias=sh_ap(c), scale=1.0),
                            reads=[tb] + extra_reads, writes=[hB])

            def norm_bufs(ph):
                sq, B_sq = None, None
                rstd, B_rstd = SB(ph, "rstd", [128, 512], F32)
                tt = [SB(ph, "nt%d" % i, [128, 512], F32) for i in range(2)]
                return (sq, B_sq, rstd, B_rstd, tt)

            def wload(wt, wb, src):
                P.op("pool", lambda: nc.gpsimd.dma_start(out=wt(), in_=src()), writes=[wb], dma=True)

            for l in range(2):
                last = (l == 1)
                with ExitStack() as lay:
                    OT, B_OT = SB(lay, "OT", [128, 4, NT], BF16)
                    OcT, B_OcT = SB(lay, "OcT", [128, 4, CT], BF16)
                    FT, B_FT = SB(lay, "FT", [128, 4, NT], BF16)
                    FcT, B_FcT = SB(lay, "FcT", [128, 4, CT], BF16)
                    att = ExitStack()
                    KT, B_KT = SB(att, "KT", [128, 2, NT], BF16)
                    Vown, B_V = SB(att, "Vown", [128, 16, 2, 80], BF16)
                    KcT, B_KcT = SB(att, "KcT", [128, 2, CT], BF16)
                    Vc, B_Vc = SB(att, "Vc", [128, 2, 2, 80], BF16)
                    hp, B_hp = SB(att, "hp", [128, 1024], BF16)
                    hn, B_hn = SB(att, "hn", [128, 1024], BF16)
                    P.op("pool", lambda: nc.gpsimd.memset(Vown[:], 1.0), writes=[B_V])
                    P.op("pool", lambda: nc.gpsimd.memset(Vc[:], 1.0), writes=[B_Vc])

                    def mods(ts, base):
                        s = ts["s"]
                        return lambda c: mod[:, l, base + c, s:s + 1]

                    with ExitStack() as ph:
                        nb = norm_bufs(ph)
                        hTs = [SB(ph, "hT%d" % i, [128, 8, 512], BF16) for i in range(2)]
                        hq = [0]
                        wu, B_wu = SB(ph, "wu", [128, 8, 512], BF16)
                        wk, B_wk = SB(ph, "wk", [128, 8, 512], BF16)
                        wv, B_wv = SB(ph, "wv", [128, 8, 128], BF16)
                        rc, B_rc = SB(ph, "rc", [128, NT], BF16)
                        rs, B_rs = SB(ph, "rs", [128, NT], BF16)
                        ust = [SB(ph, "ust%d" % i, [128, 512], BF16) for i in range(2)]
                        r1 = [SB(ph, "r1_%d" % i, [128, 512], F32) for i in range(2)]
                        r2 = [SB(ph, "r2_%d" % i, [128, 512], F32) for i in range(2)]
                        wsrc = lambda o, n: (lambda: win_d[l, :, o:o + n].rearrange("(kc p) n -> p kc n", p=128))
                        wload(lambda: wu[:], B_wu, wsrc(O_U, 512))
                        wload(lambda: wk[:], B_wk, wsrc(O_K, 512))
                        wload(lambda: wv[:], B_wv, wsrc(O_V, 128))
                        ld(lambda: rc[:], B_rc, lambda: cos_d[:, :])
                        ld(lambda: rs[:], B_rs, lambda: sin_d[:, :])
                        uq = [0]
                        for ts in ([CTX, LAT]):
                            isl = ts is LAT
                            for (g, off, n) in ts["tgs"]:
                                hT, B_hT = hTs[hq[0] % 2]
                                hq[0] += 1
                                norm_tile(nb, ts, g, off, n, lambda c, ts=ts: g1m[:, l, c, ts["s"]:ts["s"] + 1], mods(ts, 0), hT, B_hT, [B_g1l[l], B_modl[l]])
                                for b in range(n // 128):
                                    gb = off // 128 + b
                                    pi = nps()
                                    for kc in range(8):
                                        mm(lambda pi=pi: ps[pi][:, :], lambda kc=kc, b=b: hT[:, kc, b * 128:(b + 1) * 128],
                                           lambda kc=kc: wu[:, kc, :], kc == 0, kc == 7, [B_hT, B_wu], psB[pi])
                                    ut, ub = ust[uq[0] % 2]
                                    uq[0] += 1
                                    P.op("dve", lambda pi=pi, ut=ut: nc.vector.tensor_copy(out=ut[:], in_=ps[pi][:]), reads=[psB[pi]], writes=[ub])
                                    udst, uB = (u_own[gb // 8], B_uown) if isl else (u_ctx, B_uctx)
                                    gr = gb % 8 if isl else gb
                                    P.op("sp", lambda ut=ut, udst=udst, gr=gr: nc.sync.dma_start(out=udst[gr * 128:(gr + 1) * 128, :], in_=ut[:]),
                                         reads=[ub], writes=[uB], dma=True)
                                    pi = nps()
                                    for kc in range(8):
                                        mm(lambda pi=pi: ps[pi][:, 0:128], lambda kc=kc, b=b: hT[:, kc, b * 128:(b + 1) * 128],
                                           lambda kc=kc: wv[:, kc, :], kc == 0, kc == 7, [B_hT, B_wv], psB[pi])
                                    vdst, vB = (Vown, B_V) if isl else (Vc, B_Vc)
                                    P.op("dve", lambda pi=pi, vdst=vdst, gb=gb: nc.vector.tensor_copy(
                                        out=vdst[:, gb, :, 0:64], in_=ps[pi][:, 0:128].rearrange("p (h d) -> p h d", h=2)),
                                        reads=[psB[pi]], writes=[vB])
                                for h in range(2):
                                    pa = nps()
                                    for kc in range(8):
                                        mm(lambda pa=pa: ps[pa][0:64, :n], lambda kc=kc, h=h: wk[:, kc, h * 128:h * 128 + 64],
                                           lambda kc=kc: hT[:, kc, :n], kc == 0, kc == 7, [B_hT, B_wk], psB[pa])
                                    if not isl:
                                        P.op("dve", lambda pa=pa, h=h: nc.vector.tensor_copy(out=KcT[0:64, h, :], in_=ps[pa][0:64, :n]),
                                             reads=[psB[pa]], writes=[B_KcT])
                                        continue
                                    pb = nps()
                                    for kc in range(8):
                                        mm(lambda pb=pb: ps[pb][0:64, :n], lambda kc=kc, h=h: wk[:, kc, (2 + h) * 128:(2 + h) * 128 + 64],
                                           lambda kc=kc: hT[:, kc, :n], kc == 0, kc == 7, [B_hT, B_wk], psB[pb])
                                    t1, b1 = r1[h]
                                    t2, b2 = r2[h]
                                    P.op("dve", lambda pa=pa, t1=t1, off=off: nc.vector.tensor_tensor(out=t1[0:64, :], in0=ps[pa][0:64, :], in1=rc[0:64, off:off + 512], op=ALU.mult),
                                         reads=[psB[pa], B_rc], writes=[b1])
                                    P.op("dve", lambda pb=pb, t2=t2, off=off: nc.vector.tensor_tensor(out=t2[0:64, :], in0=ps[pb][0:64, :], in1=rs[0:64, off:off + 512], op=ALU.mult),
                                         reads=[psB[pb], B_rs], writes=[b2])
                                    P.op("pool", lambda t1=t1, t2=t2, h=h, off=off: nc.gpsimd.tensor_tensor(out=KT[0:64, h, off:off + 512], in0=t1[0:64, :], in1=t2[0:64, :], op=ALU.add),
                                         reads=[b1, b2], writes=[B_KT])
                        stop_pt("p1b")
                        P.op("sp", lambda: nc.sync.dma_start(out=halo_in[:, 0:256].rearrange("p (h t) -> p h t", h=2), in_=KT[:, :, 0:128]),
                             reads=[B_KT], writes=[B_hin], dma=True)
                        P.op("sp", lambda: nc.sync.dma_start(out=halo_in[:, 256:512].rearrange("p (h t) -> p h t", h=2), in_=KT[:, :, NT - 128:NT]),
                             reads=[B_KT], writes=[B_hin], dma=True)
                        P.op("sp", lambda: nc.sync.dma_start(out=halo_in[:, 512:672], in_=Vown[:, 0].rearrange("p h e -> p (h e)")),
                             reads=[B_V], writes=[B_hin], dma=True)
                        P.op("sp", lambda: nc.sync.dma_start(out=halo_in[:, 672:832], in_=Vown[:, 15].rearrange("p h e -> p (h e)")),
                             reads=[B_V], writes=[B_hin], dma=True)
                        stop_pt("p1c")
                        P.op("pool", lambda: nc.gpsimd.collective_compute("AllGather", ALU.bypass, replica_groups=[[0, 1, 2, 3], [4, 5, 6, 7]],
                                                                          ins=[halo_in.ap().opt()], outs=[halo_all.ap().opt()]),
                             reads=[B_hin], writes=[B_hall], cc=True)
                        for k2 in range(2):
                            P.op("pool", lambda k2=k2: nc.gpsimd.collective_compute("AllGather", ALU.bypass, replica_groups=[[0, 1, 2, 3], [4, 5, 6, 7]],
                                                                                    ins=[u_own[k2].ap().opt()], outs=[u_all[k2].ap().opt()]),
                                 reads=[B_uown], writes=[B_uall], cc=True)
                        stop_pt("p1d")
                        hall, B_hl = SB(ph, "hall", [128, 4, 1024], BF16)
                        P.op("sp", lambda: nc.sync.dma_start(out=hall[:], in_=halo_all.ap().rearrange("(r p) n -> p r n", p=128)),
                             reads=[B_hall], writes=[B_hl], dma=True)
                        for (dst, Bd, so) in ((hp, B_hp, 0), (hn, B_hn, 4)):
                            P.op("dve", lambda dst=dst, so=so: nc.vector.tensor_scalar(out=dst[:, 0:832], in0=hall[:, 0, 0:832], scalar1=sel[:, so:so + 1], scalar2=None, op0=ALU.mult),
                                 reads=[B_hl, B_sel], writes=[Bd])
                            for r in range(1, 4):
                                P.op("dve", lambda dst=dst, so=so, r=r: nc.vector.scalar_tensor_tensor(
                                    out=dst[:, 0:832], in0=hall[:, r, 0:832], scalar=sel[:, so + r:so + r + 1], in1=dst[:, 0:832], op0=ALU.mult, op1=ALU.add),
                                    reads=[B_hl, B_sel], writes=[Bd])
                        P.barrier()
                        stop_pt("p1_%d" % l)

                    with ExitStack() as ph:
                        nb = norm_bufs(ph)
                        hT, B_hT = SB(ph, "hT", [128, 8, 512], BF16)
                        wq, B_wq = SB(ph, "wq", [128, 8, 1024], BF16)
                        rc, B_rc = SB(ph, "rc", [128, NT], BF16)
                        rs, B_rs = SB(ph, "rs", [128, NT], BF16)
                        QT, B_QT = SB(ph, "QT", [128, 8, 512], BF16)
                        r1 = [SB(ph, "r1_%d" % i, [128, 512], F32) for i in range(2)]
                        r2 = [SB(ph, "r2_%d" % i, [128, 512], F32) for i in range(2)]
                        pt = [SB(ph, "pt%d" % i, [128, 512], BF16) for i in range(6)]
                        pslist[0] = [0, 1, 2, 3]
                        qcount = [0]
                        Ons = [SB(ph, "On%d" % i, [128, 512], F32) for i in range(2)]
                        pendT = []
                        den, B_den = SB(ph, "den", [128, 8], F32)
                        wload(lambda: wq[:], B_wq, lambda: win_d[l, :, O_Q:O_Q + 1024].rearrange("(kc p) n -> p kc n", p=128))
                        ld(lambda: rc[:], B_rc, lambda: cos_d[:, :])
                        ld(lambda: rs[:], B_rs, lambda: sin_d[:, :])
                        eq = [0]
                        for ts in ([LAT] if last else [CTX, LAT]):
                            isl = ts is LAT
                            for (g, off, n) in ts["tgs"]:
                                norm_tile(nb, ts, g, off, n, lambda c, ts=ts: g1m[:, l, c, ts["s"]:ts["s"] + 1], mods(ts, 0), hT, B_hT, [B_g1l[l], B_modl[l]])
                                for hd in range(8):
                                    pa = nps()
                                    for kc in range(8):
                                        mm(lambda pa=pa: ps[pa][0:64, :n], lambda kc=kc, hd=hd: wq[:, kc, hd * 64:(hd + 1) * 64],
                                           lambda kc=kc: hT[:, kc, :n], kc == 0, kc == 7, [B_hT, B_wq], psB[pa])
                                    if not isl:
                                        P.op("dve", lambda pa=pa, hd=hd: nc.vector.tensor_copy(out=QT[0:64, hd, :n], in_=ps[pa][0:64, :n]),
                                             reads=[psB[pa]], writes=[B_QT])
                                        continue
                                    pb = nps()
                                    for kc in range(8):
                                        mm(lambda pb=pb: ps[pb][0:64, :n], lambda kc=kc, hd=hd: wq[:, kc, 512 + hd * 64:512 + (hd + 1) * 64],
                                           lambda kc=kc: hT[:, kc, :n], kc == 0, kc == 7, [B_hT, B_wq], psB[pb])
                                    t1, b1 = r1[hd % 2]
                                    t2, b2 = r2[hd % 2]
                                    P.op("dve", lambda pa=pa, t1=t1, off=off: nc.vector.tensor_tensor(out=t1[0:64, :], in0=ps[pa][0:64, :], in1=rc[0:64, off:off + 512], op=ALU.mult),
                                         reads=[psB[pa], B_rc], writes=[b1])
                                    P.op("dve", lambda pb=pb, t2=t2, off=off: nc.vector.tensor_tensor(out=t2[0:64, :], in0=ps[pb][0:64, :], in1=rs[0:64, off:off + 512], op=ALU.mult),
                                         reads=[psB[pb], B_rs], writes=[b2])
                                    P.op("pool", lambda t1=t1, t2=t2, hd=hd: nc.gpsimd.tensor_tensor(out=QT[0:64, hd, :], in0=t1[0:64, :], in1=t2[0:64, :], op=ALU.add),
                                         reads=[b1, b2], writes=[B_QT])
                                for qb in range(n // 128):
                                    gb = off // 128 + qb
                                    po2 = [4 + (qcount[0] % 2) * 2 + h for h in range(2)]
                                    On, B_On = Ons[qcount[0] % 2]
                                    qcount[0] += 1
                                    keysets = []
                                    for h in range(2):
                                        keys = []
                                        if isl:
                                            if gb == 0:
                                                keys.append((lambda h=h: hp[0:64, 256 + h * 128:256 + (h + 1) * 128],
                                                             lambda h=h: hp[:, 672 + h * 80:672 + h * 80 + 65], 2, [B_hp]))
                                            else:
                                                keys.append((lambda h=h, gb=gb: KT[0:64, h, (gb - 1) * 128:gb * 128],
                                                             lambda h=h, gb=gb: Vown[:, gb - 1, h, 0:65], 0, [B_KT, B_V]))
                                            keys.append((lambda h=h, gb=gb: KT[0:64, h, gb * 128:(gb + 1) * 128],
                                                         lambda h=h, gb=gb: Vown[:, gb, h, 0:65], None, [B_KT, B_V]))
                                            if gb == 15:
                                                keys.append((lambda h=h: hn[0:64, h * 128:(h + 1) * 128],
                                                             lambda h=h: hn[:, 512 + h * 80:512 + h * 80 + 65], 3, [B_hn]))
                                            else:
                                                keys.append((lambda h=h, gb=gb: KT[0:64, h, (gb + 1) * 128:(gb + 2) * 128],
                                                             lambda h=h, gb=gb: Vown[:, gb + 1, h, 0:65], 1, [B_KT, B_V]))
                                        for cb in range(2):
                                            keys.append((lambda h=h, cb=cb: KcT[0:64, h, cb * 128:(cb + 1) * 128],
                                                         lambda h=h, cb=cb: Vc[:, cb, h, 0:65], None, [B_KcT, B_Vc]))
                                        keysets.append(keys)
                                    nk = len(keysets[0])
                                    jobs = [(h, ki) for ki in range(nk) for h in range(2)]
                                    LAG = 3
                                    pend = []
                                    for s_ in range(len(jobs) + LAG):
                                        if s_ < len(jobs):
                                            h, ki = jobs[s_]
                                            kap, vap, mk, kr = keysets[h][ki]
                                            pi = nps()
                                            mm(lambda pi=pi: ps[pi][:, :], lambda kap=kap: kap(),
                                               lambda h=h, qb=qb: QT[0:64, 4 * h:4 * h + 4, qb * 128:(qb + 1) * 128],
                                               True, True, [B_QT] + kr, psB[pi])
                                            ptt, pb_ = pt[eq[0] % len(pt)]
                                            eq[0] += 1
                                            P.op("act", lambda pi=pi, ptt=ptt: nc.scalar.activation(out=ptt[:], in_=ps[pi][:], func=AF.Exp, scale=0.125),
                                                 reads=[psB[pi]], writes=[pb_])
                                            if mk is not None:
                                                P.op("pool", lambda ptt=ptt, mk=mk: nc.gpsimd.tensor_tensor(out=ptt[:], in0=ptt[:], in1=masks[:, mk, :], op=ALU.mult),
                                                     reads=[pb_, B_masks], writes=[pb_])
                                            pend.append((h, ki, ptt, pb_, vap, kr))
                                        if s_ == min(8, len(jobs) - 1) and pendT:
                                            pendT.pop(0)()
                                        if s_ >= LAG:
                                            h, ki, ptt, pb_, vap, kr = pend[s_ - LAG]
                                            po = po2[h]
                                            for gi in range(4):
                                                col = gi * 128
                                                mm(lambda po=po, gi=gi: ps[po][:, gi * 80:gi * 80 + 65], lambda ptt=ptt, col=col: ptt[:, col:col + 128],
                                                   lambda vap=vap: vap(), ki == 0, ki == nk - 1, [pb_] + kr, psB[po])
                                    for h in range(2):
                                        po = po2[h]
                                        P.op("dve", lambda po=po, h=h: nc.vector.tensor_tensor(
                                            out=den[:, 4 * h:4 * h + 4], in0=ps[po][:, 0:320].rearrange("p (g e) -> p g e", e=80)[:, :, 64],
                                            in1=esink[:, l * 8 + 4 * h:l * 8 + 4 * h + 4], op=ALU.add),
                                            reads=[psB[po], B_esink], writes=[B_den])
                                    P.op("dve", lambda: nc.vector.reciprocal(out=den[:], in_=den[:]), reads=[B_den], writes=[B_den])
                                    for h in range(2):
                                        po = po2[h]
                                        for gi in range(4):
                                            P.op("dve", lambda po=po, gi=gi, h=h: nc.vector.tensor_scalar(
                                                out=On[:, (4 * h + gi) * 64:(4 * h + gi + 1) * 64], in0=ps[po][:, gi * 80:gi * 80 + 64],
                                                scalar1=den[:, 4 * h + gi:4 * h + gi + 1], scalar2=None, op0=ALU.mult),
                                                reads=[psB[po], B_den], writes=[B_On])
                                    def emit_T(On=On, B_On=B_On, gb=gb, isl=isl):
                                        pi = nps()
                                        for c4 in range(4):
                                            P.op("pe", lambda pi=pi, c4=c4: nc.tensor.transpose(out=ps[pi][:, c4 * 128:(c4 + 1) * 128], in_=On[:, c4 * 128:(c4 + 1) * 128], identity=ident[:]),
                                                 reads=[B_On, B_ident], writes=[psB[pi]])
                                        odst, oB = (OT, B_OT) if isl else (OcT, B_OcT)
                                        P.op("dve", lambda pi=pi, odst=odst: nc.vector.tensor_copy(
                                            out=odst[:, :, gb * 128:(gb + 1) * 128], in_=ps[pi][:].rearrange("p (c t) -> p c t", c=4)),
                                            reads=[psB[pi]], writes=[oB])
                                    pendT.append(emit_T)
                                while pendT:
                                    pendT.pop(0)()
                        pslist[0] = list(range(8))
                        P.barrier()
                        stop_pt("pa_%d" % l)
                    att.close()

                    with ExitStack() as ph:
                        ub = [SB(ph, "ub%d" % i, [128, 512], BF16) for i in range(3)]
                        tcb = [SB(ph, "tcb%d" % i, [128, 512], BF16) for i in range(3)]
                        tsb = [SB(ph, "tsb%d" % i, [128, 512], BF16) for i in range(3)]
                        yri, B_yri = SB(ph, "yri", [128, 8, 512], BF16)
                        dq = [0]
                        for ts in ([] if last else [CTX]):
                            isl = ts is LAT
                            uB = B_uall if isl else B_uctx
                            tc_d, ts_d = (tabcc_d, tabsc_d)
                            nblk = 64 if isl else 2
                            for (g, off, n) in ts["tgs"]:
                                acc = [nps() for _ in range(8)]
                                for nbk in range(nblk):
                                    i3 = dq[0] % 3
                                    dq[0] += 1
                                    (ut, uBb), (ct, cB), (st_, sB_) = ub[i3], tcb[i3], tsb[i3]
                                    if not isl:
                                        usrc, urow = u_ctx, nbk * 128
                                        P.op("sp", lambda ut=ut, usrc=usrc, urow=urow: nc.sync.dma_start(out=ut[:], in_=usrc[urow:urow + 128, :]),
                                             reads=[uB], writes=[uBb], dma=True)
                                        uap = lambda gg, ut=ut: ut[:, gg * 128:(gg + 1) * 128]

                                    P.op("sp", lambda ct=ct, tc_d=tc_d, nbk=nbk, off=off, n=n: nc.sync.dma_start(out=ct[:, :n], in_=tc_d[nbk * 128:(nbk + 1) * 128, off:off + n]),
                                         writes=[cB], dma=True)
                                    P.op("sp", lambda st_=st_, ts_d=ts_d, nbk=nbk, off=off, n=n: nc.sync.dma_start(out=st_[:, :n], in_=ts_d[nbk * 128:(nbk + 1) * 128, off:off + n]),
                                         writes=[sB_], dma=True)
                                    for gg in range(4):
                                        mm(lambda gg=gg: ps[acc[gg]][:, :n], lambda uap=uap, gg=gg: uap(gg), lambda ct=ct: ct[:, :n],
                                           nbk == 0, nbk == nblk - 1, [uBb, cB], psB[acc[gg]])
                                        mm(lambda gg=gg: ps[acc[4 + gg]][:, :n], lambda uap=uap, gg=gg: uap(gg), lambda st_=st_: st_[:, :n],
                                           nbk == 0, nbk == nblk - 1, [uBb, sB_], psB[acc[4 + gg]])
                                for j in range(8):
                                    P.op("dve", lambda j=j: nc.vector.tensor_copy(out=yri[:, j, :n], in_=ps[acc[j]][:, :n]), reads=[psB[acc[j]]], writes=[B_yri])
                                fdst, fB = (FT, B_FT) if isl else (FcT, B_FcT)
                                for gg in range(4):
                                    pi = nps()
                                    mm(lambda pi=pi: ps[pi][:, :n], lambda: ccsc[:, 0:128], lambda gg=gg: yri[:, gg, :n], True, False, [B_ccsc, B_yri], psB[pi])
                                    mm(lambda pi=pi: ps[pi][:, :n], lambda: ccsc[:, 128:256], lambda gg=gg: yri[:, 4 + gg, :n], False, True, [B_ccsc, B_yri], psB[pi])
                                    P.op("dve", lambda pi=pi, fdst=fdst, gg=gg, off=off, n=n: nc.vector.tensor_copy(out=fdst[:, gg, off:off + n], in_=ps[pi][:, :n]),
                                         reads=[psB[pi]], writes=[fB])
                        P.barrier()
                        stop_pt("pb0c_%d" % l)
                    with ExitStack() as ph:
                        Tb_ = [SB(ph, "ctT%d" % i, [128, 8, 512], BF16) for i in range(2)]
                        st1 = [SB(ph, "st1_%d" % i, [128, 2, 8, 512], BF16) for i in range(2)]
                        def ct_loads(n2c):
                            Tt, TB = Tb_[n2c % 2]
                            for r_ in range(4):
                                for k2_ in range(2):
                                    P.op("sp", lambda Tt=Tt, r_=r_, k2_=k2_, n2c=n2c: nc.sync.dma_start(
                                        out=Tt[r_ * 32 + k2_ * 16:r_ * 32 + k2_ * 16 + 16, :, :],
                                        in_=u_all[k2_][r_ * 1024:(r_ + 1) * 1024, :].rearrange("(a n) c -> a n c", n=64)[:, n2c * 8:(n2c + 1) * 8, :]),
                                        reads=[B_uall], writes=[TB], dma=True)
                        ct_loads(0)
                        for n2c in range(8):
                            Tt, TB = Tb_[n2c % 2]
                            s1t, s1B = st1[n2c % 2]
                            if n2c + 1 < 8:
                                ct_loads(n2c + 1)
                            for n2i in range(8):
                                pr, pi_ = nps(), nps()
                                mm(lambda pr=pr: ps[pr][:, :], lambda: c128[:, 0:128], lambda Tt=Tt, n2i=n2i: Tt[:, n2i, :], True, True, [B_c128, TB], psB[pr])
                                mm(lambda pi_=pi_: ps[pi_][:, :], lambda: c128[:, 128:256], lambda Tt=Tt, n2i=n2i: Tt[:, n2i, :], True, True, [B_c128, TB], psB[pi_])
                                P.op("act", lambda pr=pr, s1t=s1t, n2i=n2i: nc.scalar.copy(out=s1t[:, 0, n2i, :], in_=ps[pr][:, :]), reads=[psB[pr]], writes=[s1B])
                                P.op("dve", lambda pi_=pi_, s1t=s1t, n2i=n2i: nc.vector.tensor_copy(out=s1t[:, 1, n2i, :], in_=ps[pi_][:, :]), reads=[psB[pi_]], writes=[s1B])
                            for ri in range(2):
                                P.op("sp", lambda s1t=s1t, ri=ri, n2c=n2c: nc.sync.dma_start(out=s1_d[ri, :, n2c * 8:(n2c + 1) * 8, :], in_=s1t[:, ri, :, :]),
                                     reads=[s1B], writes=[B_s1], dma=True)
                        P.barrier()
                        stop_pt("pb0s1_%d" % l)
                    with ExitStack() as ph:
                        Yb, B_Yb = SB(ph, "ctY", [128, 4, 2, NT], BF16)
                        Rb_ = [SB(ph, "ctR%d" % i, [128, 8, 512], BF16) for i in range(2)]
                        t23, B_t23 = SB(ph, "t23", [128, 128, 32], BF16)
                        ld(lambda: t23[:], B_t23, lambda: tab23_d.ap().rearrange("p (k c) -> p k c", c=32))
                        acc = None
                        for k1c in range(16):
                            Rt, RB = Rb_[k1c % 2]
                            for ri in range(2):
                                P.op("sp", lambda Rt=Rt, ri=ri, k1c=k1c: nc.sync.dma_start(
                                    out=Rt[ri * 64:(ri + 1) * 64, :, :], in_=s1_d[ri, k1c * 8:(k1c + 1) * 8, :, :].rearrange("k n c -> n k c")),
                                    reads=[B_s1], writes=[RB], dma=True)
                            if k1c % 2 == 0:
                                acc = [nps() for _ in range(4)]
                            for k1i in range(8):
                                k1 = k1c * 8 + k1i
                                for gg in range(4):
                                    mm(lambda gg=gg, k1=k1: ps[acc[gg]][:, (k1 % 16) * 32:(k1 % 16) * 32 + 32],
                                       lambda Rt=Rt, k1i=k1i, gg=gg: Rt[:, k1i, gg * 128:(gg + 1) * 128],
                                       lambda k1=k1: t23[:, k1, :], True, True, [RB, B_t23], psB[acc[gg]])
                            if k1c % 2 == 1:
                                kbase = (k1c - 1) * 8
                                for gg in range(4):
                                    for ro in range(2):
                                        src = lambda gg=gg, ro=ro: ps[acc[gg]][:, :].rearrange("p (a r b) -> p a r b", r=2, b=16)[:, :, ro, :]
                                        dst = lambda gg=gg, ro=ro, kbase=kbase: Yb[:, gg, ro, :].rearrange("p (b a) -> p a b", a=128)[:, kbase:kbase + 16, :]
                                        if gg % 2 == 0:
                                            P.op("act", lambda src=src, dst=dst: nc.scalar.copy(out=dst(), in_=src()), reads=[psB[acc[gg]]], writes=[B_Yb])
                                        else:
                                            P.op("dve", lambda src=src, dst=dst: nc.vector.tensor_copy(out=dst(), in_=src()), reads=[psB[acc[gg]]], writes=[B_Yb])
                        for tg_ in range(4):
                            for gg in range(4):
                                pi = nps()
                                mm(lambda pi=pi: ps[pi][:, :], lambda: ccsc[:, 0:128], lambda gg=gg, tg_=tg_: Yb[:, gg, 0, tg_ * 512:(tg_ + 1) * 512], True, False, [B_ccsc, B_Yb], psB[pi])
                                mm(lambda pi=pi: ps[pi][:, :], lambda: ccsc[:, 128:256], lambda gg=gg, tg_=tg_: Yb[:, gg, 1, tg_ * 512:(tg_ + 1) * 512], False, True, [B_ccsc, B_Yb], psB[pi])
                                P.op("dve", lambda pi=pi, gg=gg, tg_=tg_: nc.vector.tensor_copy(out=FT[:, gg, tg_ * 512:(tg_ + 1) * 512], in_=ps[pi][:, :]),
                                     reads=[psB[pi]], writes=[B_FT])
                        P.barrier()
                        stop_pt("pb0_%d" % l)

                    with ExitStack() as ph:
                        nb = norm_bufs(ph)
                        hT, B_hT = SB(ph, "hT", [128, 8, 256], BF16)
                        wg2, B_wg2 = SB(ph, "wg2", [128, 8, 2048], BF16)
                        wf, B_wf = SB(ph, "wf", [128, 4, D], BF16)
                        wa, B_wa = SB(ph, "wa", [128, 4, D], BF16)
                        wo, B_wo = SB(ph, "wo", [128, 8, D], BF16)
                        yT, B_yT = SB(ph, "yT", [128, 8, 256], BF16)
                        sg = [SB(ph, "sg%d" % i, [128, 256], F32) for i in range(4)]
                        tm = [SB(ph, "tm%d" % i, [128, 256], F32) for i in range(4)]
                        wload(lambda: wf[:], B_wf, lambda: wf_d[l].rearrange("(kc p) n -> p kc n", p=128))
                        wload(lambda: wa[:], B_wa, lambda: wa_d[l].rearrange("(kc p) n -> p kc n", p=128))
                        wload(lambda: wg2[:], B_wg2, lambda: win_d[l, :, O_G:O_G + 2048].rearrange("(kc p) n -> p kc n", p=128))
                        wload(lambda: wo[:], B_wo, lambda: wo_d[l].rearrange("(kc p) n -> p kc n", p=128))
                        for ts in ([LAT] if last else [CTX, LAT]):
                            isl = ts is LAT
                            fsrc, fB = (FT, B_FT) if isl else (FcT, B_FcT)
                            osrc, oB = (OT, B_OT) if isl else (OcT, B_OcT)
                            s = ts["s"]
                            for (g, off, n) in [(o_ // 512, o_, 256) for o_ in range(0, ts["n"], 256)]:
                                norm_tile(nb, ts, g, off, n, lambda c, s=s: g1m[:, l, c, s:s + 1], mods(ts, 0), hT, B_hT, [B_g1l[l], B_modl[l]])
                                for dc in range(8):
                                    pF, pA, pGf, pGa = nps(), nps(), nps(), nps()
                                    for gg in range(4):
                                        mm(lambda pF=pF: ps[pF][:, :n], lambda gg=gg, dc=dc: wf[:, gg, dc * 128:(dc + 1) * 128],
                                           lambda gg=gg: fsrc[:, gg, off:off + n], gg == 0, gg == 3, [B_wf, fB], psB[pF])
                                    for gg in range(4):
                                        mm(lambda pA=pA: ps[pA][:, :n], lambda gg=gg, dc=dc: wa[:, gg, dc * 128:(dc + 1) * 128],
                                           lambda gg=gg: osrc[:, gg, off:off + n], gg == 0, gg == 3, [B_wa, oB], psB[pA])
                                    for kc in range(8):
                                        mm(lambda pGf=pGf: ps[pGf][:, :n], lambda kc=kc, dc=dc: wg2[:, kc, dc * 128:(dc + 1) * 128],
                                           lambda kc=kc: hT[:, kc, :n], kc == 0, kc == 7, [B_wg2, B_hT], psB[pGf])
                                    for kc in range(8):
                                        mm(lambda pGa=pGa: ps[pGa][:, :n], lambda kc=kc, dc=dc: wg2[:, kc, D + dc * 128:D + (dc + 1) * 128],
                                           lambda kc=kc: hT[:, kc, :n], kc == 0, kc == 7, [B_wg2, B_hT], psB[pGa])
                                    i0, i1 = (dc % 2) * 2, (dc % 2) * 2 + 1
                                    P.op("act", lambda pGf=pGf, i0=i0: nc.scalar.activation(out=sg[i0][0][:, :n], in_=ps[pGf][:, :n], func=AF.Sigmoid),
                                         reads=[psB[pGf]], writes=[sg[i0][1]])
                                    P.op("act", lambda pGa=pGa, i1=i1: nc.scalar.activation(out=sg[i1][0][:, :n], in_=ps[pGa][:, :n], func=AF.Sigmoid),
                                         reads=[psB[pGa]], writes=[sg[i1][1]])
                                    P.op("dve", lambda pF=pF, i0=i0: nc.vector.tensor_tensor(out=tm[i0][0][:, :n], in0=ps[pF][:, :n], in1=sg[i0][0][:, :n], op=ALU.mult),
                                         reads=[psB[pF], sg[i0][1]], writes=[tm[i0][1]])
                                    P.op("dve", lambda pA=pA, i1=i1: nc.vector.tensor_tensor(out=tm[i1][0][:, :n], in0=ps[pA][:, :n], in1=sg[i1][0][:, :n], op=ALU.mult),
                                         reads=[psB[pA], sg[i1][1]], writes=[tm[i1][1]])
                                    P.op("pool", lambda i0=i0, i1=i1, dc=dc: nc.gpsimd.tensor_tensor(out=yT[:, dc, :n], in0=tm[i0][0][:, :n], in1=tm[i1][0][:, :n], op=ALU.add),
                                         reads=[tm[i0][1], tm[i1][1]], writes=[B_yT])
                                res, rb = ts["res"], ts["rb"]
                                for dc in range(8):
                                    pZ = nps()
                                    for kc in range(8):
                                        mm(lambda pZ=pZ: ps[pZ][:, :n], lambda kc=kc, dc=dc: wo[:, kc, dc * 128:(dc + 1) * 128],
                                           lambda kc=kc: yT[:, kc, :n], kc == 0, kc == 7, [B_wo, B_yT], psB[pZ])
                                    P.op("dve", lambda pZ=pZ, dc=dc, res=res, s=s: nc.vector.scalar_tensor_tensor(
                                        out=res[:, dc, off:off + n], in0=ps[pZ][:, :n], scalar=mod[:, l, 16 + dc, s:s + 1], in1=res[:, dc, off:off + n],
                                        op0=ALU.mult, op1=ALU.add), reads=[psB[pZ], B_modl[l], rb[(dc, g)]], writes=[rb[(dc, g)]])
                        P.barrier()

                stop_pt("mix%d" % l)
                with ExitStack() as ph:
                    nb = norm_bufs(ph)
                    NTOT = NT + (0 if last else CT)
                    h2, B_h2 = SB(ph, "h2", [128, 8, NTOT], BF16)
                    sets = [LAT] if last else [LAT, CTX]
                    tgl = []
                    for ts in sets:
                        for (g, off, n) in ts["tgs"]:
                            h2off = off if ts is LAT else NT
                            tgl.append((ts, g, off, n, h2off))
                    hB2 = {i: Buf("h2_%d" % i, True) for i in range(len(tgl))}
                    def emit_norm(ti):
                        ts, g, off, n, h2off = tgl[ti]
                        s = ts["s"]

                        class _V:
                            def __init__(self, o):
                                self.o = o

                            def __getitem__(self, k):
                                p, c, sl = k
                                return h2[p, c, self.o + (sl.start or 0):self.o + sl.stop]
                        norm_tile(nb, ts, g, off, n, lambda c, s=s: g2m[:, l, c, s:s + 1], mods(ts, 24), None, hB2[ti], [B_g2l[l], B_modl[l]],
                                  hout=lambda c, h2off=h2off, n=n: h2[:, c, h2off:h2off + n], sqb=(_V(h2off), hB2[ti]))
                    if last:
                        wr, B_wr = SB(ph, "wr", [128, 8, 8], BF16)
                        comb, B_comb = SB(ph, "comb", [128, 16, 8], F32)
                        combT, B_combT = SB(ph, "combT", [8, NT], BF16)
                        esel, B_esel = SB(ph, "esel", [8, 8, 128], BF16)
                        lg, B_lg = SB(ph, "lg", [128, 8], F32)
                        m1, B_m1 = SB(ph, "m1", [128, 4], F32)
                        e1, B_e1 = SB(ph, "e1", [128, 8], F32)
                        l2, B_l2 = SB(ph, "l2", [128, 8], F32)
                        wload(lambda: wr[:], B_wr, lambda: wr_d.ap().rearrange("(kc p) n -> p kc n", p=128))
                        ld(lambda: esel[:], B_esel, lambda: esel_d.ap().rearrange("p (e n) -> p e n", e=8))
                        B_combTi = {i: Buf("combT%d" % i, True) for i in range(4)}

                        def router_blk(blk):
                            pi = nps()
                            for kc in range(8):
                                mm(lambda pi=pi: ps[pi][:, 0:8], lambda kc=kc, blk=blk: h2[:, kc, blk * 128:(blk + 1) * 128],
                                   lambda kc=kc: wr[:, kc, :], kc == 0, kc == 7, [hB2[blk // 4], B_wr], psB[pi])
                            V = nc.vector
                            P.op("dve", lambda pi=pi: V.tensor_copy(out=lg[:], in_=ps[pi][:, 0:8]), reads=[psB[pi]], writes=[B_lg])
                            P.op("dve", lambda: V.reduce_max(out=m1[:, 0:1], in_=lg[:], axis=AX.X), reads=[B_lg], writes=[B_m1])
                            P.op("dve", lambda: V.tensor_scalar(out=e1[:], in0=lg[:], scalar1=m1[:, 0:1], scalar2=None, op0=ALU.is_equal), reads=[B_lg, B_m1], writes=[B_e1])
                            P.op("dve", lambda: V.scalar_tensor_tensor(out=l2[:], in0=e1[:], scalar=-1e30, in1=lg[:], op0=ALU.mult, op1=ALU.add), reads=[B_e1, B_lg], writes=[B_l2])
                            P.op("dve", lambda: V.reduce_max(out=m1[:, 1:2], in_=l2[:], axis=AX.X), reads=[B_l2, B_m1], writes=[B_m1])
                            P.op("dve", lambda: V.tensor_scalar(out=e1[:], in0=lg[:], scalar1=m1[:, 1:2], scalar2=None, op0=ALU.is_ge), reads=[B_lg, B_m1, B_l2], writes=[B_e1])
                            P.op("dve", lambda: V.tensor_scalar(out=l2[:], in0=lg[:], scalar1=m1[:, 0:1], scalar2=None, op0=ALU.subtract), reads=[B_lg, B_m1, B_e1], writes=[B_l2])
                            P.op("act", lambda: nc.scalar.activation(out=l2[:], in_=l2[:], func=AF.Exp), reads=[B_l2], writes=[B_l2])
                            P.op("dve", lambda: V.tensor_tensor(out=l2[:], in0=l2[:], in1=e1[:], op=ALU.mult), reads=[B_l2, B_e1], writes=[B_l2])
                            P.op("dve", lambda: V.reduce_sum(out=m1[:, 2:3], in_=l2[:], axis=AX.X), reads=[B_l2, B_m1], writes=[B_m1])
                            P.op("dve", lambda: V.reciprocal(out=m1[:, 3:4], in_=m1[:, 2:3]), reads=[B_m1], writes=[B_m1])
                            P.op("dve", lambda blk=blk: V.tensor_scalar(out=comb[:, blk, :], in0=l2[:], scalar1=m1[:, 3:4], scalar2=None, op0=ALU.mult),
                                 reads=[B_l2, B_m1], writes=[B_comb])
                            pj = nps()
                            P.op("pe", lambda pj=pj, blk=blk: nc.tensor.transpose(out=ps[pj][0:8, 0:128], in_=comb[:, blk, :], identity=ident[:]),
                                 reads=[B_comb, B_ident], writes=[psB[pj]])
                            P.op("dve", lambda pj=pj, blk=blk: V.tensor_copy(out=combT[:, blk * 128:(blk + 1) * 128], in_=ps[pj][0:8, 0:128]),
                                 reads=[psB[pj]], writes=[B_combTi[blk // 4]])
                    emitted = [0]

                    def ensure_norm(k):
                        while emitted[0] <= min(k, len(tgl) - 1):
                            ti = emitted[0]
                            emitted[0] += 1
                            emit_norm(ti)
                            if last:
                                for blk in range(4 * ti, 4 * ti + 4):
                                    router_blk(blk)
                    ensure_norm(1)
                    if last:
                        units = []
                        for e in range(8):
                            for pc in range(7):
                                units.append((e, 4, (lambda e=e, pc=pc: wge_d[e, :, pc * 512:(pc + 1) * 512]),
                                              (lambda e=e, pc=pc: wue_d[e, :, pc * 512:(pc + 1) * 512]),
                                              (lambda e=e, pc=pc: wde_d[e, pc * 512:(pc + 1) * 512, :])))
                    else:
                        units = []
                        for pc in range(6):
                            nf_ = 4 if pc < 5 else 2
                            units.append((None, nf_, (lambda pc=pc, nf_=nf_: wgd_d[:, pc * 512:pc * 512 + nf_ * 128]),
                                          (lambda pc=pc, nf_=nf_: wud_d[:, pc * 512:pc * 512 + nf_ * 128]),
                                          (lambda pc=pc, nf_=nf_: wdd_d[pc * 512:pc * 512 + nf_ * 128, :])))
                    wgu = [SB(ph, "wgu%d" % i, [128, 8, 2, 512], BF16) for i in range(2)]
                    wdn = [SB(ph, "wdn%d" % i, [128, 4, D], BF16) for i in range(2)]
                    aT = [SB(ph, "aT%d" % i, [128, 4, 512], BF16) for i in range(2)]

                    def uload(ui):
                        e, nf, gsrc, usrc_, dsrc = units[ui]
                        wt, wb = wgu[ui % 2]
                        dt_, db = wdn[ui % 2]
                        wload(lambda: wt[:, :, 0, 0:nf * 128], wb, lambda: gsrc().rearrange("(kc p) n -> p kc n", p=128))
                        wload(lambda: wt[:, :, 1, 0:nf * 128], wb, lambda: usrc_().rearrange("(kc p) n -> p kc n", p=128))
                        wload(lambda: dt_[:, 0:nf, :], db, lambda: dsrc().rearrange("(kc p) n -> p kc n", p=128))
                    uload(0)
                    sgl = [SB(ph, "fsg%d" % i, [128, 512], F32) for i in range(2)]
                    ftm = [SB(ph, "ftm%d" % i, [128, 512], F32) for i in range(2)]
                    cbc = [SB(ph, "cbc%d" % i, [128, 512], F32) for i in range(2)]
                    aq = [0]
                    fq = [0]
                    mstep = [0]
                    if not last:
                        wm1 = SB(ph, "wm1", [128, 8, 512], BF16)
                        pslist[0] = [0, 1, 2, 3, 4, 5, 6]
                        mpi1 = 7
                    for ui, (e, nf, gsrc, usrc_, dsrc) in enumerate(units):
                        wt, wb = wgu[ui % 2]
                        dt_, db = wdn[ui % 2]
                        if ui + 1 < len(units):
                            uload(ui + 1)
                        pend = None

                        def down(ti, at, ab):
                            ts, g, off, n, h2off = tgl[ti]
                            res, rb, s = ts["res"], ts["rb"], ts["s"]
                            for dc in range(8):
                                pY = nps()
                                for f in range(nf):
                                    mm(lambda pY=pY: ps[pY][:, :n], lambda f=f, dc=dc: dt_[:, f, dc * 128:(dc + 1) * 128],
                                       lambda f=f: at[:, f, :n], f == 0, f == nf - 1, [db, ab], psB[pY])
                                P.op("dve", lambda pY=pY, dc=dc: nc.vector.scalar_tensor_tensor(
                                    out=res[:, dc, off:off + n], in0=ps[pY][:, :n], scalar=mod[:, l, 40 + dc, s:s + 1], in1=res[:, dc, off:off + n],
                                    op0=ALU.mult, op1=ALU.add), reads=[psB[pY], B_modl[l], rb[(dc, g)]], writes=[rb[(dc, g)]])

                        for ti, (ts, g, off, n, h2off) in enumerate(tgl):
                            ensure_norm(ti + 2)
                            at, ab = aT[aq[0] % 2]
                            aq[0] += 1
                            if e is not None:
                                pc_ = nps()
                                ct_, cb_ = cbc[ti % 2]
                                mm(lambda pc_=pc_: ps[pc_][:, :n], lambda e=e: esel[:, e, :], lambda off=off, n=n: combT[:, off:off + n], True, True, [B_esel, B_combTi[ti]], psB[pc_])
                                P.op("dve", lambda pc_=pc_, ct_=ct_: nc.vector.tensor_copy(out=ct_[:, :n], in_=ps[pc_][:, :n]), reads=[psB[pc_]], writes=[cb_])
                            for f in range(nf):
                                pG, pU = nps(), nps()
                                for kc in range(8):
                                    mm(lambda pG=pG: ps[pG][:, :n], lambda kc=kc, f=f: wt[:, kc, 0, f * 128:(f + 1) * 128],
                                       lambda kc=kc: h2[:, kc, h2off:h2off + n], kc == 0, kc == 7, [wb, hB2[ti]], psB[pG])
                                for kc in range(8):
                                    mm(lambda pU=pU: ps[pU][:, :n], lambda kc=kc, f=f: wt[:, kc, 1, f * 128:(f + 1) * 128],
                                       lambda kc=kc: h2[:, kc, h2off:h2off + n], kc == 0, kc == 7, [wb, hB2[ti]], psB[pU])
                                st2, sb2 = sgl[fq[0] % 2]
                                ft2, fb2 = ftm[fq[0] % 2]
                                fq[0] += 1
                                P.op("act", lambda pG=pG, st2=st2: nc.scalar.activation(out=st2[:, :n], in_=ps[pG][:, :n], func=AF.Silu),
                                     reads=[psB[pG]], writes=[sb2])
                                if e is None:
                                    P.op("dve", lambda pU=pU, st2=st2, at=at, f=f: nc.vector.tensor_tensor(out=at[:, f, :n], in0=ps[pU][:, :n], in1=st2[:, :n], op=ALU.mult),
                                         reads=[psB[pU], sb2], writes=[ab])
                                else:
                                    P.op("dve", lambda pU=pU, st2=st2, ft2=ft2: nc.vector.tensor_tensor(out=ft2[:, :n], in0=ps[pU][:, :n], in1=st2[:, :n], op=ALU.mult),
                                         reads=[psB[pU], sb2], writes=[fb2])
                                    P.op("pool", lambda ft2=ft2, ct_=ct_, at=at, f=f: nc.gpsimd.tensor_tensor(out=at[:, f, :n], in0=ft2[:, :n], in1=ct_[:, :n], op=ALU.mult),
                                         reads=[fb2, cb_], writes=[ab])
                            if pend is not None:
                                down(*pend)
                            pend = (ti, at, ab)
                            if not last and mstep[0] < 12:
                                mods_piece(1, mstep[0], wm1, mpi1)
                                mstep[0] += 1
                        down(*pend)
                    if not last:
                        while mstep[0] < 12:
                            mods_piece(1, mstep[0], wm1, mpi1)
                            mstep[0] += 1
                        mods_fin(1, mpi1)
                        pslist[0] = list(range(8))
                    P.barrier()
                stop_pt("ffn%d" % l)

            P.skip = False
            P.final = True
            with ExitStack() as ph:
                nb = norm_bufs(ph)
                oT, B_oT = SB(ph, "oT", [128, 8, 512], F32)
                sqo = SB(ph, "sqo", [128, 8, 512], BF16)
                ost = [SB(ph, "ost%d" % i, [128, D], F32) for i in range(2)]
                B_out = Buf("out")
                for (g, off, n) in LAT["tgs"]:
                    norm_tile(nb, LAT, g, off, n, lambda c: fing[:, c:c + 1], None, oT, B_oT, [B_fing], sqb=sqo)
                    for b in range(4):
                        gb = g * 4 + b
                        o_t, o_b = ost[gb % 2]
                        for half in range(2):
                            pi = nps()
                            for c in range(4):
                                cc = half * 4 + c
                                P.op("pe", lambda pi=pi, c=c, cc=cc, b=b: nc.tensor.transpose(
                                    out=ps[pi][:, c * 128:(c + 1) * 128], in_=oT[:, cc, b * 128:(b + 1) * 128], identity=ident[:]),
                                    reads=[B_oT, B_ident], writes=[psB[pi]])
                            P.op("dve", lambda pi=pi, o_t=o_t, half=half: nc.vector.tensor_copy(out=o_t[:, half * 512:(half + 1) * 512], in_=ps[pi][:]),
                                 reads=[psB[pi]], writes=[o_b])
                        P.op("sp", lambda o_t=o_t, gb=gb: nc.sync.dma_start(out=out_d[gb * 128:(gb + 1) * 128, :], in_=o_t[:]),
                             reads=[o_b], writes=[B_out], dma=True)
                if not P.dry:
                    for q in ("sp",):
                        m = P.dma_no[q]
                        for i in range(NDMASEM):
                            cnt = (m - i + NDMASEM - 1) // NDMASEM if m > i else 0
                            if cnt > 0:
                                P._wait("sp", (q, i), 16 * cnt)
    return nc


_CACHE = {}


def _consts():
    if "c" in _CACHE:
        return _CACHE["c"]
    c = {}
    c["ident"] = np.eye(128, dtype=np.float32)
    _a = 2 * np.pi * (np.outer(np.arange(128), np.arange(128)) % 128) / 128
    c["c128"] = np.concatenate([np.cos(_a), -np.sin(_a)], axis=1).astype(NPBF)
    c["ones1024"] = np.full((128, 128), 1.0 / 1024, dtype=NPBF)
    d = np.arange(64)
    partner = np.where((d % 32) < 16, d + 16, d - 16)
    c["partner"] = partner
    sign = np.where((d % 32) < 16, -1.0, 1.0)
    inv = 10000.0 ** (-np.arange(16, dtype=np.float64) / 16)
    c["rope"] = (sign, inv)
    kq = np.arange(128)
    mprev = (kq[:, None] >= kq[None, :]).astype(np.float32)
    mnext = (kq[:, None] <= kq[None, :]).astype(np.float32)
    c["mprev"] = np.tile(mprev, (1, 4))
    c["mnext"] = np.tile(mnext, (1, 4))
    ch = np.arange(128)
    ang = 2 * np.pi * ((np.outer(ch, ch)) % 128) / 128
    c["ccsc"] = np.concatenate([np.cos(ang), np.sin(ang)], axis=1) / np.sqrt(128.0)
    n = np.arange(CT)
    ang = 2 * np.pi * ((np.outer(n, n)) % CT) / CT
    c["tabcc"] = (np.cos(ang) / np.sqrt(CT)).astype(NPBF)
    c["tabsc"] = (-np.sin(ang) / np.sqrt(CT)).astype(NPBF)
    es = np.zeros((8, 8, 128), np.float32)
    for e in range(8):
        es[e, e, :] = 1.0
    c["esel"] = es.reshape(8, 1024).astype(NPBF)
    for j in range(4):
        n2 = np.arange(64, dtype=np.int64)
        k1 = np.arange(128, dtype=np.int64)
        k2 = np.arange(16, dtype=np.int64) + 16 * j
        kk = k1[:, None] + 128 * k2[None, :]
        ph_ = 2 * np.pi * ((n2[:, None, None] * kk[None, :, :]) % T).astype(np.float64) / T
        cph, sph = np.cos(ph_) / np.sqrt(T), np.sin(ph_) / np.sqrt(T)
        tb = np.zeros((128, 128, 32), np.float64)
        tb[:64, :, :16] = cph
        tb[64:, :, :16] = sph
        tb[:64, :, 16:] = -sph
        tb[64:, :, 16:] = cph
        c["tab23_%d" % j] = tb.reshape(128, 128 * 32).astype(NPBF)
        t = np.arange(NT) + NT * j
        rows = (t // 64).astype(np.float64)
        cols = (t % 64).astype(np.float64)
        dd = np.arange(128) % 64
        fi = dd % 16
        pos = np.where((dd < 32)[:, None], rows[None, :], cols[None, :])
        a = pos * inv[fi][:, None]
        c["cos%d" % j] = np.cos(a).astype(NPBF)
        c["sin%d" % j] = (np.sin(a) * sign[dd][:, None]).astype(NPBF)
    _CACHE["c"] = c
    return c


def _fm(v, nch):
    return np.ascontiguousarray(np.asarray(v, np.float32).reshape(nch, 128).T)


def kernel(x, c, ctx, c_ctx, w_mod, b_mod, norm1_g, norm2_g, w_in, sink, w_fourier, w_attn, w_out,
           w_gate_d, w_up_d, w_down_d, w_router, w_gate_e, w_up_e, w_down_e, final_g):
    K = _consts()
    f = lambda a: np.ascontiguousarray(np.asarray(a, dtype=np.float32))
    x, c, ctx, c_ctx = f(x), f(c), f(ctx), f(c_ctx)
    w_in = f(w_in)
    partner = K["partner"]
    qcols = np.arange(512)
    qpcols = (qcols // 64) * 64 + partner[qcols % 64]
    kcols = []
    for h in range(2):
        kcols += list(512 + 512 + h * 64 + np.arange(64)) * 2
    kpcols = []
    for h in range(2):
        kpcols += list(512 + 512 + h * 64 + partner) * 2
    cols = np.concatenate([np.arange(512), np.array(kcols), np.array(kpcols), 1152 + np.arange(128),
                           512 + qcols, 512 + qpcols, 1280 + np.arange(2048)]).astype(np.int64)
    assert cols.shape[0] == NCEXT
    w_in_ext = np.ascontiguousarray(w_in[:, :, cols])
    bmod = np.stack([np.repeat(_fm(b_mod[l], 48)[:, :, None], 2, axis=2).reshape(128, 96) for l in range(2)], axis=1).reshape(128, 192)
    n1g = np.stack([np.repeat(_fm(norm1_g[l], 8)[:, :, None], 2, axis=2).reshape(128, 16) for l in range(2)], axis=1).reshape(128, 32)
    n2g = np.stack([np.repeat(_fm(norm2_g[l], 8)[:, :, None], 2, axis=2).reshape(128, 16) for l in range(2)], axis=1).reshape(128, 32)
    fing = _fm(final_g, 8)
    sinkb = np.ascontiguousarray(np.broadcast_to(f(sink).reshape(1, 16), (128, 16)))
    shared = dict(w_mod=f(w_mod), bmod=np.ascontiguousarray(bmod), n1g=np.ascontiguousarray(n1g), n2g=np.ascontiguousarray(n2g),
                  fing=fing, w_in_ext=w_in_ext, sinkb=sinkb, w_fourier=f(w_fourier), w_attn=f(w_attn), w_out=f(w_out),
                  w_gate_d=f(w_gate_d)[0], w_up_d=f(w_up_d)[0], w_down_d=f(w_down_d)[0], w_router=f(w_router)[0],
                  w_gate_e=f(w_gate_e)[0], w_up_e=f(w_up_e)[0], w_down_e=f(w_down_e)[0],
                  ident=K["ident"], ones1024=K["ones1024"], tabcc=K["tabcc"], tabsc=K["tabsc"],
                  ccsc=K["ccsc"].astype(NPBF), esel=K["esel"])
    in_maps = []
    zeros = np.zeros((128, 512), np.float32)
    for i in range(8):
        b, j = i // 4, i % 4
        m = dict(shared)
        m["x"] = np.ascontiguousarray(x[b, j * NT:(j + 1) * NT])
        m["ctx"] = np.ascontiguousarray(ctx[b])
        cv = np.stack([_fm(c[b], 8), _fm(c_ctx, 8)], axis=2).reshape(128, 16)
        m["cvec"] = np.ascontiguousarray(cv)
        m["ropecos"] = K["cos%d" % j]
        m["ropesin"] = K["sin%d" % j]
        m["masks"] = np.concatenate([K["mprev"], K["mnext"], K["mprev"] if j > 0 else zeros, K["mnext"] if j < 3 else zeros], axis=1).astype(NPBF)
        sl = np.zeros((128, 8), np.float32)
        if j > 0:
            sl[:, j - 1] = 1.0
        if j < 3:
            sl[:, 4 + j + 1] = 1.0
        m["sel"] = sl
        m["tab23"] = K["tab23_%d" % j]
        m["c128"] = K["c128"]
        in_maps.append(m)

    if "nc" not in _CACHE:
        nc0 = bass.Bass("TRN2", target_bir_lowering=False)
        P0 = Prog(nc0)
        build(nc0, P0)
        nc = bass.Bass("TRN2", target_bir_lowering=False)
        P1 = Prog(nc, sigset=P0.need)
        build(nc, P1)
        _CACHE["nc"] = nc
    nc = _CACHE["nc"]
    in_maps = [{k: v for k, v in m.items() if k in nc._mk_declared} for m in in_maps]
    res = run_bass_kernel_spmd(nc, in_maps, core_ids=list(range(8)))
    out = np.zeros((2, T, D), np.float32)
    for i in range(8):
        b, j = i // 4, i % 4
        out[b, j * NT:(j + 1) * NT] = np.asarray(res.results[i]["out"], dtype=np.float32)
    return out
```

```python
import os
from contextlib import ExitStack
import numpy as np
import ml_dtypes
import concourse.bass as bass
import concourse.mybir as mybir
from concourse.bass_utils import run_bass_kernel_spmd

F32 = mybir.dt.float32
BF16 = mybir.dt.bfloat16
AF = mybir.ActivationFunctionType
ALU = mybir.AluOpType
AX = mybir.AxisListType
NPBF = ml_dtypes.bfloat16

D = 1024
T = 8192
NT = 2048
CT = 256
NCEXT = 4224
O_U, O_K, O_V, O_Q, O_G = 0, 512, 1024, 1152, 2176
DFF = 2816
DFE = 3584
NDMASEM = 8
STOP = os.environ.get("MK_STOP", "")


class Buf:
    __slots__ = ("name", "w", "r", "phase")

    def __init__(self, name, phase=False):
        self.name = name
        self.w = None
        self.r = {}
        self.phase = phase


class Prog:
    COMPUTE = ("pe", "act", "dve", "pool")

    def __init__(self, nc, sigset=None):
        self.nc = nc
        self.dry = sigset is None
        self.sigset = sigset if sigset is not None else set()
        self.need = set()
        self.n = 0
        self.meta = []
        self.cnt = {e: 0 for e in self.COMPUTE}
        self.sigval = {}
        self.waited = {}
        self.dma_no = {"sp": 0, "pool": 0, "act": 0}
        self.PH = Buf("PHASE")
        self.eng = {"pe": nc.tensor, "act": nc.scalar, "dve": nc.vector, "pool": nc.gpsimd, "sp": nc.sync}
        self.sems = {}
        self.ncc = 0

    def alloc_sems(self, stack):
        for e in self.COMPUTE:
            self.sems[e] = stack.enter_context(self.nc.semaphore("s_" + e))
        for q in ("sp", "pool"):
            for i in range(NDMASEM):
                self.sems[(q, i)] = stack.enter_context(self.nc.semaphore("d_%s%d" % (q, i)))
        for i in range(8):
            self.sems[("cc", i)] = stack.enter_context(self.nc.semaphore("cc%d" % i))

    def _wait(self, eng, semkey, val):
        k = (eng, semkey)
        if self.waited.get(k, 0) >= val:
            return
        self.waited[k] = val
        self.eng[eng].wait_ge(self.sems[semkey], val)

    skip = False
    limit = int(os.environ.get("MK_LIMIT", "0")) or None
    final = False

    def op(self, eng, fn, reads=(), writes=(), dma=False, cc=False):
        if self.skip:
            return None
        if self.limit is not None and self.n >= self.limit and not self.final:
            return None
        if os.environ.get("MK_TRACE") and self.dry:
            import inspect
            fr = inspect.currentframe().f_back
            print("OP", self.n, eng, fr.f_lineno, "dma" if dma else ("cc" if cc else ""))
        idx = self.n
        self.n += 1
        reads = list(reads)
        writes = list(writes)
        if any(b.phase for b in reads) or any(b.phase for b in writes):
            if self.PH not in writes:
                reads.append(self.PH)
        deps = set()
        for b in reads:
            if b.w is not None:
                deps.add(b.w)
        for b in writes:
            if b.w is not None:
                deps.add(b.w)
            deps.update(b.r.values())
        deps.discard(idx)
        async_op = dma or cc
        rkey = ("a", idx) if async_op else eng
        for b in writes:
            b.w = idx
            b.r = {}
        for b in reads:
            if b not in writes:
                b.r[rkey] = idx
        self.meta.append((eng, async_op))
        real = []
        for dpt in deps:
            deng, dasync = self.meta[dpt]
            if (not dasync) and deng == eng and (not async_op) and eng == "pe":
                continue
            real.append(dpt)
        if self.dry:
            for dpt in real:
                self.need.add(dpt)
            return idx
        for dpt in sorted(real):
            semkey, val = self.sigval[dpt]
            self._wait(eng, semkey, val)
        if dma:
            m = self.dma_no[eng]
            self.dma_no[eng] = m + 1
            semkey = (eng, m % NDMASEM)
            if m >= NDMASEM:
                self._wait(eng, semkey, 16 * (m // NDMASEM))
            ins = fn()
            ins.then_inc(self.sems[semkey], 16)
            self.sigval[idx] = (semkey, 16 * (m // NDMASEM + 1))
        elif cc:
            semkey = ("cc", self.ncc)
            self.ncc += 1
            ins = fn()
            ins.then_inc(self.sems[semkey])
            self.sigval[idx] = (semkey, 1)
        else:
            ins = fn()
            if idx in self.sigset:
                self.cnt[eng] += 1
                ins.then_inc(self.sems[eng], 1)
                self.sigval[idx] = (eng, self.cnt[eng])
        return idx

    def barrier(self):
        self.op("dve", lambda: self.nc.vector.engine_nop(), writes=[self.PH])


def build(nc, P):
    declared = set()
    nc._mk_declared = declared

    def din(name, shape, dt=F32):
        declared.add(name)
        return nc.dram_tensor(name, list(shape), dt, kind="ExternalInput")

    x_d = din("x", [NT, D])
    ctx_d = din("ctx", [CT, D])
    cvec_d = din("cvec", [128, 16])
    wmod_d = din("w_mod", [2, D, 6 * D])
    bmod_d = din("bmod", [128, 2 * 96])
    n1g_d = din("n1g", [128, 32])
    n2g_d = din("n2g", [128, 32])
    fing_d = din("fing", [128, 8])
    win_d = din("w_in_ext", [2, D, NCEXT])
    sink_d = din("sinkb", [128, 16])
    wf_d = din("w_fourier", [2, 512, D])
    wa_d = din("w_attn", [2, 512, D])
    wo_d = din("w_out", [2, D, D])
    need_d = STOP in ("", "ffn0", "p1_1", "pa_1", "pb0_1", "mix1", "ffn1")
    need_e = STOP in ("", "ffn1")
    wgd_d = wud_d = wdd_d = wr_d = wge_d = wue_d = wde_d = None
    if need_d:
        wgd_d = din("w_gate_d", [D, DFF])
        wud_d = din("w_up_d", [D, DFF])
        wdd_d = din("w_down_d", [DFF, D])
    if need_e:
        wr_d = din("w_router", [D, 8])
        wge_d = din("w_gate_e", [8, D, DFE])
        wue_d = din("w_up_e", [8, D, DFE])
        wde_d = din("w_down_e", [8, DFE, D])
    ident_d = din("ident", [128, 128])
    ones_d = din("ones1024", [128, 128], BF16)
    cos_d = din("ropecos", [128, NT], BF16)
    sin_d = din("ropesin", [128, NT], BF16)
    masks_d = din("masks", [128, 4 * 512], BF16)
    sel_d = din("sel", [128, 8])
    c128_d = din("c128", [128, 256], BF16)
    tab23_d = din("tab23", [128, 128 * 32], BF16)
    s1_d = nc.dram_tensor("s1_scratch", [2, 128, 64, 512], BF16)
    B_s1 = Buf("s1")
    tabcc_d = din("tabcc", [CT, CT], BF16)
    tabsc_d = din("tabsc", [CT, CT], BF16)
    ccsc_d = din("ccsc", [128, 256], BF16)
    esel_d = din("esel", [8, 8 * 128], BF16)
    out_d = nc.dram_tensor("out", [NT, D], F32, kind="ExternalOutput")

    u_own = [nc.dram_tensor("u_own%d" % k, [1024, 512], BF16) for k in range(2)]
    u_all = [nc.dram_tensor("u_all%d" % k, [4096, 512], BF16) for k in range(2)]
    u_ctx = nc.dram_tensor("u_ctx", [CT, 512], BF16)
    halo_in = nc.dram_tensor("halo_in", [128, 832], BF16)
    halo_all = nc.dram_tensor("halo_all", [512, 832], BF16)
    B_uown, B_uall, B_uctx, B_hin, B_hall = Buf("uown"), Buf("uall"), Buf("uctx"), Buf("hin"), Buf("hall")

    with ExitStack() as st:
        block = st.enter_context(nc.Block())
        P.alloc_sems(st)

        sbn = [0]

        def SB(stack, name, shape, dt, phase=True):
            sbn[0] += 1
            return stack.enter_context(nc.sbuf_tensor("sb%d_%s" % (sbn[0], name), shape, dt)), Buf(name, phase)

        ps = [st.enter_context(nc.psum_tensor("ps%d" % i, [128, 512], F32)) for i in range(8)]
        psB = [Buf("ps%d" % i) for i in range(8)]
        psrr = [0]

        pslist = [list(range(8))]

        def nps():
            i = pslist[0][psrr[0] % len(pslist[0])]
            psrr[0] += 1
            return i

        XT, _ = SB(st, "XT", [128, 8, NT], F32, False)
        XB = {(c, g): Buf("XT%d_%d" % (c, g)) for c in range(8) for g in range(4)}
        XcT, _ = SB(st, "XcT", [128, 8, CT], F32, False)
        XcB = {(c, 0): Buf("XcT%d" % c) for c in range(8)}
        mod, _ = SB(st, "mod", [128, 2, 48, 2], F32, False)
        g1m, _ = SB(st, "g1m", [128, 2, 8, 2], F32, False)
        g2m, _ = SB(st, "g2m", [128, 2, 8, 2], F32, False)
        B_modl = [Buf("mod0"), Buf("mod1")]
        B_g1l = [Buf("g1m0"), Buf("g1m1")]
        B_g2l = [Buf("g2m0"), Buf("g2m1")]
        ident, B_ident = SB(st, "ident", [128, 128], F32, False)
        ones, B_ones = SB(st, "ones", [128, 128], BF16, False)
        epsb, B_eps = SB(st, "epsb", [128, 1], F32, False)
        esink, B_esink = SB(st, "esink", [128, 16], F32, False)
        fing, B_fing = SB(st, "fing", [128, 8], F32, False)
        masks, B_masks = SB(st, "masks", [128, 4, 512], BF16, False)
        sel, B_sel = SB(st, "sel", [128, 8], F32, False)
        ccsc, B_ccsc = SB(st, "ccsc", [128, 256], BF16, False)
        c128, B_c128 = SB(st, "c128", [128, 256], BF16, False)

        cv, B_cv = SB(st, "cv", [128, 16], F32, False)
        scb, B_scb = SB(st, "scb", [128, 8, 2], BF16, False)
        bm, B_bm = SB(st, "bm", [128, 2, 96], F32, False)
        ng1, B_ng1 = SB(st, "ng1", [128, 32], F32, False)
        ng2, B_ng2 = SB(st, "ng2", [128, 32], F32, False)
        tmpm, B_tmpm = SB(st, "tmpm", [128, 16], F32, False)
        LAT = dict(name="lat", res=XT, rb=XB, n=NT, s=0, tgs=[(g, g * 512, 512) for g in range(4)])
        CTX = dict(name="ctx", res=XcT, rb=XcB, n=CT, s=1, tgs=[(0, 0, CT)])

        def mm(o, lhsT, rhs, start, stop, reads, pw):
            P.op("pe", lambda: nc.tensor.matmul(o(), lhsT=lhsT(), rhs=rhs(), start=start, stop=stop),
                 reads=reads, writes=[pw])

        def stop_pt(tag):
            if STOP == tag:
                P.skip = True

        @block.sync
        def _(sync):
            def ld(dst, B, src):
                P.op("sp", lambda: nc.sync.dma_start(out=dst(), in_=src()), writes=[B], dma=True)
            ld(lambda: ident[:], B_ident, lambda: ident_d[:, :])
            ld(lambda: ones[:], B_ones, lambda: ones_d[:, :])
            ld(lambda: esink[:], B_esink, lambda: sink_d[:, :])
            ld(lambda: fing[:], B_fing, lambda: fing_d[:, :])
            ld(lambda: masks[:], B_masks, lambda: masks_d.ap().rearrange("p (m n) -> p m n", m=4))
            ld(lambda: sel[:], B_sel, lambda: sel_d[:, :])
            ld(lambda: ccsc[:], B_ccsc, lambda: ccsc_d[:, :])
            ld(lambda: c128[:], B_c128, lambda: c128_d[:, :])
            P.op("dve", lambda: nc.vector.memset(epsb[:], 1e-6), writes=[B_eps])
            P.op("act", lambda: nc.scalar.activation(out=esink[:], in_=esink[:], func=AF.Exp),
                 reads=[B_esink], writes=[B_esink])

            def mods_piece(l, piece, wmb, pi):
                wt, wb = wmb
                P.op("pool", lambda: nc.gpsimd.dma_start(
                    out=wt[:], in_=wmod_d[l, :, piece * 512:(piece + 1) * 512].rearrange("(kc p) n -> p kc n", p=128)),
                    writes=[wb], dma=True)
                for sub in range(4):
                    ch = piece * 4 + sub
                    for kc in range(8):
                        mm(lambda ch=ch: ps[pi][:, ch * 2:ch * 2 + 2],
                           lambda kc=kc, sub=sub: wt[:, kc, sub * 128:(sub + 1) * 128],
                           lambda kc=kc: scb[:, kc, :], kc == 0, kc == 7, [wb, B_scb], psB[pi])

            def mods_fin(l, pi):
                P.op("dve", lambda: nc.vector.tensor_tensor(
                    out=mod[:, l].rearrange("p c s -> p (c s)"), in0=ps[pi][:, 0:96], in1=bm[:, l, :], op=ALU.add),
                    reads=[psB[pi], B_bm], writes=[B_modl[l]])
                for (gm, Bg, ng, Bn, o) in ((g1m, B_g1l, ng1, B_ng1, 8), (g2m, B_g2l, ng2, B_ng2, 32)):
                    P.op("dve", lambda o=o: nc.vector.tensor_scalar(
                        out=tmpm[:], in0=mod[:, l, o:o + 8, :].rearrange("p c s -> p (c s)"), scalar1=1.0, scalar2=None, op0=ALU.add),
                        reads=[B_modl[l]], writes=[B_tmpm])
                    P.op("dve", lambda gm=gm, ng=ng: nc.vector.tensor_tensor(
                        out=gm[:, l].rearrange("p c s -> p (c s)"), in0=tmpm[:], in1=ng[:, l * 16:(l + 1) * 16], op=ALU.mult),
                        reads=[B_tmpm, Bn], writes=[Bg[l]])

            with ExitStack() as ph:
                xs = [SB(ph, "xs%d" % i, [128, D], F32) for i in range(2)]

                def load_T(src, nblk, res, rb):
                    for blk in range(nblk):
                        xt, xb = xs[blk % 2]
                        P.op("sp", lambda xt=xt, blk=blk: nc.sync.dma_start(out=xt[:], in_=src[blk * 128:(blk + 1) * 128, :]),
                             writes=[xb], dma=True)
                        for half in range(2):
                            pi = nps()
                            for c in range(4):
                                cc = half * 4 + c
                                P.op("pe", lambda xt=xt, pi=pi, c=c, cc=cc: nc.tensor.transpose(
                                    out=ps[pi][:, c * 128:(c + 1) * 128], in_=xt[:, cc * 128:(cc + 1) * 128], identity=ident[:]),
                                    reads=[xb, B_ident], writes=[psB[pi]])
                            g = (blk * 128) // 512
                            P.op("dve", lambda pi=pi, half=half, blk=blk: nc.vector.tensor_copy(
                                out=res[:, half * 4:(half + 1) * 4, blk * 128:(blk + 1) * 128],
                                in_=ps[pi][:].rearrange("p (c t) -> p c t", c=4)),
                                reads=[psB[pi]], writes=[rb[(half * 4 + c, g)] for c in range(4)])
                load_T(x_d, 16, XT, XB)
                load_T(ctx_d, 2, XcT, XcB)
                stop_pt("load")

                wm = [SB(ph, "wm%d" % i, [128, 8, 512], BF16) for i in range(2)]
                ld(lambda: cv[:], B_cv, lambda: cvec_d[:, :])
                ld(lambda: bm[:], B_bm, lambda: bmod_d.ap().rearrange("p (l n) -> p l n", l=2))
                ld(lambda: ng1[:], B_ng1, lambda: n1g_d[:, :])
                ld(lambda: ng2[:], B_ng2, lambda: n2g_d[:, :])
                P.op("act", lambda: nc.scalar.activation(out=cv[:], in_=cv[:], func=AF.Silu), reads=[B_cv], writes=[B_cv])
                P.op("dve", lambda: nc.vector.tensor_copy(out=scb[:], in_=cv[:].rearrange("p (k s) -> p k s", s=2)),
                     reads=[B_cv], writes=[B_scb])
                mpi0 = nps()
                for piece in range(12):
                    mods_piece(0, piece, wm[piece % 2], mpi0)
                mods_fin(0, mpi0)
                P.barrier()
                stop_pt("mods")

            def norm_tile(ph_bufs, ts, g, off, n, gm_ap, sh_ap, hT, hB, extra_reads, hout=None, sqb=None):
                _sq, _B_sq, rstd, B_rstd, tt = ph_bufs
                if hout is None:
                    hout = lambda c: hT[:, c, :n]
                sq, B_sq = sqb if sqb is not None else (hT, hB)
                res, rb = ts["res"], ts["rb"]
                for c in range(8):
                    P.op("act", lambda c=c: nc.scalar.activation(out=sq[:, c, :n], in_=res[:, c, off:off + n], func=AF.Square),
                         reads=[rb[(c, g)]], writes=[B_sq])
                pi = nps()
                for c in range(8):
                    mm(lambda: ps[pi][:, :n], lambda: ones[:], lambda c=c: sq[:, c, :n], c == 0, c == 7, [B_ones, B_sq], psB[pi])
                P.op("act", lambda: nc.scalar.activation(out=rstd[:, :n], in_=ps[pi][:, :n], func=AF.Sqrt, bias=epsb[:, 0:1], scale=1.0),
                     reads=[psB[pi], B_eps], writes=[B_rstd])
                P.op("dve", lambda: nc.vector.reciprocal(out=rstd[:, :n], in_=rstd[:, :n]), reads=[B_rstd], writes=[B_rstd])
                for c in range(8):
                    t, tb = tt[c % 2]
                    if sh_ap is None:
                        P.op("dve", lambda c=c: nc.vector.scalar_tensor_tensor(
                            out=hout(c), in0=res[:, c, off:off + n], scalar=gm_ap(c), in1=rstd[:, :n], op0=ALU.mult, op1=ALU.mult),
                            reads=[rb[(c, g)], B_rstd] + extra_reads, writes=[hB])
                    else:
                        P.op("dve", lambda c=c, t=t: nc.vector.scalar_tensor_tensor(
                            out=t[:, :n], in0=res[:, c, off:off + n], scalar=gm_ap(c), in1=rstd[:, :n], op0=ALU.mult, op1=ALU.mult),
                            reads=[rb[(c, g)], B_rstd] + extra_reads, writes=[tb])
                        P.op("act", lambda c=c, t=t: nc.scalar.activation(
                            out=hout(c), in_=t[:, :n], func=AF.Identity, bias=sh_ap(c), scale=1.0),
                            reads=[tb] + extra_reads, writes=[hB])

            def norm_bufs(ph):
                sq, B_sq = None, None
                rstd, B_rstd = SB(ph, "rstd", [128, 512], F32)
                tt = [SB(ph, "nt%d" % i, [128, 512], F32) for i in range(2)]
                return (sq, B_sq, rstd, B_rstd, tt)

            def wload(wt, wb, src):
                P.op("pool", lambda: nc.gpsimd.dma_start(out=wt(), in_=src()), writes=[wb], dma=True)

            for l in range(2):
                last = (l == 1)
                with ExitStack() as lay:
                    OT, B_OT = SB(lay, "OT", [128, 4, NT], BF16)
                    OcT, B_OcT = SB(lay, "OcT", [128, 4, CT], BF16)
                    FT, B_FT = SB(lay, "FT", [128, 4, NT], BF16)
                    FcT, B_FcT = SB(lay, "FcT", [128, 4, CT], BF16)
                    att = ExitStack()
                    KT, B_KT = SB(att, "KT", [128, 2, NT], BF16)
                    Vown, B_V = SB(att, "Vown", [128, 16, 2, 80], BF16)
                    KcT, B_KcT = SB(att, "KcT", [128, 2, CT], BF16)
                    Vc, B_Vc = SB(att, "Vc", [128, 2, 2, 80], BF16)
                    hp, B_hp = SB(att, "hp", [128, 832], BF16)
                    hn, B_hn = SB(att, "hn", [128, 832], BF16)
                    P.op("pool", lambda: nc.gpsimd.memset(KT[:], 0.0), writes=[B_KT])
                    P.op("pool", lambda: nc.gpsimd.memset(Vown[:], 1.0), writes=[B_V])
                    P.op("pool", lambda: nc.gpsimd.memset(Vc[:], 1.0), writes=[B_Vc])

                    def mods(ts, base):
                        s = ts["s"]
                        return lambda c: mod[:, l, base + c, s:s + 1]

                    with ExitStack() as ph:
                        nb = norm_bufs(ph)
                        hTs = [SB(ph, "hT%d" % i, [128, 8, 512], BF16) for i in range(2)]
                        hq = [0]
                        wu, B_wu = SB(ph, "wu", [128, 8, 512], BF16)
                        wk, B_wk = SB(ph, "wk", [128, 8, 512], BF16)
                        wv, B_wv = SB(ph, "wv", [128, 8, 128], BF16)
                        rc, B_rc = SB(ph, "rc", [128, NT], BF16)
                        rs, B_rs = SB(ph, "rs", [128, NT], BF16)
                        ust = [SB(ph, "ust%d" % i, [128, 512], BF16) for i in range(2)]
                        r1 = [SB(ph, "r1_%d" % i, [128, 512], F32) for i in range(2)]
                        r2 = [SB(ph, "r2_%d" % i, [128, 512], F32) for i in range(2)]
                        wsrc = lambda o, n: (lambda: win_d[l, :, o:o + n].rearrange("(kc p) n -> p kc n", p=128))
                        wload(lambda: wu[:], B_wu, wsrc(O_U, 512))
                        wload(lambda: wk[:], B_wk, wsrc(O_K, 512))
                        wload(lambda: wv[:], B_wv, wsrc(O_V, 128))
                        ld(lambda: rc[:], B_rc, lambda: cos_d[:, :])
                        ld(lambda: rs[:], B_rs, lambda: sin_d[:, :])
                        uq = [0]
                        for ts in ([CTX, LAT]):
                            isl = ts is LAT
                            for (g, off, n) in ts["tgs"]:
                                hT, B_hT = hTs[hq[0] % 2]
                                hq[0] += 1
                                norm_tile(nb, ts, g, off, n, lambda c, ts=ts: g1m[:, l, c, ts["s"]:ts["s"] + 1], mods(ts, 0), hT, B_hT, [B_g1l[l], B_modl[l]])
                                for b in range(n // 128):
                                    gb = off // 128 + b
                                    pi = nps()
                                    for kc in range(8):
                                        mm(lambda pi=pi: ps[pi][:, :], lambda kc=kc, b=b: hT[:, kc, b * 128:(b + 1) * 128],
                                           lambda kc=kc: wu[:, kc, :], kc == 0, kc == 7, [B_hT, B_wu], psB[pi])
                                    ut, ub = ust[uq[0] % 2]
                                    uq[0] += 1
                                    P.op("dve", lambda pi=pi, ut=ut: nc.vector.tensor_copy(out=ut[:], in_=ps[pi][:]), reads=[psB[pi]], writes=[ub])
                                    udst, uB = (u_own[gb // 8], B_uown) if isl else (u_ctx, B_uctx)
                                    gr = gb % 8 if isl else gb
                                    P.op("sp", lambda ut=ut, udst=udst, gr=gr: nc.sync.dma_start(out=udst[gr * 128:(gr + 1) * 128, :], in_=ut[:]),
                                         reads=[ub], writes=[uB], dma=True)
                                    pi = nps()
                                    for kc in range(8):
                                        mm(lambda pi=pi: ps[pi][:, 0:128], lambda kc=kc, b=b: hT[:, kc, b * 128:(b + 1) * 128],
                                           lambda kc=kc: wv[:, kc, :], kc == 0, kc == 7, [B_hT, B_wv], psB[pi])
                                    vdst, vB = (Vown, B_V) if isl else (Vc, B_Vc)
                                    P.op("dve", lambda pi=pi, vdst=vdst, gb=gb: nc.vector.tensor_copy(
                                        out=vdst[:, gb, :, 0:64], in_=ps[pi][:, 0:128].rearrange("p (h d) -> p h d", h=2)),
                                        reads=[psB[pi]], writes=[vB])
                                for h in range(2):
                                    pa = nps()
                                    for kc in range(8):
                                        mm(lambda pa=pa: ps[pa][0:64, :n], lambda kc=kc, h=h: wk[:, kc, h * 128:h * 128 + 64],
                                           lambda kc=kc: hT[:, kc, :n], kc == 0, kc == 7, [B_hT, B_wk], psB[pa])
                                    if not isl:
                                        P.op("dve", lambda pa=pa, h=h: nc.vector.tensor_copy(out=KcT[0:64, h, :], in_=ps[pa][0:64, :n]),
                                             reads=[psB[pa]], writes=[B_KcT])
                                        continue
                                    pb = nps()
                                    for kc in range(8):
                                        mm(lambda pb=pb: ps[pb][0:64, :n], lambda kc=kc, h=h: wk[:, kc, (2 + h) * 128:(2 + h) * 128 + 64],
                                           lambda kc=kc: hT[:, kc, :n], kc == 0, kc == 7, [B_hT, B_wk], psB[pb])
                                    t1, b1 = r1[h]
                                    t2, b2 = r2[h]
                                    P.op("dve", lambda pa=pa, t1=t1, off=off: nc.vector.tensor_tensor(out=t1[0:64, :], in0=ps[pa][0:64, :], in1=rc[0:64, off:off + 512], op=ALU.mult),
                                         reads=[psB[pa], B_rc], writes=[b1])
                                    P.op("dve", lambda pb=pb, t2=t2, off=off: nc.vector.tensor_tensor(out=t2[0:64, :], in0=ps[pb][0:64, :], in1=rs[0:64, off:off + 512], op=ALU.mult),
                                         reads=[psB[pb], B_rs], writes=[b2])
                                    P.op("pool", lambda t1=t1, t2=t2, h=h, off=off: nc.gpsimd.tensor_tensor(out=KT[0:64, h, off:off + 512], in0=t1[0:64, :], in1=t2[0:64, :], op=ALU.add),
                                         reads=[b1, b2], writes=[B_KT])
                        stop_pt("p1b")
                        P.op("sp", lambda: nc.sync.dma_start(out=halo_in[:, 0:256].rearrange("p (h t) -> p h t", h=2), in_=KT[:, :, 0:128]),
                             reads=[B_KT], writes=[B_hin], dma=True)
                        P.op("sp", lambda: nc.sync.dma_start(out=halo_in[:, 256:512].rearrange("p (h t) -> p h t", h=2), in_=KT[:, :, NT - 128:NT]),
                             reads=[B_KT], writes=[B_hin], dma=True)
                        P.op("sp", lambda: nc.sync.dma_start(out=halo_in[:, 512:672], in_=Vown[:, 0].rearrange("p h e -> p (h e)")),
                             reads=[B_V], writes=[B_hin], dma=True)
                        P.op("sp", lambda: nc.sync.dma_start(out=halo_in[:, 672:832], in_=Vown[:, 15].rearrange("p h e -> p (h e)")),
                             reads=[B_V], writes=[B_hin], dma=True)
                        stop_pt("p1c")
                        P.op("pool", lambda: nc.gpsimd.collective_compute("AllGather", ALU.bypass, replica_groups=[[0, 1, 2, 3], [4, 5, 6, 7]],
                                                                          ins=[halo_in.ap().opt()], outs=[halo_all.ap().opt()]),
                             reads=[B_hin], writes=[B_hall], cc=True)
                        for k2 in range(2):
                            P.op("pool", lambda k2=k2: nc.gpsimd.collective_compute("AllGather", ALU.bypass, replica_groups=[[0, 1, 2, 3], [4, 5, 6, 7]],
                                                                                    ins=[u_own[k2].ap().opt()], outs=[u_all[k2].ap().opt()]),
                                 reads=[B_uown], writes=[B_uall], cc=True)
                        stop_pt("p1d")
                        hall, B_hl = SB(ph, "hall", [128, 4, 832], BF16)
                        P.op("sp", lambda: nc.sync.dma_start(out=hall[:], in_=halo_all.ap().rearrange("(r p) n -> p r n", p=128)),
                             reads=[B_hall], writes=[B_hl], dma=True)
                        for (dst, Bd, so) in ((hp, B_hp, 0), (hn, B_hn, 4)):
                            P.op("dve", lambda dst=dst, so=so: nc.vector.tensor_scalar(out=dst[:, 0:832], in0=hall[:, 0, 0:832], scalar1=sel[:, so:so + 1], scalar2=None, op0=ALU.mult),
                                 reads=[B_hl, B_sel], writes=[Bd])
                            for r in range(1, 4):
                                P.op("dve", lambda dst=dst, so=so, r=r: nc.vector.scalar_tensor_tensor(
                                    out=dst[:, 0:832], in0=hall[:, r, 0:832], scalar=sel[:, so + r:so + r + 1], in1=dst[:, 0:832], op0=ALU.mult, op1=ALU.add),
                                    reads=[B_hl, B_sel], writes=[Bd])
                        P.barrier()
                        stop_pt("p1_%d" % l)

                    with ExitStack() as ph:
                        nb = norm_bufs(ph)
                        hT, B_hT = SB(ph, "hT", [128, 8, 512], BF16)
                        wq, B_wq = SB(ph, "wq", [128, 8, 1024], BF16)
                        rc, B_rc = SB(ph, "rc", [128, NT], BF16)
                        rs, B_rs = SB(ph, "rs", [128, NT], BF16)
                        QT, B_QT = SB(ph, "QT", [128, 8, 512], BF16)
                        r1 = [SB(ph, "r1_%d" % i, [128, 512], F32) for i in range(2)]
                        r2 = [SB(ph, "r2_%d" % i, [128, 512], F32) for i in range(2)]
                        pt = [SB(ph, "pt%d" % i, [128, 512], BF16) for i in range(6)]
                        pslist[0] = [0, 1, 2, 3]
                        qcount = [0]
                        Ons = [SB(ph, "On%d" % i, [128, 512], F32) for i in range(2)]
                        pendT = []
                        den, B_den = SB(ph, "den", [128, 8], F32)
                        wload(lambda: wq[:], B_wq, lambda: win_d[l, :, O_Q:O_Q + 1024].rearrange("(kc p) n -> p kc n", p=128))
                        ld(lambda: rc[:], B_rc, lambda: cos_d[:, :])
                        ld(lambda: rs[:], B_rs, lambda: sin_d[:, :])
                        eq = [0]
                        for ts in ([LAT] if last else [CTX, LAT]):
                            isl = ts is LAT
                            for (g, off, n) in ts["tgs"]:
                                norm_tile(nb, ts, g, off, n, lambda c, ts=ts: g1m[:, l, c, ts["s"]:ts["s"] + 1], mods(ts, 0), hT, B_hT, [B_g1l[l], B_modl[l]])
                                for hd in range(8):
                                    pa = nps()
                                    for kc in range(8):
                                        mm(lambda pa=pa: ps[pa][0:64, :n], lambda kc=kc, hd=hd: wq[:, kc, hd * 64:(hd + 1) * 64],
                                           lambda kc=kc: hT[:, kc, :n], kc == 0, kc == 7, [B_hT, B_wq], psB[pa])
                                    if not isl:
                                        P.op("dve", lambda pa=pa, hd=hd: nc.vector.tensor_copy(out=QT[0:64, hd, :n], in_=ps[pa][0:64, :n]),
                                             reads=[psB[pa]], writes=[B_QT])
                                        continue
                                    pb = nps()
                                    for kc in range(8):
                                        mm(lambda pb=pb: ps[pb][0:64, :n], lambda kc=kc, hd=hd: wq[:, kc, 512 + hd * 64:512 + (hd + 1) * 64],
                                           lambda kc=kc: hT[:, kc, :n], kc == 0, kc == 7, [B_hT, B_wq], psB[pb])
                                    t1, b1 = r1[hd % 2]
                                    t2, b2 = r2[hd % 2]
                                    P.op("dve", lambda pa=pa, t1=t1, off=off: nc.vector.tensor_tensor(out=t1[0:64, :], in0=ps[pa][0:64, :], in1=rc[0:64, off:off + 512], op=ALU.mult),
                                         reads=[psB[pa], B_rc], writes=[b1])
                                    P.op("dve", lambda pb=pb, t2=t2, off=off: nc.vector.tensor_tensor(out=t2[0:64, :], in0=ps[pb][0:64, :], in1=rs[0:64, off:off + 512], op=ALU.mult),
                                         reads=[psB[pb], B_rs], writes=[b2])
                                    P.op("pool", lambda t1=t1, t2=t2, hd=hd: nc.gpsimd.tensor_tensor(out=QT[0:64, hd, :], in0=t1[0:64, :], in1=t2[0:64, :], op=ALU.add),
                                         reads=[b1, b2], writes=[B_QT])
                                for qb in range(n // 128):
                                    gb = off // 128 + qb
                                    po2 = [4 + (qcount[0] % 2) * 2 + h for h in range(2)]
                                    On, B_On = Ons[qcount[0] % 2]
                                    qcount[0] += 1
                                    keysets = []
                                    for h in range(2):
                                        keys = []
                                        if isl:
                                            if gb == 0:
                                                keys.append((lambda h=h: hp[0:64, 256 + h * 128:256 + (h + 1) * 128],
                                                             lambda h=h: hp[:, 672 + h * 80:672 + h * 80 + 65], 2, [B_hp]))
                                            else:
                                                keys.append((lambda h=h, gb=gb: KT[0:64, h, (gb - 1) * 128:gb * 128],
                                                             lambda h=h, gb=gb: Vown[:, gb - 1, h, 0:65], 0, [B_KT, B_V]))
                                            keys.append((lambda h=h, gb=gb: KT[0:64, h, gb * 128:(gb + 1) * 128],
                                                         lambda h=h, gb=gb: Vown[:, gb, h, 0:65], None, [B_KT, B_V]))
                                            if gb == 15:
                                                keys.append((lambda h=h: hn[0:64, h * 128:(h + 1) * 128],
                                                             lambda h=h: hn[:, 512 + h * 80:512 + h * 80 + 65], 3, [B_hn]))
                                            else:
                                                keys.append((lambda h=h, gb=gb: KT[0:64, h, (gb + 1) * 128:(gb + 2) * 128],
                                                             lambda h=h, gb=gb: Vown[:, gb + 1, h, 0:65], 1, [B_KT, B_V]))
                                        for cb in range(2):
                                            keys.append((lambda h=h, cb=cb: KcT[0:64, h, cb * 128:(cb + 1) * 128],
                                                         lambda h=h, cb=cb: Vc[:, cb, h, 0:65], None, [B_KcT, B_Vc]))
                                        keysets.append(keys)
                                    nk = len(keysets[0])
                                    jobs = [(h, ki) for ki in range(nk) for h in range(2)]
                                    LAG = 3
                                    pend = []
                                    for s_ in range(len(jobs) + LAG):
                                        if s_ < len(jobs):
                                            h, ki = jobs[s_]
                                            kap, vap, mk, kr = keysets[h][ki]
                                            pi = nps()
                                            mm(lambda pi=pi: ps[pi][:, :], lambda kap=kap: kap(),
                                               lambda h=h, qb=qb: QT[0:64, 4 * h:4 * h + 4, qb * 128:(qb + 1) * 128],
                                               True, True, [B_QT] + kr, psB[pi])
                                            ptt, pb_ = pt[eq[0] % len(pt)]
                                            eq[0] += 1
                                            P.op("act", lambda pi=pi, ptt=ptt: nc.scalar.activation(out=ptt[:], in_=ps[pi][:], func=AF.Exp, scale=0.125),
                                                 reads=[psB[pi]], writes=[pb_])
                                            if mk is not None:
                                                P.op("pool", lambda ptt=ptt, mk=mk: nc.gpsimd.tensor_tensor(out=ptt[:], in0=ptt[:], in1=masks[:, mk, :], op=ALU.mult),
                                                     reads=[pb_, B_masks], writes=[pb_])
                                            pend.append((h, ki, ptt, pb_, vap, kr))
                                        if s_ == min(8, len(jobs) - 1) and pendT:
                                            pendT.pop(0)()
                                        if s_ >= LAG:
                                            h, ki, ptt, pb_, vap, kr = pend[s_ - LAG]
                                            po = po2[h]
                                            for gi in range(4):
                                                col = gi * 128
                                                mm(lambda po=po, gi=gi: ps[po][:, gi * 80:gi * 80 + 65], lambda ptt=ptt, col=col: ptt[:, col:col + 128],
                                                   lambda vap=vap: vap(), ki == 0, ki == nk - 1, [pb_] + kr, psB[po])
                                    for h in range(2):
                                        po = po2[h]
                                        P.op("dve", lambda po=po, h=h: nc.vector.tensor_tensor(
                                            out=den[:, 4 * h:4 * h + 4], in0=ps[po][:, 0:320].rearrange("p (g e) -> p g e", e=80)[:, :, 64],
                                            in1=esink[:, l * 8 + 4 * h:l * 8 + 4 * h + 4], op=ALU.add),
                                            reads=[psB[po], B_esink], writes=[B_den])
                                    P.op("dve", lambda: nc.vector.reciprocal(out=den[:], in_=den[:]), reads=[B_den], writes=[B_den])
                                    for h in range(2):
                                        po = po2[h]
                                        for gi in range(4):
                                            P.op("dve", lambda po=po, gi=gi, h=h: nc.vector.tensor_scalar(
                                                out=On[:, (4 * h + gi) * 64:(4 * h + gi + 1) * 64], in0=ps[po][:, gi * 80:gi * 80 + 64],
                                                scalar1=den[:, 4 * h + gi:4 * h + gi + 1], scalar2=None, op0=ALU.mult),
                                                reads=[psB[po], B_den], writes=[B_On])
                                    def emit_T(On=On, B_On=B_On, gb=gb, isl=isl):
                                        pi = nps()
                                        for c4 in range(4):
                                            P.op("pe", lambda pi=pi, c4=c4: nc.tensor.transpose(out=ps[pi][:, c4 * 128:(c4 + 1) * 128], in_=On[:, c4 * 128:(c4 + 1) * 128], identity=ident[:]),
                                                 reads=[B_On, B_ident], writes=[psB[pi]])
                                        odst, oB = (OT, B_OT) if isl else (OcT, B_OcT)
                                        P.op("dve", lambda pi=pi, odst=odst: nc.vector.tensor_copy(
                                            out=odst[:, :, gb * 128:(gb + 1) * 128], in_=ps[pi][:].rearrange("p (c t) -> p c t", c=4)),
                                            reads=[psB[pi]], writes=[oB])
                                    pendT.append(emit_T)
                                while pendT:
                                    pendT.pop(0)()
                        pslist[0] = list(range(8))
                        P.barrier()
                        stop_pt("pa_%d" % l)
                    att.close()

                    with ExitStack() as ph:
                        ub = [SB(ph, "ub%d" % i, [128, 512], BF16) for i in range(3)]
                        tcb = [SB(ph, "tcb%d" % i, [128, 512], BF16) for i in range(3)]
                        tsb = [SB(ph, "tsb%d" % i, [128, 512], BF16) for i in range(3)]
                        yri, B_yri = SB(ph, "yri", [128, 8, 512], BF16)
                        dq = [0]
                        for ts in ([] if last else [CTX]):
                            isl = ts is LAT
                            uB = B_uall if isl else B_uctx
                            tc_d, ts_d = (tabcc_d, tabsc_d)
                            nblk = 64 if isl else 2
                            for (g, off, n) in ts["tgs"]:
                                acc = [nps() for _ in range(8)]
                                for nbk in range(nblk):
                                    i3 = dq[0] % 3
                                    dq[0] += 1
                                    (ut, uBb), (ct, cB), (st_, sB_) = ub[i3], tcb[i3], tsb[i3]
                                    if not isl:
                                        usrc, urow = u_ctx, nbk * 128
                                        P.op("sp", lambda ut=ut, usrc=usrc, urow=urow: nc.sync.dma_start(out=ut[:], in_=usrc[urow:urow + 128, :]),
                                             reads=[uB], writes=[uBb], dma=True)
                                        uap = lambda gg, ut=ut: ut[:, gg * 128:(gg + 1) * 128]

                                    P.op("sp", lambda ct=ct, tc_d=tc_d, nbk=nbk, off=off, n=n: nc.sync.dma_start(out=ct[:, :n], in_=tc_d[nbk * 128:(nbk + 1) * 128, off:off + n]),
                                         writes=[cB], dma=True)
                                    P.op("sp", lambda st_=st_, ts_d=ts_d, nbk=nbk, off=off, n=n: nc.sync.dma_start(out=st_[:, :n], in_=ts_d[nbk * 128:(nbk + 1) * 128, off:off + n]),
                                         writes=[sB_], dma=True)
                                    for gg in range(4):
                                        mm(lambda gg=gg: ps[acc[gg]][:, :n], lambda uap=uap, gg=gg: uap(gg), lambda ct=ct: ct[:, :n],
                                           nbk == 0, nbk == nblk - 1, [uBb, cB], psB[acc[gg]])
                                        mm(lambda gg=gg: ps[acc[4 + gg]][:, :n], lambda uap=uap, gg=gg: uap(gg), lambda st_=st_: st_[:, :n],
                                           nbk == 0, nbk == nblk - 1, [uBb, sB_], psB[acc[4 + gg]])
                                for j in range(8):
                                    P.op("dve", lambda j=j: nc.vector.tensor_copy(out=yri[:, j, :n], in_=ps[acc[j]][:, :n]), reads=[psB[acc[j]]], writes=[B_yri])
                                fdst, fB = (FT, B_FT) if isl else (FcT, B_FcT)
                                for gg in range(4):
                                    pi = nps()
                                    mm(lambda pi=pi: ps[pi][:, :n], lambda: ccsc[:, 0:128], lambda gg=gg: yri[:, gg, :n], True, False, [B_ccsc, B_yri], psB[pi])
                                    mm(lambda pi=pi: ps[pi][:, :n], lambda: ccsc[:, 128:256], lambda gg=gg: yri[:, 4 + gg, :n], False, True, [B_ccsc, B_yri], psB[pi])
                                    P.op("dve", lambda pi=pi, fdst=fdst, gg=gg, off=off, n=n: nc.vector.tensor_copy(out=fdst[:, gg, off:off + n], in_=ps[pi][:, :n]),
                                         reads=[psB[pi]], writes=[fB])
                        P.barrier()
                        stop_pt("pb0c_%d" % l)
                    with ExitStack() as ph:
                        Tb_ = [SB(ph, "ctT%d" % i, [128, 8, 512], BF16) for i in range(2)]
                        st1 = [SB(ph, "st1_%d" % i, [128, 2, 8, 512], BF16) for i in range(2)]
                        def ct_loads(n2c):
                            Tt, TB = Tb_[n2c % 2]
                            for r_ in range(4):
                                for k2_ in range(2):
                                    P.op("sp", lambda Tt=Tt, r_=r_, k2_=k2_, n2c=n2c: nc.sync.dma_start(
                                        out=Tt[r_ * 32 + k2_ * 16:r_ * 32 + k2_ * 16 + 16, :, :],
                                        in_=u_all[k2_][r_ * 1024:(r_ + 1) * 1024, :].rearrange("(a n) c -> a n c", n=64)[:, n2c * 8:(n2c + 1) * 8, :]),
                                        reads=[B_uall], writes=[TB], dma=True)
                        ct_loads(0)
                        for n2c in range(8):
                            Tt, TB = Tb_[n2c % 2]
                            s1t, s1B = st1[n2c % 2]
                            if n2c + 1 < 8:
                                ct_loads(n2c + 1)
                            for n2i in range(8):
                                pr, pi_ = nps(), nps()
                                mm(lambda pr=pr: ps[pr][:, :], lambda: c128[:, 0:128], lambda Tt=Tt, n2i=n2i: Tt[:, n2i, :], True, True, [B_c128, TB], psB[pr])
                                mm(lambda pi_=pi_: ps[pi_][:, :], lambda: c128[:, 128:256], lambda Tt=Tt, n2i=n2i: Tt[:, n2i, :], True, True, [B_c128, TB], psB[pi_])
                                P.op("act", lambda pr=pr, s1t=s1t, n2i=n2i: nc.scalar.copy(out=s1t[:, 0, n2i, :], in_=ps[pr][:, :]), reads=[psB[pr]], writes=[s1B])
                                P.op("dve", lambda pi_=pi_, s1t=s1t, n2i=n2i: nc.vector.tensor_copy(out=s1t[:, 1, n2i, :], in_=ps[pi_][:, :]), reads=[psB[pi_]], writes=[s1B])
                            for ri in range(2):
                                P.op("sp", lambda s1t=s1t, ri=ri, n2c=n2c: nc.sync.dma_start(out=s1_d[ri, :, n2c * 8:(n2c + 1) * 8, :], in_=s1t[:, ri, :, :]),
                                     reads=[s1B], writes=[B_s1], dma=True)
                        P.barrier()
                        stop_pt("pb0s1_%d" % l)
                    with ExitStack() as ph:
                        Yb, B_Yb = SB(ph, "ctY", [128, 4, 2, NT], BF16)
                        Rb_ = [SB(ph, "ctR%d" % i, [128, 8, 512], BF16) for i in range(2)]
                        t23, B_t23 = SB(ph, "t23", [128, 128, 32], BF16)
                        ld(lambda: t23[:], B_t23, lambda: tab23_d.ap().rearrange("p (k c) -> p k c", c=32))
                        acc = None
                        for k1c in range(16):
                            Rt, RB = Rb_[k1c % 2]
                            for ri in range(2):
                                P.op("sp", lambda Rt=Rt, ri=ri, k1c=k1c: nc.sync.dma_start(
                                    out=Rt[ri * 64:(ri + 1) * 64, :, :], in_=s1_d[ri, k1c * 8:(k1c + 1) * 8, :, :].rearrange("k n c -> n k c")),
                                    reads=[B_s1], writes=[RB], dma=True)
                            if k1c % 2 == 0:
                                acc = [nps() for _ in range(4)]
                            for k1i in range(8):
                                k1 = k1c * 8 + k1i
                                for gg in range(4):
                                    mm(lambda gg=gg, k1=k1: ps[acc[gg]][:, (k1 % 16) * 32:(k1 % 16) * 32 + 32],
                                       lambda Rt=Rt, k1i=k1i, gg=gg: Rt[:, k1i, gg * 128:(gg + 1) * 128],
                                       lambda k1=k1: t23[:, k1, :], True, True, [RB, B_t23], psB[acc[gg]])
                            if k1c % 2 == 1:
                                kbase = (k1c - 1) * 8
                                for gg in range(4):
                                    for ro in range(2):
                                        src = lambda gg=gg, ro=ro: ps[acc[gg]][:, :].rearrange("p (a r b) -> p a r b", r=2, b=16)[:, :, ro, :]
                                        dst = lambda gg=gg, ro=ro, kbase=kbase: Yb[:, gg, ro, :].rearrange("p (b a) -> p a b", a=128)[:, kbase:kbase + 16, :]
                                        if gg % 2 == 0:
                                            P.op("act", lambda src=src, dst=dst: nc.scalar.copy(out=dst(), in_=src()), reads=[psB[acc[gg]]], writes=[B_Yb])
                                        else:
                                            P.op("dve", lambda src=src, dst=dst: nc.vector.tensor_copy(out=dst(), in_=src()), reads=[psB[acc[gg]]], writes=[B_Yb])
                        for tg_ in range(4):
                            for gg in range(4):
                                pi = nps()
                                mm(lambda pi=pi: ps[pi][:, :], lambda: ccsc[:, 0:128], lambda gg=gg, tg_=tg_: Yb[:, gg, 0, tg_ * 512:(tg_ + 1) * 512], True, False, [B_ccsc, B_Yb], psB[pi])
                                mm(lambda pi=pi: ps[pi][:, :], lambda: ccsc[:, 128:256], lambda gg=gg, tg_=tg_: Yb[:, gg, 1, tg_ * 512:(tg_ + 1) * 512], False, True, [B_ccsc, B_Yb], psB[pi])
                                P.op("dve", lambda pi=pi, gg=gg, tg_=tg_: nc.vector.tensor_copy(out=FT[:, gg, tg_ * 512:(tg_ + 1) * 512], in_=ps[pi][:, :]),
                                     reads=[psB[pi]], writes=[B_FT])
                        P.barrier()
                        stop_pt("pb0_%d" % l)

                    with ExitStack() as ph:
                        nb = norm_bufs(ph)
                        hT, B_hT = SB(ph, "hT", [128, 8, 256], BF16)
                        wg2, B_wg2 = SB(ph, "wg2", [128, 8, 2048], BF16)
                        wf, B_wf = SB(ph, "wf", [128, 4, D], BF16)
                        wa, B_wa = SB(ph, "wa", [128, 4, D], BF16)
                        wo, B_wo = SB(ph, "wo", [128, 8, D], BF16)
                        yT, B_yT = SB(ph, "yT", [128, 8, 256], BF16)
                        sg = [SB(ph, "sg%d" % i, [128, 256], F32) for i in range(4)]
                        tm = [SB(ph, "tm%d" % i, [128, 256], F32) for i in range(4)]
                        wload(lambda: wf[:], B_wf, lambda: wf_d[l].rearrange("(kc p) n -> p kc n", p=128))
                        wload(lambda: wa[:], B_wa, lambda: wa_d[l].rearrange("(kc p) n -> p kc n", p=128))
                        wload(lambda: wg2[:], B_wg2, lambda: win_d[l, :, O_G:O_G + 2048].rearrange("(kc p) n -> p kc n", p=128))
                        wload(lambda: wo[:], B_wo, lambda: wo_d[l].rearrange("(kc p) n -> p kc n", p=128))
                        for ts in ([LAT] if last else [CTX, LAT]):
                            isl = ts is LAT
                            fsrc, fB = (FT, B_FT) if isl else (FcT, B_FcT)
                            osrc, oB = (OT, B_OT) if isl else (OcT, B_OcT)
                            s = ts["s"]
                            for (g, off, n) in [(o_ // 512, o_, 256) for o_ in range(0, ts["n"], 256)]:
                                norm_tile(nb, ts, g, off, n, lambda c, s=s: g1m[:, l, c, s:s + 1], mods(ts, 0), hT, B_hT, [B_g1l[l], B_modl[l]])
                                for dc in range(8):
                                    pF, pA, pGf, pGa = nps(), nps(), nps(), nps()
                                    for gg in range(4):
                                        mm(lambda pF=pF: ps[pF][:, :n], lambda gg=gg, dc=dc: wf[:, gg, dc * 128:(dc + 1) * 128],
                                           lambda gg=gg: fsrc[:, gg, off:off + n], gg == 0, gg == 3, [B_wf, fB], psB[pF])
                                    for gg in range(4):
                                        mm(lambda pA=pA: ps[pA][:, :n], lambda gg=gg, dc=dc: wa[:, gg, dc * 128:(dc + 1) * 128],
                                           lambda gg=gg: osrc[:, gg, off:off + n], gg == 0, gg == 3, [B_wa, oB], psB[pA])
                                    for kc in range(8):
                                        mm(lambda pGf=pGf: ps[pGf][:, :n], lambda kc=kc, dc=dc: wg2[:, kc, dc * 128:(dc + 1) * 128],
                                           lambda kc=kc: hT[:, kc, :n], kc == 0, kc == 7, [B_wg2, B_hT], psB[pGf])
                                    for kc in range(8):
                                        mm(lambda pGa=pGa: ps[pGa][:, :n], lambda kc=kc, dc=dc: wg2[:, kc, D + dc * 128:D + (dc + 1) * 128],
                                           lambda kc=kc: hT[:, kc, :n], kc == 0, kc == 7, [B_wg2, B_hT], psB[pGa])
                                    i0, i1 = (dc % 2) * 2, (dc % 2) * 2 + 1
                                    P.op("act", lambda pGf=pGf, i0=i0: nc.scalar.activation(out=sg[i0][0][:, :n], in_=ps[pGf][:, :n], func=AF.Sigmoid),
                                         reads=[psB[pGf]], writes=[sg[i0][1]])
                                    P.op("act", lambda pGa=pGa, i1=i1: nc.scalar.activation(out=sg[i1][0][:, :n], in_=ps[pGa][:, :n], func=AF.Sigmoid),
                                         reads=[psB[pGa]], writes=[sg[i1][1]])
                                    P.op("dve", lambda pF=pF, i0=i0: nc.vector.tensor_tensor(out=tm[i0][0][:, :n], in0=ps[pF][:, :n], in1=sg[i0][0][:, :n], op=ALU.mult),
                                         reads=[psB[pF], sg[i0][1]], writes=[tm[i0][1]])
                                    P.op("dve", lambda pA=pA, i1=i1: nc.vector.tensor_tensor(out=tm[i1][0][:, :n], in0=ps[pA][:, :n], in1=sg[i1][0][:, :n], op=ALU.mult),
                                         reads=[psB[pA], sg[i1][1]], writes=[tm[i1][1]])
                                    P.op("pool", lambda i0=i0, i1=i1, dc=dc: nc.gpsimd.tensor_tensor(out=yT[:, dc, :n], in0=tm[i0][0][:, :n], in1=tm[i1][0][:, :n], op=ALU.add),
                                         reads=[tm[i0][1], tm[i1][1]], writes=[B_yT])
                                res, rb = ts["res"], ts["rb"]
                                for dc in range(8):
                                    pZ = nps()
                                    for kc in range(8):
                                        mm(lambda pZ=pZ: ps[pZ][:, :n], lambda kc=kc, dc=dc: wo[:, kc, dc * 128:(dc + 1) * 128],
                                           lambda kc=kc: yT[:, kc, :n], kc == 0, kc == 7, [B_wo, B_yT], psB[pZ])
                                    P.op("dve", lambda pZ=pZ, dc=dc, res=res, s=s: nc.vector.scalar_tensor_tensor(
                                        out=res[:, dc, off:off + n], in0=ps[pZ][:, :n], scalar=mod[:, l, 16 + dc, s:s + 1], in1=res[:, dc, off:off + n],
                                        op0=ALU.mult, op1=ALU.add), reads=[psB[pZ], B_modl[l], rb[(dc, g)]], writes=[rb[(dc, g)]])
                        P.barrier()

                stop_pt("mix%d" % l)
                with ExitStack() as ph:
                    nb = norm_bufs(ph)
                    NTOT = NT + (0 if last else CT)
                    h2, B_h2 = SB(ph, "h2", [128, 8, NTOT], BF16)
                    sets = [LAT] if last else [LAT, CTX]
                    tgl = []
                    for ts in sets:
                        for (g, off, n) in ts["tgs"]:
                            h2off = off if ts is LAT else NT
                            tgl.append((ts, g, off, n, h2off))
                    hB2 = {i: Buf("h2_%d" % i, True) for i in range(len(tgl))}
                    def emit_norm(ti):
                        ts, g, off, n, h2off = tgl[ti]
                        s = ts["s"]

                        class _V:
                            def __init__(self, o):
                                self.o = o

                            def __getitem__(self, k):
                                p, c, sl = k
                                return h2[p, c, self.o + (sl.start or 0):self.o + sl.stop]
                        norm_tile(nb, ts, g, off, n, lambda c, s=s: g2m[:, l, c, s:s + 1], mods(ts, 24), None, hB2[ti], [B_g2l[l], B_modl[l]],
                                  hout=lambda c, h2off=h2off, n=n: h2[:, c, h2off:h2off + n], sqb=(_V(h2off), hB2[ti]))
                    if last:
                        wr, B_wr = SB(ph, "wr", [128, 8, 8], BF16)
                        comb, B_comb = SB(ph, "comb", [128, 16, 8], F32)
                        combT, B_combT = SB(ph, "combT", [8, NT], BF16)
                        esel, B_esel = SB(ph, "esel", [8, 8, 128], BF16)
                        lg, B_lg = SB(ph, "lg", [128, 8], F32)
                        m1, B_m1 = SB(ph, "m1", [128, 4], F32)
                        e1, B_e1 = SB(ph, "e1", [128, 8], F32)
                        l2, B_l2 = SB(ph, "l2", [128, 8], F32)
                        wload(lambda: wr[:], B_wr, lambda: wr_d.ap().rearrange("(kc p) n -> p kc n", p=128))
                        ld(lambda: esel[:], B_esel, lambda: esel_d.ap().rearrange("p (e n) -> p e n", e=8))
                        B_combTi = {i: Buf("combT%d" % i, True) for i in range(4)}

                        def router_blk(blk):
                            pi = nps()
                            for kc in range(8):
                                mm(lambda pi=pi: ps[pi][:, 0:8], lambda kc=kc, blk=blk: h2[:, kc, blk * 128:(blk + 1) * 128],
                                   lambda kc=kc: wr[:, kc, :], kc == 0, kc == 7, [hB2[blk // 4], B_wr], psB[pi])
                            V = nc.vector
                            P.op("dve", lambda pi=pi: V.tensor_copy(out=lg[:], in_=ps[pi][:, 0:8]), reads=[psB[pi]], writes=[B_lg])
                            P.op("dve", lambda: V.reduce_max(out=m1[:, 0:1], in_=lg[:], axis=AX.X), reads=[B_lg], writes=[B_m1])
                            P.op("dve", lambda: V.tensor_scalar(out=e1[:], in0=lg[:], scalar1=m1[:, 0:1], scalar2=None, op0=ALU.is_equal), reads=[B_lg, B_m1], writes=[B_e1])
                            P.op("dve", lambda: V.scalar_tensor_tensor(out=l2[:], in0=e1[:], scalar=-1e30, in1=lg[:], op0=ALU.mult, op1=ALU.add), reads=[B_e1, B_lg], writes=[B_l2])
                            P.op("dve", lambda: V.reduce_max(out=m1[:, 1:2], in_=l2[:], axis=AX.X), reads=[B_l2, B_m1], writes=[B_m1])
                            P.op("dve", lambda: V.tensor_scalar(out=e1[:], in0=lg[:], scalar1=m1[:, 1:2], scalar2=None, op0=ALU.is_ge), reads=[B_lg, B_m1, B_l2], writes=[B_e1])
                            P.op("dve", lambda: V.tensor_scalar(out=l2[:], in0=lg[:], scalar1=m1[:, 0:1], scalar2=None, op0=ALU.subtract), reads=[B_lg, B_m1, B_e1], writes=[B_l2])
                            P.op("act", lambda: nc.scalar.activation(out=l2[:], in_=l2[:], func=AF.Exp), reads=[B_l2], writes=[B_l2])
                            P.op("dve", lambda: V.tensor_tensor(out=l2[:], in0=l2[:], in1=e1[:], op=ALU.mult), reads=[B_l2, B_e1], writes=[B_l2])
                            P.op("dve", lambda: V.reduce_sum(out=m1[:, 2:3], in_=l2[:], axis=AX.X), reads=[B_l2, B_m1], writes=[B_m1])
                            P.op("dve", lambda: V.reciprocal(out=m1[:, 3:4], in_=m1[:, 2:3]), reads=[B_m1], writes=[B_m1])
                            P.op("dve", lambda blk=blk: V.tensor_scalar(out=comb[:, blk, :], in0=l2[:], scalar1=m1[:, 3:4], scalar2=None, op0=ALU.mult),
                                 reads=[B_l2, B_m1], writes=[B_comb])
                            pj = nps()
                            P.op("pe", lambda pj=pj, blk=blk: nc.tensor.transpose(out=ps[pj][0:8, 0:128], in_=comb[:, blk, :], identity=ident[:]),
                                 reads=[B_comb, B_ident], writes=[psB[pj]])
                            P.op("dve", lambda pj=pj, blk=blk: V.tensor_copy(out=combT[:, blk * 128:(blk + 1) * 128], in_=ps[pj][0:8, 0:128]),
                                 reads=[psB[pj]], writes=[B_combTi[blk // 4]])
                    emitted = [0]

                    def ensure_norm(k):
                        while emitted[0] <= min(k, len(tgl) - 1):
                            ti = emitted[0]
                            emitted[0] += 1
                            emit_norm(ti)
                            if last:
                                for blk in range(4 * ti, 4 * ti + 4):
                                    router_blk(blk)
                    ensure_norm(1)
                    if last:
                        units = []
                        for e in range(8):
                            for pc in range(7):
                                units.append((e, 4, (lambda e=e, pc=pc: wge_d[e, :, pc * 512:(pc + 1) * 512]),
                                              (lambda e=e, pc=pc: wue_d[e, :, pc * 512:(pc + 1) * 512]),
                                              (lambda e=e, pc=pc: wde_d[e, pc * 512:(pc + 1) * 512, :])))
                    else:
                        units = []
                        for pc in range(6):
                            nf_ = 4 if pc < 5 else 2
                            units.append((None, nf_, (lambda pc=pc, nf_=nf_: wgd_d[:, pc * 512:pc * 512 + nf_ * 128]),
                                          (lambda pc=pc, nf_=nf_: wud_d[:, pc * 512:pc * 512 + nf_ * 128]),
                                          (lambda pc=pc, nf_=nf_: wdd_d[pc * 512:pc * 512 + nf_ * 128, :])))
                    wgu = [SB(ph, "wgu%d" % i, [128, 8, 2, 512], BF16) for i in range(2)]
                    wdn = [SB(ph, "wdn%d" % i, [128, 4, D], BF16) for i in range(2)]
                    aT = [SB(ph, "aT%d" % i, [128, 4, 512], BF16) for i in range(2)]

                    def uload(ui):
                        e, nf, gsrc, usrc_, dsrc = units[ui]
                        wt, wb = wgu[ui % 2]
                        dt_, db = wdn[ui % 2]
                        wload(lambda: wt[:, :, 0, 0:nf * 128], wb, lambda: gsrc().rearrange("(kc p) n -> p kc n", p=128))
                        wload(lambda: wt[:, :, 1, 0:nf * 128], wb, lambda: usrc_().rearrange("(kc p) n -> p kc n", p=128))
                        wload(lambda: dt_[:, 0:nf, :], db, lambda: dsrc().rearrange("(kc p) n -> p kc n", p=128))
                    uload(0)
                    sgl = [SB(ph, "fsg%d" % i, [128, 512], F32) for i in range(2)]
                    ftm = [SB(ph, "ftm%d" % i, [128, 512], F32) for i in range(2)]
                    cbc = [SB(ph, "cbc%d" % i, [128, 512], F32) for i in range(2)]
                    aq = [0]
                    fq = [0]
                    mstep = [0]
                    if not last:
                        wm1 = SB(ph, "wm1", [128, 8, 512], BF16)
                        pslist[0] = [0, 1, 2, 3, 4, 5, 6]
                        mpi1 = 7
                    for ui, (e, nf, gsrc, usrc_, dsrc) in enumerate(units):
                        wt, wb = wgu[ui % 2]
                        dt_, db = wdn[ui % 2]
                        if ui + 1 < len(units):
                            uload(ui + 1)
                        pend = None

                        def down(ti, at, ab):
                            ts, g, off, n, h2off = tgl[ti]
                            res, rb, s = ts["res"], ts["rb"], ts["s"]
                            for dc in range(8):
                                pY = nps()
                                for f in range(nf):
                                    mm(lambda pY=pY: ps[pY][:, :n], lambda f=f, dc=dc: dt_[:, f, dc * 128:(dc + 1) * 128],
                                       lambda f=f: at[:, f, :n], f == 0, f == nf - 1, [db, ab], psB[pY])
                                P.op("dve", lambda pY=pY, dc=dc: nc.vector.scalar_tensor_tensor(
                                    out=res[:, dc, off:off + n], in0=ps[pY][:, :n], scalar=mod[:, l, 40 + dc, s:s + 1], in1=res[:, dc, off:off + n],
                                    op0=ALU.mult, op1=ALU.add), reads=[psB[pY], B_modl[l], rb[(dc, g)]], writes=[rb[(dc, g)]])

                        for ti, (ts, g, off, n, h2off) in enumerate(tgl):
                            ensure_norm(ti + 2)
                            at, ab = aT[aq[0] % 2]
                            aq[0] += 1
                            if e is not None:
                                pc_ = nps()
                                ct_, cb_ = cbc[ti % 2]
                                mm(lambda pc_=pc_: ps[pc_][:, :n], lambda e=e: esel[:, e, :], lambda off=off, n=n: combT[:, off:off + n], True, True, [B_esel, B_combTi[ti]], psB[pc_])
                                P.op("dve", lambda pc_=pc_, ct_=ct_: nc.vector.tensor_copy(out=ct_[:, :n], in_=ps[pc_][:, :n]), reads=[psB[pc_]], writes=[cb_])
                            for f in range(nf):
                                pG, pU = nps(), nps()
                                for kc in range(8):
                                    mm(lambda pG=pG: ps[pG][:, :n], lambda kc=kc, f=f: wt[:, kc, 0, f * 128:(f + 1) * 128],
                                       lambda kc=kc: h2[:, kc, h2off:h2off + n], kc == 0, kc == 7, [wb, hB2[ti]], psB[pG])
                                for kc in range(8):
                                    mm(lambda pU=pU: ps[pU][:, :n], lambda kc=kc, f=f: wt[:, kc, 1, f * 128:(f + 1) * 128],
                                       lambda kc=kc: h2[:, kc, h2off:h2off + n], kc == 0, kc == 7, [wb, hB2[ti]], psB[pU])
                                st2, sb2 = sgl[fq[0] % 2]
                                ft2, fb2 = ftm[fq[0] % 2]
                                fq[0] += 1
                                P.op("act", lambda pG=pG, st2=st2: nc.scalar.activation(out=st2[:, :n], in_=ps[pG][:, :n], func=AF.Silu),
                                     reads=[psB[pG]], writes=[sb2])
                                if e is None:
                                    P.op("dve", lambda pU=pU, st2=st2, at=at, f=f: nc.vector.tensor_tensor(out=at[:, f, :n], in0=ps[pU][:, :n], in1=st2[:, :n], op=ALU.mult),
                                         reads=[psB[pU], sb2], writes=[ab])
                                else:
                                    P.op("dve", lambda pU=pU, st2=st2, ft2=ft2: nc.vector.tensor_tensor(out=ft2[:, :n], in0=ps[pU][:, :n], in1=st2[:, :n], op=ALU.mult),
                                         reads=[psB[pU], sb2], writes=[fb2])
                                    P.op("pool", lambda ft2=ft2, ct_=ct_, at=at, f=f: nc.gpsimd.tensor_tensor(out=at[:, f, :n], in0=ft2[:, :n], in1=ct_[:, :n], op=ALU.mult),
                                         reads=[fb2, cb_], writes=[ab])
                            if pend is not None:
                                down(*pend)
                            pend = (ti, at, ab)
                            if not last and mstep[0] < 12:
                                mods_piece(1, mstep[0], wm1, mpi1)
                                mstep[0] += 1
                        down(*pend)
                    if not last:
                        while mstep[0] < 12:
                            mods_piece(1, mstep[0], wm1, mpi1)
                            mstep[0] += 1
                        mods_fin(1, mpi1)
                        pslist[0] = list(range(8))
                    P.barrier()
                stop_pt("ffn%d" % l)

            P.skip = False
            P.final = True
            with ExitStack() as ph:
                nb = norm_bufs(ph)
                oT, B_oT = SB(ph, "oT", [128, 8, 512], F32)
                sqo = SB(ph, "sqo", [128, 8, 512], BF16)
                ost = [SB(ph, "ost%d" % i, [128, D], F32) for i in range(2)]
                B_out = Buf("out")
                for (g, off, n) in LAT["tgs"]:
                    norm_tile(nb, LAT, g, off, n, lambda c: fing[:, c:c + 1], None, oT, B_oT, [B_fing], sqb=sqo)
                    for b in range(4):
                        gb = g * 4 + b
                        o_t, o_b = ost[gb % 2]
                        for half in range(2):
                            pi = nps()
                            for c in range(4):
                                cc = half * 4 + c
                                P.op("pe", lambda pi=pi, c=c, cc=cc, b=b: nc.tensor.transpose(
                                    out=ps[pi][:, c * 128:(c + 1) * 128], in_=oT[:, cc, b * 128:(b + 1) * 128], identity=ident[:]),
                                    reads=[B_oT, B_ident], writes=[psB[pi]])
                            P.op("dve", lambda pi=pi, o_t=o_t, half=half: nc.vector.tensor_copy(out=o_t[:, half * 512:(half + 1) * 512], in_=ps[pi][:]),
                                 reads=[psB[pi]], writes=[o_b])
                        P.op("sp", lambda o_t=o_t, gb=gb: nc.sync.dma_start(out=out_d[gb * 128:(gb + 1) * 128, :], in_=o_t[:]),
                             reads=[o_b], writes=[B_out], dma=True)
                if not P.dry:
                    for q in ("sp",):
                        m = P.dma_no[q]
                        for i in range(NDMASEM):
                            cnt = (m - i + NDMASEM - 1) // NDMASEM if m > i else 0
                            if cnt > 0:
                                P._wait("sp", (q, i), 16 * cnt)
    return nc


_CACHE = {}


def _consts():
    if "c" in _CACHE:
        return _CACHE["c"]
    c = {}
    c["ident"] = np.eye(128, dtype=np.float32)
    _a = 2 * np.pi * (np.outer(np.arange(128), np.arange(128)) % 128) / 128
    c["c128"] = np.concatenate([np.cos(_a), -np.sin(_a)], axis=1).astype(NPBF)
    c["ones1024"] = np.full((128, 128), 1.0 / 1024, dtype=NPBF)
    d = np.arange(64)
    partner = np.where((d % 32) < 16, d + 16, d - 16)
    c["partner"] = partner
    sign = np.where((d % 32) < 16, -1.0, 1.0)
    inv = 10000.0 ** (-np.arange(16, dtype=np.float64) / 16)
    c["rope"] = (sign, inv)
    kq = np.arange(128)
    mprev = (kq[:, None] >= kq[None, :]).astype(np.float32)
    mnext = (kq[:, None] <= kq[None, :]).astype(np.float32)
    c["mprev"] = np.tile(mprev, (1, 4))
    c["mnext"] = np.tile(mnext, (1, 4))
    ch = np.arange(128)
    ang = 2 * np.pi * ((np.outer(ch, ch)) % 128) / 128
    c["ccsc"] = np.concatenate([np.cos(ang), np.sin(ang)], axis=1) / np.sqrt(128.0)
    n = np.arange(CT)
    ang = 2 * np.pi * ((np.outer(n, n)) % CT) / CT
    c["tabcc"] = (np.cos(ang) / np.sqrt(CT)).astype(NPBF)
    c["tabsc"] = (-np.sin(ang) / np.sqrt(CT)).astype(NPBF)
    es = np.zeros((8, 8, 128), np.float32)
    for e in range(8):
        es[e, e, :] = 1.0
    c["esel"] = es.reshape(8, 1024).astype(NPBF)
    for j in range(4):
        n2 = np.arange(64, dtype=np.int64)
        k1 = np.arange(128, dtype=np.int64)
        k2 = np.arange(16, dtype=np.int64) + 16 * j
        kk = k1[:, None] + 128 * k2[None, :]
        ph_ = 2 * np.pi * ((n2[:, None, None] * kk[None, :, :]) % T).astype(np.float64) / T
        cph, sph = np.cos(ph_) / np.sqrt(T), np.sin(ph_) / np.sqrt(T)
        tb = np.zeros((128, 128, 32), np.float64)
        tb[:64, :, :16] = cph
        tb[64:, :, :16] = sph
        tb[:64, :, 16:] = -sph
        tb[64:, :, 16:] = cph
        c["tab23_%d" % j] = tb.reshape(128, 128 * 32).astype(NPBF)
        t = np.arange(NT) + NT * j
        rows = (t // 64).astype(np.float64)
        cols = (t % 64).astype(np.float64)
        dd = np.arange(128) % 64
        fi = dd % 16
        pos = np.where((dd < 32)[:, None], rows[None, :], cols[None, :])
        a = pos * inv[fi][:, None]
        c["cos%d" % j] = np.cos(a).astype(NPBF)
        c["sin%d" % j] = (np.sin(a) * sign[dd][:, None]).astype(NPBF)
    _CACHE["c"] = c
    return c


def _fm(v, nch):
    return np.ascontiguousarray(np.asarray(v, np.float32).reshape(nch, 128).T)


def kernel(x, c, ctx, c_ctx, w_mod, b_mod, norm1_g, norm2_g, w_in, sink, w_fourier, w_attn, w_out,
           w_gate_d, w_up_d, w_down_d, w_router, w_gate_e, w_up_e, w_down_e, final_g):
    K = _consts()
    f = lambda a: np.ascontiguousarray(np.asarray(a, dtype=np.float32))
    x, c, ctx, c_ctx = f(x), f(c), f(ctx), f(c_ctx)
    w_in = f(w_in)
    partner = K["partner"]
    qcols = np.arange(512)
    qpcols = (qcols // 64) * 64 + partner[qcols % 64]
    kcols = []
    for h in range(2):
        kcols += list(512 + 512 + h * 64 + np.arange(64)) * 2
    kpcols = []
    for h in range(2):
        kpcols += list(512 + 512 + h * 64 + partner) * 2
    cols = np.concatenate([np.arange(512), np.array(kcols), np.array(kpcols), 1152 + np.arange(128),
                           512 + qcols, 512 + qpcols, 1280 + np.arange(2048)]).astype(np.int64)
    assert cols.shape[0] == NCEXT
    w_in_ext = np.ascontiguousarray(w_in[:, :, cols])
    bmod = np.stack([np.repeat(_fm(b_mod[l], 48)[:, :, None], 2, axis=2).reshape(128, 96) for l in range(2)], axis=1).reshape(128, 192)
    n1g = np.stack([np.repeat(_fm(norm1_g[l], 8)[:, :, None], 2, axis=2).reshape(128, 16) for l in range(2)], axis=1).reshape(128, 32)
    n2g = np.stack([np.repeat(_fm(norm2_g[l], 8)[:, :, None], 2, axis=2).reshape(128, 16) for l in range(2)], axis=1).reshape(128, 32)
    fing = _fm(final_g, 8)
    sinkb = np.ascontiguousarray(np.broadcast_to(f(sink).reshape(1, 16), (128, 16)))
    shared = dict(w_mod=f(w_mod), bmod=np.ascontiguousarray(bmod), n1g=np.ascontiguousarray(n1g), n2g=np.ascontiguousarray(n2g),
                  fing=fing, w_in_ext=w_in_ext, sinkb=sinkb, w_fourier=f(w_fourier), w_attn=f(w_attn), w_out=f(w_out),
                  w_gate_d=f(w_gate_d)[0], w_up_d=f(w_up_d)[0], w_down_d=f(w_down_d)[0], w_router=f(w_router)[0],
                  w_gate_e=f(w_gate_e)[0], w_up_e=f(w_up_e)[0], w_down_e=f(w_down_e)[0],
                  ident=K["ident"], ones1024=K["ones1024"], tabcc=K["tabcc"], tabsc=K["tabsc"],
                  ccsc=K["ccsc"].astype(NPBF), esel=K["esel"])
    in_maps = []
    zeros = np.zeros((128, 512), np.float32)
    for i in range(8):
        b, j = i // 4, i % 4
        m = dict(shared)
        m["x"] = np.ascontiguousarray(x[b, j * NT:(j + 1) * NT])
        m["ctx"] = np.ascontiguousarray(ctx[b])
        cv = np.stack([_fm(c[b], 8), _fm(c_ctx, 8)], axis=2).reshape(128, 16)
        m["cvec"] = np.ascontiguousarray(cv)
        m["ropecos"] = K["cos%d" % j]
        m["ropesin"] = K["sin%d" % j]
        m["masks"] = np.concatenate([K["mprev"], K["mnext"], K["mprev"] if j > 0 else zeros, K["mnext"] if j < 3 else zeros], axis=1).astype(NPBF)
        sl = np.zeros((128, 8), np.float32)
        if j > 0:
            sl[:, j - 1] = 1.0
        if j < 3:
            sl[:, 4 + j + 1] = 1.0
        m["sel"] = sl
        m["tab23"] = K["tab23_%d" % j]
        m["c128"] = K["c128"]
        in_maps.append(m)

    if "nc" not in _CACHE:
        nc0 = bass.Bass("TRN2", target_bir_lowering=False)
        P0 = Prog(nc0)
        build(nc0, P0)
        nc = bass.Bass("TRN2", target_bir_lowering=False)
        P1 = Prog(nc, sigset=P0.need)
        build(nc, P1)
        _CACHE["nc"] = nc
    nc = _CACHE["nc"]
    in_maps = [{k: v for k, v in m.items() if k in nc._mk_declared} for m in in_maps]
    res = run_bass_kernel_spmd(nc, in_maps, core_ids=list(range(8)))
    out = np.zeros((2, T, D), np.float32)
    for i in range(8):
        b, j = i // 4, i % 4
        out[b, j * NT:(j + 1) * NT] = np.asarray(res.results[i]["out"], dtype=np.float32)
    return out
```

```python
import os
from contextlib import ExitStack
import numpy as np
import ml_dtypes
import concourse.bass as bass
import concourse.mybir as mybir
from concourse.bass_utils import run_bass_kernel_spmd

F32 = mybir.dt.float32
BF16 = mybir.dt.bfloat16
AF = mybir.ActivationFunctionType
ALU = mybir.AluOpType
AX = mybir.AxisListType
NPBF = ml_dtypes.bfloat16

D = 1024
T = 8192
NT = 2048
CT = 256
NCEXT = 4224
O_U, O_K, O_V, O_Q, O_G = 0, 512, 1024, 1152, 2176
DFF = 2816
DFE = 3584
NDMASEM = 8
STOP = os.environ.get("MK_STOP", "")


class Buf:
    __slots__ = ("name", "w", "r", "phase")

    def __init__(self, name, phase=False):
        self.name = name
        self.w = None
        self.r = {}
        self.phase = phase


class Prog:
    COMPUTE = ("pe", "act", "dve", "pool")

    def __init__(self, nc, sigset=None):
        self.nc = nc
        self.dry = sigset is None
        self.sigset = sigset if sigset is not None else set()
        self.need = set()
        self.n = 0
        self.meta = []
        self.cnt = {e: 0 for e in self.COMPUTE}
        self.sigval = {}
        self.waited = {}
        self.dma_no = {"sp": 0, "pool": 0, "act": 0}
        self.PH = Buf("PHASE")
        self.eng = {"pe": nc.tensor, "act": nc.scalar, "dve": nc.vector, "pool": nc.gpsimd, "sp": nc.sync}
        self.sems = {}
        self.ncc = 0

    def alloc_sems(self, stack):
        for e in self.COMPUTE:
            self.sems[e] = stack.enter_context(self.nc.semaphore("s_" + e))
        for q in ("sp", "pool"):
            for i in range(NDMASEM):
                self.sems[(q, i)] = stack.enter_context(self.nc.semaphore("d_%s%d" % (q, i)))
        for i in range(8):
            self.sems[("cc", i)] = stack.enter_context(self.nc.semaphore("cc%d" % i))

    def _wait(self, eng, semkey, val):
        k = (eng, semkey)
        if self.waited.get(k, 0) >= val:
            return
        self.waited[k] = val
        self.eng[eng].wait_ge(self.sems[semkey], val)

    skip = False
    limit = int(os.environ.get("MK_LIMIT", "0")) or None
    final = False

    def op(self, eng, fn, reads=(), writes=(), dma=False, cc=False):
        if self.skip:
            return None
        if self.limit is not None and self.n >= self.limit and not self.final:
            return None
        if os.environ.get("MK_TRACE") and self.dry:
            import inspect
            fr = inspect.currentframe().f_back
            print("OP", self.n, eng, fr.f_lineno, "dma" if dma else ("cc" if cc else ""))
        idx = self.n
        self.n += 1
        reads = list(reads)
        writes = list(writes)
        if any(b.phase for b in reads) or any(b.phase for b in writes):
            if self.PH not in writes:
                reads.append(self.PH)
        deps = set()
        for b in reads:
            if b.w is not None:
                deps.add(b.w)
        for b in writes:
            if b.w is not None:
                deps.add(b.w)
            deps.update(b.r.values())
        deps.discard(idx)
        async_op = dma or cc
        rkey = ("a", idx) if async_op else eng
        for b in writes:
            b.w = idx
            b.r = {}
        for b in reads:
            if b not in writes:
                b.r[rkey] = idx
        self.meta.append((eng, async_op))
        real = []
        for dpt in deps:
            deng, dasync = self.meta[dpt]
            if (not dasync) and deng == eng and (not async_op) and eng == "pe":
                continue
            real.append(dpt)
        if self.dry:
            for dpt in real:
                self.need.add(dpt)
            return idx
        for dpt in sorted(real):
            semkey, val = self.sigval[dpt]
            self._wait(eng, semkey, val)
        if dma:
            m = self.dma_no[eng]
            self.dma_no[eng] = m + 1
            semkey = (eng, m % NDMASEM)
            if m >= NDMASEM:
                self._wait(eng, semkey, 16 * (m // NDMASEM))
            ins = fn()
            ins.then_inc(self.sems[semkey], 16)
            self.sigval[idx] = (semkey, 16 * (m // NDMASEM + 1))
        elif cc:
            semkey = ("cc", self.ncc)
            self.ncc += 1
            ins = fn()
            ins.then_inc(self.sems[semkey])
            self.sigval[idx] = (semkey, 1)
        else:
            ins = fn()
            if idx in self.sigset:
                self.cnt[eng] += 1
                ins.then_inc(self.sems[eng], 1)
                self.sigval[idx] = (eng, self.cnt[eng])
        return idx

    def barrier(self):
        self.op("dve", lambda: self.nc.vector.engine_nop(), writes=[self.PH])


def build(nc, P):
    declared = set()
    nc._mk_declared = declared

    def din(name, shape, dt=F32):
        declared.add(name)
        return nc.dram_tensor(name, list(shape), dt, kind="ExternalInput")

    x_d = din("x", [NT, D])
    ctx_d = din("ctx", [CT, D])
    cvec_d = din("cvec", [128, 16])
    wmod_d = din("w_mod", [2, D, 6 * D])
    bmod_d = din("bmod", [128, 2 * 96])
    n1g_d = din("n1g", [128, 32])
    n2g_d = din("n2g", [128, 32])
    fing_d = din("fing", [128, 8])
    win_d = din("w_in_ext", [2, D, NCEXT])
    sink_d = din("sinkb", [128, 16])
    wf_d = din("w_fourier", [2, 512, D])
    wa_d = din("w_attn", [2, 512, D])
    wo_d = din("w_out", [2, D, D])
    need_d = STOP in ("", "ffn0", "p1_1", "pa_1", "pb0_1", "mix1", "ffn1")
    need_e = STOP in ("", "ffn1")
    wgd_d = wud_d = wdd_d = wr_d = wge_d = wue_d = wde_d = None
    if need_d:
        wgd_d = din("w_gate_d", [D, DFF])
        wud_d = din("w_up_d", [D, DFF])
        wdd_d = din("w_down_d", [DFF, D])
    if need_e:
        wr_d = din("w_router", [D, 8])
        wge_d = din("w_gate_e", [8, D, DFE])
        wue_d = din("w_up_e", [8, D, DFE])
        wde_d = din("w_down_e", [8, DFE, D])
    ident_d = din("ident", [128, 128])
    ones_d = din("ones1024", [128, 128], BF16)
    cos_d = din("ropecos", [128, NT], BF16)
    sin_d = din("ropesin", [128, NT], BF16)
    masks_d = din("masks", [128, 4 * 512], BF16)
    sel_d = din("sel", [128, 8])
    c128_d = din("c128", [128, 256], BF16)
    tab23_d = din("tab23", [128, 128 * 32], BF16)
    s1_d = nc.dram_tensor("s1_scratch", [2, 128, 64, 512], BF16)
    B_s1 = Buf("s1")
    tabcc_d = din("tabcc", [CT, CT], BF16)
    tabsc_d = din("tabsc", [CT, CT], BF16)
    ccsc_d = din("ccsc", [128, 256], BF16)
    esel_d = din("esel", [8, 8 * 128], BF16)
    out_d = nc.dram_tensor("out", [NT, D], F32, kind="ExternalOutput")

    u_own = [nc.dram_tensor("u_own%d" % k, [1024, 512], BF16) for k in range(2)]
    u_all = [nc.dram_tensor("u_all%d" % k, [4096, 512], BF16) for k in range(2)]
    u_ctx = nc.dram_tensor("u_ctx", [CT, 512], BF16)
    halo_in = nc.dram_tensor("halo_in", [128, 832], BF16)
    halo_all = nc.dram_tensor("halo_all", [512, 832], BF16)
    B_uown, B_uall, B_uctx, B_hin, B_hall = Buf("uown"), Buf("uall"), Buf("uctx"), Buf("hin"), Buf("hall")

    with ExitStack() as st:
        block = st.enter_context(nc.Block())
        P.alloc_sems(st)

        sbn = [0]

        def SB(stack, name, shape, dt, phase=True):
            sbn[0] += 1
            return stack.enter_context(nc.sbuf_tensor("sb%d_%s" % (sbn[0], name), shape, dt)), Buf(name, phase)

        ps = [st.enter_context(nc.psum_tensor("ps%d" % i, [128, 512], F32)) for i in range(8)]
        psB = [Buf("ps%d" % i) for i in range(8)]
        psrr = [0]

        pslist = [list(range(8))]

        def nps():
            i = pslist[0][psrr[0] % len(pslist[0])]
            psrr[0] += 1
            return i

        XT, _ = SB(st, "XT", [128, 8, NT], F32, False)
        XB = {(c, g): Buf("XT%d_%d" % (c, g)) for c in range(8) for g in range(4)}
        XcT, _ = SB(st, "XcT", [128, 8, CT], F32, False)
        XcB = {(c, 0): Buf("XcT%d" % c) for c in range(8)}
        mod, _ = SB(st, "mod", [128, 2, 48, 2], F32, False)
        g1m, _ = SB(st, "g1m", [128, 2, 8, 2], F32, False)
        g2m, _ = SB(st, "g2m", [128, 2, 8, 2], F32, False)
        B_modl = [Buf("mod0"), Buf("mod1")]
        B_g1l = [Buf("g1m0"), Buf("g1m1")]
        B_g2l = [Buf("g2m0"), Buf("g2m1")]
        ident, B_ident = SB(st, "ident", [128, 128], F32, False)
        ones, B_ones = SB(st, "ones", [128, 128], BF16, False)
        epsb, B_eps = SB(st, "epsb", [128, 1], F32, False)
        esink, B_esink = SB(st, "esink", [128, 16], F32, False)
        fing, B_fing = SB(st, "fing", [128, 8], F32, False)
        masks, B_masks = SB(st, "masks", [128, 4, 512], BF16, False)
        sel, B_sel = SB(st, "sel", [128, 8], F32, False)
        ccsc, B_ccsc = SB(st, "ccsc", [128, 256], BF16, False)
        c128, B_c128 = SB(st, "c128", [128, 256], BF16, False)

        cv, B_cv = SB(st, "cv", [128, 16], F32, False)
        scb, B_scb = SB(st, "scb", [128, 8, 2], BF16, False)
        bm, B_bm = SB(st, "bm", [128, 2, 96], F32, False)
        ng1, B_ng1 = SB(st, "ng1", [128, 32], F32, False)
        ng2, B_ng2 = SB(st, "ng2", [128, 32], F32, False)
        tmpm, B_tmpm = SB(st, "tmpm", [128, 16], F32, False)
        LAT = dict(name="lat", res=XT, rb=XB, n=NT, s=0, tgs=[(g, g * 512, 512) for g in range(4)])
        CTX = dict(name="ctx", res=XcT, rb=XcB, n=CT, s=1, tgs=[(0, 0, CT)])

        def mm(o, lhsT, rhs, start, stop, reads, pw):
            P.op("pe", lambda: nc.tensor.matmul(o(), lhsT=lhsT(), rhs=rhs(), start=start, stop=stop),
                 reads=reads, writes=[pw])

        def stop_pt(tag):
            if STOP == tag:
                P.skip = True

        @block.sync
        def _(sync):
            def ld(dst, B, src):
                P.op("sp", lambda: nc.sync.dma_start(out=dst(), in_=src()), writes=[B], dma=True)
            ld(lambda: ident[:], B_ident, lambda: ident_d[:, :])
            ld(lambda: ones[:], B_ones, lambda: ones_d[:, :])
            ld(lambda: esink[:], B_esink, lambda: sink_d[:, :])
            ld(lambda: fing[:], B_fing, lambda: fing_d[:, :])
            ld(lambda: masks[:], B_masks, lambda: masks_d.ap().rearrange("p (m n) -> p m n", m=4))
            ld(lambda: sel[:], B_sel, lambda: sel_d[:, :])
            ld(lambda: ccsc[:], B_ccsc, lambda: ccsc_d[:, :])
            ld(lambda: c128[:], B_c128, lambda: c128_d[:, :])
            P.op("dve", lambda: nc.vector.memset(epsb[:], 1e-6), writes=[B_eps])
            P.op("act", lambda: nc.scalar.activation(out=esink[:], in_=esink[:], func=AF.Exp),
                 reads=[B_esink], writes=[B_esink])

            def mods_piece(l, piece, wmb, pi):
                wt, wb = wmb
                P.op("pool", lambda: nc.gpsimd.dma_start(
                    out=wt[:], in_=wmod_d[l, :, piece * 512:(piece + 1) * 512].rearrange("(kc p) n -> p kc n", p=128)),
                    writes=[wb], dma=True)
                for sub in range(4):
                    ch = piece * 4 + sub
                    for kc in range(8):
                        mm(lambda ch=ch: ps[pi][:, ch * 2:ch * 2 + 2],
                           lambda kc=kc, sub=sub: wt[:, kc, sub * 128:(sub + 1) * 128],
                           lambda kc=kc: scb[:, kc, :], kc == 0, kc == 7, [wb, B_scb], psB[pi])

            def mods_fin(l, pi):
                P.op("dve", lambda: nc.vector.tensor_tensor(
                    out=mod[:, l].rearrange("p c s -> p (c s)"), in0=ps[pi][:, 0:96], in1=bm[:, l, :], op=ALU.add),
                    reads=[psB[pi], B_bm], writes=[B_modl[l]])
                for (gm, Bg, ng, Bn, o) in ((g1m, B_g1l, ng1, B_ng1, 8), (g2m, B_g2l, ng2, B_ng2, 32)):
                    P.op("dve", lambda o=o: nc.vector.tensor_scalar(
                        out=tmpm[:], in0=mod[:, l, o:o + 8, :].rearrange("p c s -> p (c s)"), scalar1=1.0, scalar2=None, op0=ALU.add),
                        reads=[B_modl[l]], writes=[B_tmpm])
                    P.op("dve", lambda gm=gm, ng=ng: nc.vector.tensor_tensor(
                        out=gm[:, l].rearrange("p c s -> p (c s)"), in0=tmpm[:], in1=ng[:, l * 16:(l + 1) * 16], op=ALU.mult),
                        reads=[B_tmpm, Bn], writes=[Bg[l]])

            with ExitStack() as ph:
                xs = [SB(ph, "xs%d" % i, [128, D], F32) for i in range(2)]

                def load_T(src, nblk, res, rb):
                    for blk in range(nblk):
                        xt, xb = xs[blk % 2]
                        P.op("sp", lambda xt=xt, blk=blk: nc.sync.dma_start(out=xt[:], in_=src[blk * 128:(blk + 1) * 128, :]),
                             writes=[xb], dma=True)
                        for half in range(2):
                            pi = nps()
                            for c in range(4):
                                cc = half * 4 + c
                                P.op("pe", lambda xt=xt, pi=pi, c=c, cc=cc: nc.tensor.transpose(
                                    out=ps[pi][:, c * 128:(c + 1) * 128], in_=xt[:, cc * 128:(cc + 1) * 128], identity=ident[:]),
                                    reads=[xb, B_ident], writes=[psB[pi]])
                            g = (blk * 128) // 512
                            P.op("dve", lambda pi=pi, half=half, blk=blk: nc.vector.tensor_copy(
                                out=res[:, half * 4:(half + 1) * 4, blk * 128:(blk + 1) * 128],
                                in_=ps[pi][:].rearrange("p (c t) -> p c t", c=4)),
                                reads=[psB[pi]], writes=[rb[(half * 4 + c, g)] for c in range(4)])
                load_T(x_d, 16, XT, XB)
                load_T(ctx_d, 2, XcT, XcB)
                stop_pt("load")

                wm = [SB(ph, "wm%d" % i, [128, 8, 512], BF16) for i in range(2)]
                ld(lambda: cv[:], B_cv, lambda: cvec_d[:, :])
                ld(lambda: bm[:], B_bm, lambda: bmod_d.ap().rearrange("p (l n) -> p l n", l=2))
                ld(lambda: ng1[:], B_ng1, lambda: n1g_d[:, :])
                ld(lambda: ng2[:], B_ng2, lambda: n2g_d[:, :])
                P.op("act", lambda: nc.scalar.activation(out=cv[:], in_=cv[:], func=AF.Silu), reads=[B_cv], writes=[B_cv])
                P.op("dve", lambda: nc.vector.tensor_copy(out=scb[:], in_=cv[:].rearrange("p (k s) -> p k s", s=2)),
                     reads=[B_cv], writes=[B_scb])
                mpi0 = nps()
                for piece in range(12):
                    mods_piece(0, piece, wm[piece % 2], mpi0)
                mods_fin(0, mpi0)
                P.barrier()
                stop_pt("mods")

            def norm_tile(ph_bufs, ts, g, off, n, gm_ap, sh_ap, hT, hB, extra_reads, hout=None, sqb=None):
                _sq, _B_sq, rstd, B_rstd, tt = ph_bufs
                if hout is None:
                    hout = lambda c: hT[:, c, :n]
                sq, B_sq = sqb if sqb is not None else (hT, hB)
                res, rb = ts["res"], ts["rb"]
                for c in range(8):
                    P.op("act", lambda c=c: nc.scalar.activation(out=sq[:, c, :n], in_=res[:, c, off:off + n], func=AF.Square),
                         reads=[rb[(c, g)]], writes=[B_sq])
                pi = nps()
                for c in range(8):
                    mm(lambda: ps[pi][:, :n], lambda: ones[:], lambda c=c: sq[:, c, :n], c == 0, c == 7, [B_ones, B_sq], psB[pi])
                P.op("act", lambda: nc.scalar.activation(out=rstd[:, :n], in_=ps[pi][:, :n], func=AF.Sqrt, bias=epsb[:, 0:1], scale=1.0),
                     reads=[psB[pi], B_eps], writes=[B_rstd])
                P.op("dve", lambda: nc.vector.reciprocal(out=rstd[:, :n], in_=rstd[:, :n]), reads=[B_rstd], writes=[B_rstd])
                for c in range(8):
                    t, tb = tt[c % 2]
                    if sh_ap is None:
                        P.op("dve", lambda c=c: nc.vector.scalar_tensor_tensor(
                            out=hout(c), in0=res[:, c, off:off + n], scalar=gm_ap(c), in1=rstd[:, :n], op0=ALU.mult, op1=ALU.mult),
                            reads=[rb[(c, g)], B_rstd] + extra_reads, writes=[hB])
                    else:
                        P.op("dve", lambda c=c, t=t: nc.vector.scalar_tensor_tensor(
                            out=t[:, :n], in0=res[:, c, off:off + n], scalar=gm_ap(c), in1=rstd[:, :n], op0=ALU.mult, op1=ALU.mult),
                            reads=[rb[(c, g)], B_rstd] + extra_reads, writes=[tb])
                        P.op("act", lambda c=c, t=t: nc.scalar.activation(
                            out=hout(c), in_=t[:, :n], func=AF.Identity, bias=sh_ap(c), scale=1.0),
                            reads=[tb] + extra_reads, writes=[hB])

            def norm_bufs(ph):
                sq, B_sq = None, None
                rstd, B_rstd = SB(ph, "rstd", [128, 512], F32)
                tt = [SB(ph, "nt%d" % i, [128, 512], F32) for i in range(2)]
                return (sq, B_sq, rstd, B_rstd, tt)

            def wload(wt, wb, src):
                P.op("pool", lambda: nc.gpsimd.dma_start(out=wt(), in_=src()), writes=[wb], dma=True)

            for l in range(2):
                last = (l == 1)
                with ExitStack() as lay:
                    OT, B_OT = SB(lay, "OT", [128, 4, NT], BF16)
                    OcT, B_OcT = SB(lay, "OcT", [128, 4, CT], BF16)
                    FT, B_FT = SB(lay, "FT", [128, 4, NT], BF16)
                    FcT, B_FcT = SB(lay, "FcT", [128, 4, CT], BF16)
                    att = ExitStack()
                    KT, B_KT = SB(att, "KT", [128, 2, NT], BF16)
                    Vown, B_V = SB(att, "Vown", [128, 16, 2, 80], BF16)
                    KcT, B_KcT = SB(att, "KcT", [128, 2, CT], BF16)
                    Vc, B_Vc = SB(att, "Vc", [128, 2, 2, 80], BF16)
                    hp, B_hp = SB(att, "hp", [128, 832], BF16)
                    hn, B_hn = SB(att, "hn", [128, 832], BF16)
                    P.op("pool", lambda: nc.gpsimd.memset(KT[:], 0.0), writes=[B_KT])
                    P.op("pool", lambda: nc.gpsimd.memset(Vown[:], 1.0), writes=[B_V])
                    P.op("pool", lambda: nc.gpsimd.memset(Vc[:], 1.0), writes=[B_Vc])

                    def mods(ts, base):
                        s = ts["s"]
                        return lambda c: mod[:, l, base + c, s:s + 1]

                    with ExitStack() as ph:
                        nb = norm_bufs(ph)
                        hTs = [SB(ph, "hT%d" % i, [128, 8, 512], BF16) for i in range(2)]
                        hq = [0]
                        wu, B_wu = SB(ph, "wu", [128, 8, 512], BF16)
                        wk, B_wk = SB(ph, "wk", [128, 8, 512], BF16)
                        wv, B_wv = SB(ph, "wv", [128, 8, 128], BF16)
                        rc, B_rc = SB(ph, "rc", [128, NT], BF16)
                        rs, B_rs = SB(ph, "rs", [128, NT], BF16)
                        ust = [SB(ph, "ust%d" % i, [128, 512], BF16) for i in range(2)]
                        r1 = [SB(ph, "r1_%d" % i, [128, 512], F32) for i in range(2)]
                        r2 = [SB(ph, "r2_%d" % i, [128, 512], F32) for i in range(2)]
                        wsrc = lambda o, n: (lambda: win_d[l, :, o:o + n].rearrange("(kc p) n -> p kc n", p=128))
                        wload(lambda: wu[:], B_wu, wsrc(O_U, 512))
                        wload(lambda: wk[:], B_wk, wsrc(O_K, 512))
                        wload(lambda: wv[:], B_wv, wsrc(O_V, 128))
                        ld(lambda: rc[:], B_rc, lambda: cos_d[:, :])
                        ld(lambda: rs[:], B_rs, lambda: sin_d[:, :])
                        uq = [0]
                        for ts in ([CTX, LAT]):
                            isl = ts is LAT
                            for (g, off, n) in ts["tgs"]:
                                hT, B_hT = hTs[hq[0] % 2]
                                hq[0] += 1
                                norm_tile(nb, ts, g, off, n, lambda c, ts=ts: g1m[:, l, c, ts["s"]:ts["s"] + 1], mods(ts, 0), hT, B_hT, [B_g1l[l], B_modl[l]])
                                for b in range(n // 128):
                                    gb = off // 128 + b
                                    pi = nps()
                                    for kc in range(8):
                                        mm(lambda pi=pi: ps[pi][:, :], lambda kc=kc, b=b: hT[:, kc, b * 128:(b + 1) * 128],
                                           lambda kc=kc: wu[:, kc, :], kc == 0, kc == 7, [B_hT, B_wu], psB[pi])
                                    ut, ub = ust[uq[0] % 2]
                                    uq[0] += 1
                                    P.op("dve", lambda pi=pi, ut=ut: nc.vector.tensor_copy(out=ut[:], in_=ps[pi][:]), reads=[psB[pi]], writes=[ub])
                                    udst, uB = (u_own[gb // 8], B_uown) if isl else (u_ctx, B_uctx)
                                    gr = gb % 8 if isl else gb
                                    P.op("sp", lambda ut=ut, udst=udst, gr=gr: nc.sync.dma_start(out=udst[gr * 128:(gr + 1) * 128, :], in_=ut[:]),
                                         reads=[ub], writes=[uB], dma=True)
                                    pi = nps()
                                    for kc in range(8):
                                        mm(lambda pi=pi: ps[pi][:, 0:128], lambda kc=kc, b=b: hT[:, kc, b * 128:(b + 1) * 128],
                                           lambda kc=kc: wv[:, kc, :], kc == 0, kc == 7, [B_hT, B_wv], psB[pi])
                                    vdst, vB = (Vown, B_V) if isl else (Vc, B_Vc)
                                    P.op("dve", lambda pi=pi, vdst=vdst, gb=gb: nc.vector.tensor_copy(
                                        out=vdst[:, gb, :, 0:64], in_=ps[pi][:, 0:128].rearrange("p (h d) -> p h d", h=2)),
                                        reads=[psB[pi]], writes=[vB])
                                for h in range(2):
                                    pa = nps()
                                    for kc in range(8):
                                        mm(lambda pa=pa: ps[pa][0:64, :n], lambda kc=kc, h=h: wk[:, kc, h * 128:h * 128 + 64],
                                           lambda kc=kc: hT[:, kc, :n], kc == 0, kc == 7, [B_hT, B_wk], psB[pa])
                                    if not isl:
                                        P.op("dve", lambda pa=pa, h=h: nc.vector.tensor_copy(out=KcT[0:64, h, :], in_=ps[pa][0:64, :n]),
                                             reads=[psB[pa]], writes=[B_KcT])
                                        continue
                                    pb = nps()
                                    for kc in range(8):
                                        mm(lambda pb=pb: ps[pb][0:64, :n], lambda kc=kc, h=h: wk[:, kc, (2 + h) * 128:(2 + h) * 128 + 64],
                                           lambda kc=kc: hT[:, kc, :n], kc == 0, kc == 7, [B_hT, B_wk], psB[pb])
                                    t1, b1 = r1[h]
                                    t2, b2 = r2[h]
                                    P.op("dve", lambda pa=pa, t1=t1, off=off: nc.vector.tensor_tensor(out=t1[0:64, :], in0=ps[pa][0:64, :], in1=rc[0:64, off:off + 512], op=ALU.mult),
                                         reads=[psB[pa], B_rc], writes=[b1])
                                    P.op("dve", lambda pb=pb, t2=t2, off=off: nc.vector.tensor_tensor(out=t2[0:64, :], in0=ps[pb][0:64, :], in1=rs[0:64, off:off + 512], op=ALU.mult),
                                         reads=[psB[pb], B_rs], writes=[b2])
                                    P.op("pool", lambda t1=t1, t2=t2, h=h, off=off: nc.gpsimd.tensor_tensor(out=KT[0:64, h, off:off + 512], in0=t1[0:64, :], in1=t2[0:64, :], op=ALU.add),
                                         reads=[b1, b2], writes=[B_KT])
                        stop_pt("p1b")
                        P.op("sp", lambda: nc.sync.dma_start(out=halo_in[:, 0:256].rearrange("p (h t) -> p h t", h=2), in_=KT[:, :, 0:128]),
                             reads=[B_KT], writes=[B_hin], dma=True)
                        P.op("sp", lambda: nc.sync.dma_start(out=halo_in[:, 256:512].rearrange("p (h t) -> p h t", h=2), in_=KT[:, :, NT - 128:NT]),
                             reads=[B_KT], writes=[B_hin], dma=True)
                        P.op("sp", lambda: nc.sync.dma_start(out=halo_in[:, 512:672], in_=Vown[:, 0].rearrange("p h e -> p (h e)")),
                             reads=[B_V], writes=[B_hin], dma=True)
                        P.op("sp", lambda: nc.sync.dma_start(out=halo_in[:, 672:832], in_=Vown[:, 15].rearrange("p h e -> p (h e)")),
                             reads=[B_V], writes=[B_hin], dma=True)
                        stop_pt("p1c")
                        P.op("pool", lambda: nc.gpsimd.collective_compute("AllGather", ALU.bypass, replica_groups=[[0, 1, 2, 3], [4, 5, 6, 7]],
                                                                          ins=[halo_in.ap().opt()], outs=[halo_all.ap().opt()]),
                             reads=[B_hin], writes=[B_hall], cc=True)
                        for k2 in range(2):
                            P.op("pool", lambda k2=k2: nc.gpsimd.collective_compute("AllGather", ALU.bypass, replica_groups=[[0, 1, 2, 3], [4, 5, 6, 7]],
                                                                                    ins=[u_own[k2].ap().opt()], outs=[u_all[k2].ap().opt()]),
                                 reads=[B_uown], writes=[B_uall], cc=True)
                        stop_pt("p1d")
                        hall, B_hl = SB(ph, "hall", [128, 4, 832], BF16)
                        P.op("sp", lambda: nc.sync.dma_start(out=hall[:], in_=halo_all.ap().rearrange("(r p) n -> p r n", p=128)),
                             reads=[B_hall], writes=[B_hl], dma=True)
                        for (dst, Bd, so) in ((hp, B_hp, 0), (hn, B_hn, 4)):
                            P.op("dve", lambda dst=dst, so=so: nc.vector.tensor_scalar(out=dst[:, 0:832], in0=hall[:, 0, 0:832], scalar1=sel[:, so:so + 1], scalar2=None, op0=ALU.mult),
                                 reads=[B_hl, B_sel], writes=[Bd])
                            for r in range(1, 4):
                                P.op("dve", lambda dst=dst, so=so, r=r: nc.vector.scalar_tensor_tensor(
                                    out=dst[:, 0:832], in0=hall[:, r, 0:832], scalar=sel[:, so + r:so + r + 1], in1=dst[:, 0:832], op0=ALU.mult, op1=ALU.add),
                                    reads=[B_hl, B_sel], writes=[Bd])
                        P.barrier()
                        stop_pt("p1_%d" % l)

                    with ExitStack() as ph:
                        nb = norm_bufs(ph)
                        hT, B_hT = SB(ph, "hT", [128, 8, 512], BF16)
                        wq, B_wq = SB(ph, "wq", [128, 8, 1024], BF16)
                        rc, B_rc = SB(ph, "rc", [128, NT], BF16)
                        rs, B_rs = SB(ph, "rs", [128, NT], BF16)
                        QT, B_QT = SB(ph, "QT", [128, 8, 512], BF16)
                        r1 = [SB(ph, "r1_%d" % i, [128, 512], F32) for i in range(2)]
                        r2 = [SB(ph, "r2_%d" % i, [128, 512], F32) for i in range(2)]
                        pt = [SB(ph, "pt%d" % i, [128, 512], BF16) for i in range(10)]
                        pslist[0] = [0, 1, 2, 3]
                        qcount = [0]
                        Ons = [SB(ph, "On%d" % i, [128, 512], F32) for i in range(2)]
                        pendT = []
                        den, B_den = SB(ph, "den", [128, 8], F32)
                        wload(lambda: wq[:], B_wq, lambda: win_d[l, :, O_Q:O_Q + 1024].rearrange("(kc p) n -> p kc n", p=128))
                        ld(lambda: rc[:], B_rc, lambda: cos_d[:, :])
                        ld(lambda: rs[:], B_rs, lambda: sin_d[:, :])
                        eq = [0]
                        for ts in ([LAT] if last else [CTX, LAT]):
                            isl = ts is LAT
                            for (g, off, n) in ts["tgs"]:
                                norm_tile(nb, ts, g, off, n, lambda c, ts=ts: g1m[:, l, c, ts["s"]:ts["s"] + 1], mods(ts, 0), hT, B_hT, [B_g1l[l], B_modl[l]])
                                for hd in range(8):
                                    pa = nps()
                                    for kc in range(8):
                                        mm(lambda pa=pa: ps[pa][0:64, :n], lambda kc=kc, hd=hd: wq[:, kc, hd * 64:(hd + 1) * 64],
                                           lambda kc=kc: hT[:, kc, :n], kc == 0, kc == 7, [B_hT, B_wq], psB[pa])
                                    if not isl:
                                        P.op("dve", lambda pa=pa, hd=hd: nc.vector.tensor_copy(out=QT[0:64, hd, :n], in_=ps[pa][0:64, :n]),
                                             reads=[psB[pa]], writes=[B_QT])
                                        continue
                                    pb = nps()
                                    for kc in range(8):
                                        mm(lambda pb=pb: ps[pb][0:64, :n], lambda kc=kc, hd=hd: wq[:, kc, 512 + hd * 64:512 + (hd + 1) * 64],
                                           lambda kc=kc: hT[:, kc, :n], kc == 0, kc == 7, [B_hT, B_wq], psB[pb])
                                    t1, b1 = r1[hd % 2]
                                    t2, b2 = r2[hd % 2]
                                    P.op("dve", lambda pa=pa, t1=t1, off=off: nc.vector.tensor_tensor(out=t1[0:64, :], in0=ps[pa][0:64, :], in1=rc[0:64, off:off + 512], op=ALU.mult),
                                         reads=[psB[pa], B_rc], writes=[b1])
                                    P.op("dve", lambda pb=pb, t2=t2, off=off: nc.vector.tensor_tensor(out=t2[0:64, :], in0=ps[pb][0:64, :], in1=rs[0:64, off:off + 512], op=ALU.mult),
                                         reads=[psB[pb], B_rs], writes=[b2])
                                    P.op("pool", lambda t1=t1, t2=t2, hd=hd: nc.gpsimd.tensor_tensor(out=QT[0:64, hd, :], in0=t1[0:64, :], in1=t2[0:64, :], op=ALU.add),
                                         reads=[b1, b2], writes=[B_QT])
                                for qb in range(n // 128):
                                    gb = off // 128 + qb
                                    po2 = [4 + (qcount[0] % 2) * 2 + h for h in range(2)]
                                    On, B_On = Ons[qcount[0] % 2]
                                    qcount[0] += 1
                                    keysets = []
                                    for h in range(2):
                                        keys = []
                                        if isl:
                                            if gb == 0:
                                                keys.append((lambda h=h: hp[0:64, 256 + h * 128:256 + (h + 1) * 128],
                                                             lambda h=h: hp[:, 672 + h * 80:672 + h * 80 + 65], 2, [B_hp]))
                                            else:
                                                keys.append((lambda h=h, gb=gb: KT[0:64, h, (gb - 1) * 128:gb * 128],
                                                             lambda h=h, gb=gb: Vown[:, gb - 1, h, 0:65], 0, [B_KT, B_V]))
                                            keys.append((lambda h=h, gb=gb: KT[0:64, h, gb * 128:(gb + 1) * 128],
                                                         lambda h=h, gb=gb: Vown[:, gb, h, 0:65], None, [B_KT, B_V]))
                                            if gb == 15:
                                                keys.append((lambda h=h: hn[0:64, h * 128:(h + 1) * 128],
                                                             lambda h=h: hn[:, 512 + h * 80:512 + h * 80 + 65], 3, [B_hn]))
                                            else:
                                                keys.append((lambda h=h, gb=gb: KT[0:64, h, (gb + 1) * 128:(gb + 2) * 128],
                                                             lambda h=h, gb=gb: Vown[:, gb + 1, h, 0:65], 1, [B_KT, B_V]))
                                        for cb in range(2):
                                            keys.append((lambda h=h, cb=cb: KcT[0:64, h, cb * 128:(cb + 1) * 128],
                                                         lambda h=h, cb=cb: Vc[:, cb, h, 0:65], None, [B_KcT, B_Vc]))
                                        keysets.append(keys)
                                    nk = len(keysets[0])
                                    jobs = [(h, ki) for ki in range(nk) for h in range(2)]
                                    ptl = {}
                                    for ji, (h, ki) in enumerate(jobs):
                                        kap, vap, mk, kr = keysets[h][ki]
                                        pi = nps()
                                        mm(lambda pi=pi: ps[pi][:, :], lambda kap=kap: kap(),
                                           lambda h=h, qb=qb: QT[0:64, 4 * h:4 * h + 4, qb * 128:(qb + 1) * 128],
                                           True, True, [B_QT] + kr, psB[pi])
                                        ptt, pb_ = pt[h * 5 + ki]
                                        P.op("act", lambda pi=pi, ptt=ptt: nc.scalar.activation(out=ptt[:], in_=ps[pi][:], func=AF.Exp, scale=0.125),
                                             reads=[psB[pi]], writes=[pb_])
                                        if mk is not None:
                                            P.op("pool", lambda ptt=ptt, mk=mk: nc.gpsimd.tensor_tensor(out=ptt[:], in0=ptt[:], in1=masks[:, mk, :], op=ALU.mult),
                                                 reads=[pb_, B_masks], writes=[pb_])
                                        ptl[(h, ki)] = (ptt, pb_, vap, kr)
                                        if ji == 3 and pendT:
                                            pendT.pop(0)()
                                    for h in range(2):
                                        po = po2[h]
                                        for gi in range(4):
                                            col = gi * 128
                                            for ki in range(nk):
                                                ptt, pb_, vap, kr = ptl[(h, ki)]
                                                mm(lambda po=po, gi=gi: ps[po][:, gi * 80:gi * 80 + 65], lambda ptt=ptt, col=col: ptt[:, col:col + 128],
                                                   lambda vap=vap: vap(), ki == 0, ki == nk - 1, [pb_] + kr, psB[po])
                                    for h in range(2):
                                        po = po2[h]
                                        P.op("dve", lambda po=po, h=h: nc.vector.tensor_tensor(
                                            out=den[:, 4 * h:4 * h + 4], in0=ps[po][:, 0:320].rearrange("p (g e) -> p g e", e=80)[:, :, 64],
                                            in1=esink[:, l * 8 + 4 * h:l * 8 + 4 * h + 4], op=ALU.add),
                                            reads=[psB[po], B_esink], writes=[B_den])
                                    P.op("dve", lambda: nc.vector.reciprocal(out=den[:], in_=den[:]), reads=[B_den], writes=[B_den])
                                    for h in range(2):
                                        po = po2[h]
                                        for gi in range(4):
                                            P.op("dve", lambda po=po, gi=gi, h=h: nc.vector.tensor_scalar(
                                                out=On[:, (4 * h + gi) * 64:(4 * h + gi + 1) * 64], in0=ps[po][:, gi * 80:gi * 80 + 64],
                                                scalar1=den[:, 4 * h + gi:4 * h + gi + 1], scalar2=None, op0=ALU.mult),
                                                reads=[psB[po], B_den], writes=[B_On])
                                    def emit_T(On=On, B_On=B_On, gb=gb, isl=isl):
                                        pi = nps()
                                        for c4 in range(4):
                                            P.op("pe", lambda pi=pi, c4=c4: nc.tensor.transpose(out=ps[pi][:, c4 * 128:(c4 + 1) * 128], in_=On[:, c4 * 128:(c4 + 1) * 128], identity=ident[:]),
                                                 reads=[B_On, B_ident], writes=[psB[pi]])
                                        odst, oB = (OT, B_OT) if isl else (OcT, B_OcT)
                                        P.op("dve", lambda pi=pi, odst=odst: nc.vector.tensor_copy(
                                            out=odst[:, :, gb * 128:(gb + 1) * 128], in_=ps[pi][:].rearrange("p (c t) -> p c t", c=4)),
                                            reads=[psB[pi]], writes=[oB])
                                    pendT.append(emit_T)
                                while pendT:
                                    pendT.pop(0)()
                        pslist[0] = list(range(8))
                        P.barrier()
                        stop_pt("pa_%d" % l)
                    att.close()

                    with ExitStack() as ph:
                        ub = [SB(ph, "ub%d" % i, [128, 512], BF16) for i in range(3)]
                        tcb = [SB(ph, "tcb%d" % i, [128, 512], BF16) for i in range(3)]
                        tsb = [SB(ph, "tsb%d" % i, [128, 512], BF16) for i in range(3)]
                        yri, B_yri = SB(ph, "yri", [128, 8, 512], BF16)
                        dq = [0]
                        for ts in ([] if last else [CTX]):
                            isl = ts is LAT
                            uB = B_uall if isl else B_uctx
                            tc_d, ts_d = (tabcc_d, tabsc_d)
                            nblk = 64 if isl else 2
                            for (g, off, n) in ts["tgs"]:
                                acc = [nps() for _ in range(8)]
                                for nbk in range(nblk):
                                    i3 = dq[0] % 3
                                    dq[0] += 1
                                    (ut, uBb), (ct, cB), (st_, sB_) = ub[i3], tcb[i3], tsb[i3]
                                    if not isl:
                                        usrc, urow = u_ctx, nbk * 128
                                        P.op("sp", lambda ut=ut, usrc=usrc, urow=urow: nc.sync.dma_start(out=ut[:], in_=usrc[urow:urow + 128, :]),
                                             reads=[uB], writes=[uBb], dma=True)
                                        uap = lambda gg, ut=ut: ut[:, gg * 128:(gg + 1) * 128]

                                    P.op("sp", lambda ct=ct, tc_d=tc_d, nbk=nbk, off=off, n=n: nc.sync.dma_start(out=ct[:, :n], in_=tc_d[nbk * 128:(nbk + 1) * 128, off:off + n]),
                                         writes=[cB], dma=True)
                                    P.op("sp", lambda st_=st_, ts_d=ts_d, nbk=nbk, off=off, n=n: nc.sync.dma_start(out=st_[:, :n], in_=ts_d[nbk * 128:(nbk + 1) * 128, off:off + n]),
                                         writes=[sB_], dma=True)
                                    for gg in range(4):
                                        mm(lambda gg=gg: ps[acc[gg]][:, :n], lambda uap=uap, gg=gg: uap(gg), lambda ct=ct: ct[:, :n],
                                           nbk == 0, nbk == nblk - 1, [uBb, cB], psB[acc[gg]])
                                        mm(lambda gg=gg: ps[acc[4 + gg]][:, :n], lambda uap=uap, gg=gg: uap(gg), lambda st_=st_: st_[:, :n],
                                           nbk == 0, nbk == nblk - 1, [uBb, sB_], psB[acc[4 + gg]])
                                for j in range(8):
                                    P.op("dve", lambda j=j: nc.vector.tensor_copy(out=yri[:, j, :n], in_=ps[acc[j]][:, :n]), reads=[psB[acc[j]]], writes=[B_yri])
                                fdst, fB = (FT, B_FT) if isl else (FcT, B_FcT)
                                for gg in range(4):
                                    pi = nps()
                                    mm(lambda pi=pi: ps[pi][:, :n], lambda: ccsc[:, 0:128], lambda gg=gg: yri[:, gg, :n], True, False, [B_ccsc, B_yri], psB[pi])
                                    mm(lambda pi=pi: ps[pi][:, :n], lambda: ccsc[:, 128:256], lambda gg=gg: yri[:, 4 + gg, :n], False, True, [B_ccsc, B_yri], psB[pi])
                                    P.op("dve", lambda pi=pi, fdst=fdst, gg=gg, off=off, n=n: nc.vector.tensor_copy(out=fdst[:, gg, off:off + n], in_=ps[pi][:, :n]),
                                         reads=[psB[pi]], writes=[fB])
                        P.barrier()
                        stop_pt("pb0c_%d" % l)
                    with ExitStack() as ph:
                        Tb_ = [SB(ph, "ctT%d" % i, [128, 8, 512], BF16) for i in range(2)]
                        st1 = [SB(ph, "st1_%d" % i, [128, 2, 8, 512], BF16) for i in range(2)]
                        def ct_loads(n2c):
                            Tt, TB = Tb_[n2c % 2]
                            for r_ in range(4):
                                for k2_ in range(2):
                                    P.op("sp", lambda Tt=Tt, r_=r_, k2_=k2_, n2c=n2c: nc.sync.dma_start(
                                        out=Tt[r_ * 32 + k2_ * 16:r_ * 32 + k2_ * 16 + 16, :, :],
                                        in_=u_all[k2_][r_ * 1024:(r_ + 1) * 1024, :].rearrange("(a n) c -> a n c", n=64)[:, n2c * 8:(n2c + 1) * 8, :]),
                                        reads=[B_uall], writes=[TB], dma=True)
                        ct_loads(0)
                        for n2c in range(8):
                            Tt, TB = Tb_[n2c % 2]
                            s1t, s1B = st1[n2c % 2]
                            if n2c + 1 < 8:
                                ct_loads(n2c + 1)
                            for n2i in range(8):
                                pr, pi_ = nps(), nps()
                                mm(lambda pr=pr: ps[pr][:, :], lambda: c128[:, 0:128], lambda Tt=Tt, n2i=n2i: Tt[:, n2i, :], True, True, [B_c128, TB], psB[pr])
                                mm(lambda pi_=pi_: ps[pi_][:, :], lambda: c128[:, 128:256], lambda Tt=Tt, n2i=n2i: Tt[:, n2i, :], True, True, [B_c128, TB], psB[pi_])
                                P.op("act", lambda pr=pr, s1t=s1t, n2i=n2i: nc.scalar.copy(out=s1t[:, 0, n2i, :], in_=ps[pr][:, :]), reads=[psB[pr]], writes=[s1B])
                                P.op("dve", lambda pi_=pi_, s1t=s1t, n2i=n2i: nc.vector.tensor_copy(out=s1t[:, 1, n2i, :], in_=ps[pi_][:, :]), reads=[psB[pi_]], writes=[s1B])
                            for ri in range(2):
                                P.op("sp", lambda s1t=s1t, ri=ri, n2c=n2c: nc.sync.dma_start(out=s1_d[ri, :, n2c * 8:(n2c + 1) * 8, :], in_=s1t[:, ri, :, :]),
                                     reads=[s1B], writes=[B_s1], dma=True)
                        P.barrier()
                        stop_pt("pb0s1_%d" % l)
                    with ExitStack() as ph:
                        Yb, B_Yb = SB(ph, "ctY", [128, 4, 2, NT], BF16)
                        Rb_ = [SB(ph, "ctR%d" % i, [128, 8, 512], BF16) for i in range(2)]
                        t23, B_t23 = SB(ph, "t23", [128, 128, 32], BF16)
                        ld(lambda: t23[:], B_t23, lambda: tab23_d.ap().rearrange("p (k c) -> p k c", c=32))
                        acc = None
                        for k1c in range(16):
                            Rt, RB = Rb_[k1c % 2]
                            for ri in range(2):
                                P.op("sp", lambda Rt=Rt, ri=ri, k1c=k1c: nc.sync.dma_start(
                                    out=Rt[ri * 64:(ri + 1) * 64, :, :], in_=s1_d[ri, k1c * 8:(k1c + 1) * 8, :, :].rearrange("k n c -> n k c")),
                                    reads=[B_s1], writes=[RB], dma=True)
                            if k1c % 2 == 0:
                                acc = [nps() for _ in range(4)]
                            for k1i in range(8):
                                k1 = k1c * 8 + k1i
                                for gg in range(4):
                                    mm(lambda gg=gg, k1=k1: ps[acc[gg]][:, (k1 % 16) * 32:(k1 % 16) * 32 + 32],
                                       lambda Rt=Rt, k1i=k1i, gg=gg: Rt[:, k1i, gg * 128:(gg + 1) * 128],
                                       lambda k1=k1: t23[:, k1, :], True, True, [RB, B_t23], psB[acc[gg]])
                            if k1c % 2 == 1:
                                kbase = (k1c - 1) * 8
                                for gg in range(4):
                                    for ro in range(2):
                                        src = lambda gg=gg, ro=ro: ps[acc[gg]][:, :].rearrange("p (a r b) -> p a r b", r=2, b=16)[:, :, ro, :]
                                        dst = lambda gg=gg, ro=ro, kbase=kbase: Yb[:, gg, ro, :].rearrange("p (b a) -> p a b", a=128)[:, kbase:kbase + 16, :]
                                        if gg % 2 == 0:
                                            P.op("act", lambda src=src, dst=dst: nc.scalar.copy(out=dst(), in_=src()), reads=[psB[acc[gg]]], writes=[B_Yb])
                                        else:
                                            P.op("dve", lambda src=src, dst=dst: nc.vector.tensor_copy(out=dst(), in_=src()), reads=[psB[acc[gg]]], writes=[B_Yb])
                        for tg_ in range(4):
                            for gg in range(4):
                                pi = nps()
                                mm(lambda pi=pi: ps[pi][:, :], lambda: ccsc[:, 0:128], lambda gg=gg, tg_=tg_: Yb[:, gg, 0, tg_ * 512:(tg_ + 1) * 512], True, False, [B_ccsc, B_Yb], psB[pi])
                                mm(lambda pi=pi: ps[pi][:, :], lambda: ccsc[:, 128:256], lambda gg=gg, tg_=tg_: Yb[:, gg, 1, tg_ * 512:(tg_ + 1) * 512], False, True, [B_ccsc, B_Yb], psB[pi])
                                P.op("dve", lambda pi=pi, gg=gg, tg_=tg_: nc.vector.tensor_copy(out=FT[:, gg, tg_ * 512:(tg_ + 1) * 512], in_=ps[pi][:, :]),
                                     reads=[psB[pi]], writes=[B_FT])
                        P.barrier()
                        stop_pt("pb0_%d" % l)

                    with ExitStack() as ph:
                        nb = norm_bufs(ph)
                        hT, B_hT = SB(ph, "hT", [128, 8, 256], BF16)
                        wg2, B_wg2 = SB(ph, "wg2", [128, 8, 2048], BF16)
                        wf, B_wf = SB(ph, "wf", [128, 4, D], BF16)
                        wa, B_wa = SB(ph, "wa", [128, 4, D], BF16)
                        wo, B_wo = SB(ph, "wo", [128, 8, D], BF16)
                        yT, B_yT = SB(ph, "yT", [128, 8, 256], BF16)
                        sg = [SB(ph, "sg%d" % i, [128, 256], F32) for i in range(4)]
                        tm = [SB(ph, "tm%d" % i, [128, 256], F32) for i in range(4)]
                        wload(lambda: wf[:], B_wf, lambda: wf_d[l].rearrange("(kc p) n -> p kc n", p=128))
                        wload(lambda: wa[:], B_wa, lambda: wa_d[l].rearrange("(kc p) n -> p kc n", p=128))
                        wload(lambda: wg2[:], B_wg2, lambda: win_d[l, :, O_G:O_G + 2048].rearrange("(kc p) n -> p kc n", p=128))
                        wload(lambda: wo[:], B_wo, lambda: wo_d[l].rearrange("(kc p) n -> p kc n", p=128))
                        for ts in ([LAT] if last else [CTX, LAT]):
                            isl = ts is LAT
                            fsrc, fB = (FT, B_FT) if isl else (FcT, B_FcT)
                            osrc, oB = (OT, B_OT) if isl else (OcT, B_OcT)
                            s = ts["s"]
                            for (g, off, n) in [(o_ // 512, o_, 256) for o_ in range(0, ts["n"], 256)]:
                                norm_tile(nb, ts, g, off, n, lambda c, s=s: g1m[:, l, c, s:s + 1], mods(ts, 0), hT, B_hT, [B_g1l[l], B_modl[l]])
                                for dc in range(8):
                                    pF, pA, pGf, pGa = nps(), nps(), nps(), nps()
                                    for gg in range(4):
                                        mm(lambda pF=pF: ps[pF][:, :n], lambda gg=gg, dc=dc: wf[:, gg, dc * 128:(dc + 1) * 128],
                                           lambda gg=gg: fsrc[:, gg, off:off + n], gg == 0, gg == 3, [B_wf, fB], psB[pF])
                                    for gg in range(4):
                                        mm(lambda pA=pA: ps[pA][:, :n], lambda gg=gg, dc=dc: wa[:, gg, dc * 128:(dc + 1) * 128],
                                           lambda gg=gg: osrc[:, gg, off:off + n], gg == 0, gg == 3, [B_wa, oB], psB[pA])
                                    for kc in range(8):
                                        mm(lambda pGf=pGf: ps[pGf][:, :n], lambda kc=kc, dc=dc: wg2[:, kc, dc * 128:(dc + 1) * 128],
                                           lambda kc=kc: hT[:, kc, :n], kc == 0, kc == 7, [B_wg2, B_hT], psB[pGf])
                                    for kc in range(8):
                                        mm(lambda pGa=pGa: ps[pGa][:, :n], lambda kc=kc, dc=dc: wg2[:, kc, D + dc * 128:D + (dc + 1) * 128],
                                           lambda kc=kc: hT[:, kc, :n], kc == 0, kc == 7, [B_wg2, B_hT], psB[pGa])
                                    i0, i1 = (dc % 2) * 2, (dc % 2) * 2 + 1
                                    P.op("act", lambda pGf=pGf, i0=i0: nc.scalar.activation(out=sg[i0][0][:, :n], in_=ps[pGf][:, :n], func=AF.Sigmoid),
                                         reads=[psB[pGf]], writes=[sg[i0][1]])
                                    P.op("act", lambda pGa=pGa, i1=i1: nc.scalar.activation(out=sg[i1][0][:, :n], in_=ps[pGa][:, :n], func=AF.Sigmoid),
                                         reads=[psB[pGa]], writes=[sg[i1][1]])
                                    P.op("dve", lambda pF=pF, i0=i0: nc.vector.tensor_tensor(out=tm[i0][0][:, :n], in0=ps[pF][:, :n], in1=sg[i0][0][:, :n], op=ALU.mult),
                                         reads=[psB[pF], sg[i0][1]], writes=[tm[i0][1]])
                                    P.op("dve", lambda pA=pA, i1=i1: nc.vector.tensor_tensor(out=tm[i1][0][:, :n], in0=ps[pA][:, :n], in1=sg[i1][0][:, :n], op=ALU.mult),
                                         reads=[psB[pA], sg[i1][1]], writes=[tm[i1][1]])
                                    P.op("pool", lambda i0=i0, i1=i1, dc=dc: nc.gpsimd.tensor_tensor(out=yT[:, dc, :n], in0=tm[i0][0][:, :n], in1=tm[i1][0][:, :n], op=ALU.add),
                                         reads=[tm[i0][1], tm[i1][1]], writes=[B_yT])
                                res, rb = ts["res"], ts["rb"]
                                for dc in range(8):
                                    pZ = nps()
                                    for kc in range(8):
                                        mm(lambda pZ=pZ: ps[pZ][:, :n], lambda kc=kc, dc=dc: wo[:, kc, dc * 128:(dc + 1) * 128],
                                           lambda kc=kc: yT[:, kc, :n], kc == 0, kc == 7, [B_wo, B_yT], psB[pZ])
                                    P.op("dve", lambda pZ=pZ, dc=dc, res=res, s=s: nc.vector.scalar_tensor_tensor(
                                        out=res[:, dc, off:off + n], in0=ps[pZ][:, :n], scalar=mod[:, l, 16 + dc, s:s + 1], in1=res[:, dc, off:off + n],
                                        op0=ALU.mult, op1=ALU.add), reads=[psB[pZ], B_modl[l], rb[(dc, g)]], writes=[rb[(dc, g)]])
                        P.barrier()

                stop_pt("mix%d" % l)
                with ExitStack() as ph:
                    nb = norm_bufs(ph)
                    NTOT = NT + (0 if last else CT)
                    h2, B_h2 = SB(ph, "h2", [128, 8, NTOT], BF16)
                    sets = [LAT] if last else [LAT, CTX]
                    tgl = []
                    for ts in sets:
                        for (g, off, n) in ts["tgs"]:
                            h2off = off if ts is LAT else NT
                            tgl.append((ts, g, off, n, h2off))
                    hB2 = {i: Buf("h2_%d" % i, True) for i in range(len(tgl))}
                    def emit_norm(ti):
                        ts, g, off, n, h2off = tgl[ti]
                        s = ts["s"]

                        class _V:
                            def __init__(self, o):
                                self.o = o

                            def __getitem__(self, k):
                                p, c, sl = k
                                return h2[p, c, self.o + (sl.start or 0):self.o + sl.stop]
                        norm_tile(nb, ts, g, off, n, lambda c, s=s: g2m[:, l, c, s:s + 1], mods(ts, 24), None, hB2[ti], [B_g2l[l], B_modl[l]],
                                  hout=lambda c, h2off=h2off, n=n: h2[:, c, h2off:h2off + n], sqb=(_V(h2off), hB2[ti]))
                    if last:
                        wr, B_wr = SB(ph, "wr", [128, 8, 8], BF16)
                        comb, B_comb = SB(ph, "comb", [128, 16, 8], F32)
                        combT, B_combT = SB(ph, "combT", [8, NT], BF16)
                        esel, B_esel = SB(ph, "esel", [8, 8, 128], BF16)
                        lg, B_lg = SB(ph, "lg", [128, 8], F32)
                        m1, B_m1 = SB(ph, "m1", [128, 4], F32)
                        e1, B_e1 = SB(ph, "e1", [128, 8], F32)
                        l2, B_l2 = SB(ph, "l2", [128, 8], F32)
                        wload(lambda: wr[:], B_wr, lambda: wr_d.ap().rearrange("(kc p) n -> p kc n", p=128))
                        ld(lambda: esel[:], B_esel, lambda: esel_d.ap().rearrange("p (e n) -> p e n", e=8))
                        B_combTi = {i: Buf("combT%d" % i, True) for i in range(4)}

                        def router_blk(blk):
                            pi = nps()
                            for kc in range(8):
                                mm(lambda pi=pi: ps[pi][:, 0:8], lambda kc=kc, blk=blk: h2[:, kc, blk * 128:(blk + 1) * 128],
                                   lambda kc=kc: wr[:, kc, :], kc == 0, kc == 7, [hB2[blk // 4], B_wr], psB[pi])
                            V = nc.vector
                            P.op("dve", lambda pi=pi: V.tensor_copy(out=lg[:], in_=ps[pi][:, 0:8]), reads=[psB[pi]], writes=[B_lg])
                            P.op("dve", lambda: V.reduce_max(out=m1[:, 0:1], in_=lg[:], axis=AX.X), reads=[B_lg], writes=[B_m1])
                            P.op("dve", lambda: V.tensor_scalar(out=e1[:], in0=lg[:], scalar1=m1[:, 0:1], scalar2=None, op0=ALU.is_equal), reads=[B_lg, B_m1], writes=[B_e1])
                            P.op("dve", lambda: V.scalar_tensor_tensor(out=l2[:], in0=e1[:], scalar=-1e30, in1=lg[:], op0=ALU.mult, op1=ALU.add), reads=[B_e1, B_lg], writes=[B_l2])
                            P.op("dve", lambda: V.reduce_max(out=m1[:, 1:2], in_=l2[:], axis=AX.X), reads=[B_l2, B_m1], writes=[B_m1])
                            P.op("dve", lambda: V.tensor_scalar(out=e1[:], in0=lg[:], scalar1=m1[:, 1:2], scalar2=None, op0=ALU.is_ge), reads=[B_lg, B_m1, B_l2], writes=[B_e1])
                            P.op("dve", lambda: V.tensor_scalar(out=l2[:], in0=lg[:], scalar1=m1[:, 0:1], scalar2=None, op0=ALU.subtract), reads=[B_lg, B_m1, B_e1], writes=[B_l2])
                            P.op("act", lambda: nc.scalar.activation(out=l2[:], in_=l2[:], func=AF.Exp), reads=[B_l2], writes=[B_l2])
                            P.op("dve", lambda: V.tensor_tensor(out=l2[:], in0=l2[:], in1=e1[:], op=ALU.mult), reads=[B_l2, B_e1], writes=[B_l2])
                            P.op("dve", lambda: V.reduce_sum(out=m1[:, 2:3], in_=l2[:], axis=AX.X), reads=[B_l2, B_m1], writes=[B_m1])
                            P.op("dve", lambda: V.reciprocal(out=m1[:, 3:4], in_=m1[:, 2:3]), reads=[B_m1], writes=[B_m1])
                            P.op("dve", lambda blk=blk: V.tensor_scalar(out=comb[:, blk, :], in0=l2[:], scalar1=m1[:, 3:4], scalar2=None, op0=ALU.mult),
                                 reads=[B_l2, B_m1], writes=[B_comb])
                            pj = nps()
                            P.op("pe", lambda pj=pj, blk=blk: nc.tensor.transpose(out=ps[pj][0:8, 0:128], in_=comb[:, blk, :], identity=ident[:]),
                                 reads=[B_comb, B_ident], writes=[psB[pj]])
                            P.op("dve", lambda pj=pj, blk=blk: V.tensor_copy(out=combT[:, blk * 128:(blk + 1) * 128], in_=ps[pj][0:8, 0:128]),
                                 reads=[psB[pj]], writes=[B_combTi[blk // 4]])
                    emitted = [0]

                    def ensure_norm(k):
                        while emitted[0] <= min(k, len(tgl) - 1):
                            ti = emitted[0]
                            emitted[0] += 1
                            emit_norm(ti)
                            if last:
                                for blk in range(4 * ti, 4 * ti + 4):
                                    router_blk(blk)
                    ensure_norm(1)
                    if last:
                        units = []
                        for e in range(8):
                            for pc in range(7):
                                units.append((e, 4, (lambda e=e, pc=pc: wge_d[e, :, pc * 512:(pc + 1) * 512]),
                                              (lambda e=e, pc=pc: wue_d[e, :, pc * 512:(pc + 1) * 512]),
                                              (lambda e=e, pc=pc: wde_d[e, pc * 512:(pc + 1) * 512, :])))
                    else:
                        units = []
                        for pc in range(6):
                            nf_ = 4 if pc < 5 else 2
                            units.append((None, nf_, (lambda pc=pc, nf_=nf_: wgd_d[:, pc * 512:pc * 512 + nf_ * 128]),
                                          (lambda pc=pc, nf_=nf_: wud_d[:, pc * 512:pc * 512 + nf_ * 128]),
                                          (lambda pc=pc, nf_=nf_: wdd_d[pc * 512:pc * 512 + nf_ * 128, :])))
                    wgu = [SB(ph, "wgu%d" % i, [128, 8, 2, 512], BF16) for i in range(2)]
                    wdn = [SB(ph, "wdn%d" % i, [128, 4, D], BF16) for i in range(2)]
                    aT = [SB(ph, "aT%d" % i, [128, 4, 512], BF16) for i in range(2)]

                    def uload(ui):
                        e, nf, gsrc, usrc_, dsrc = units[ui]
                        wt, wb = wgu[ui % 2]
                        dt_, db = wdn[ui % 2]
                        wload(lambda: wt[:, :, 0, 0:nf * 128], wb, lambda: gsrc().rearrange("(kc p) n -> p kc n", p=128))
                        wload(lambda: wt[:, :, 1, 0:nf * 128], wb, lambda: usrc_().rearrange("(kc p) n -> p kc n", p=128))
                        wload(lambda: dt_[:, 0:nf, :], db, lambda: dsrc().rearrange("(kc p) n -> p kc n", p=128))
                    uload(0)
                    sgl = [SB(ph, "fsg%d" % i, [128, 512], F32) for i in range(2)]
                    ftm = [SB(ph, "ftm%d" % i, [128, 512], F32) for i in range(2)]
                    cbc = [SB(ph, "cbc%d" % i, [128, 512], F32) for i in range(2)]
                    aq = [0]
                    fq = [0]
                    mstep = [0]
                    if not last:
                        wm1 = SB(ph, "wm1", [128, 8, 512], BF16)
                        pslist[0] = [0, 1, 2, 3, 4, 5, 6]
                        mpi1 = 7
                    for ui, (e, nf, gsrc, usrc_, dsrc) in enumerate(units):
                        wt, wb = wgu[ui % 2]
                        dt_, db = wdn[ui % 2]
                        if ui + 1 < len(units):
                            uload(ui + 1)
                        pend = None

                        def down(ti, at, ab):
                            ts, g, off, n, h2off = tgl[ti]
                            res, rb, s = ts["res"], ts["rb"], ts["s"]
                            for dc in range(8):
                                pY = nps()
                                for f in range(nf):
                                    mm(lambda pY=pY: ps[pY][:, :n], lambda f=f, dc=dc: dt_[:, f, dc * 128:(dc + 1) * 128],
                                       lambda f=f: at[:, f, :n], f == 0, f == nf - 1, [db, ab], psB[pY])
                                P.op("dve", lambda pY=pY, dc=dc: nc.vector.scalar_tensor_tensor(
                                    out=res[:, dc, off:off + n], in0=ps[pY][:, :n], scalar=mod[:, l, 40 + dc, s:s + 1], in1=res[:, dc, off:off + n],
                                    op0=ALU.mult, op1=ALU.add), reads=[psB[pY], B_modl[l], rb[(dc, g)]], writes=[rb[(dc, g)]])

                        for ti, (ts, g, off, n, h2off) in enumerate(tgl):
                            ensure_norm(ti + 2)
                            at, ab = aT[aq[0] % 2]
                            aq[0] += 1
                            if e is not None:
                                pc_ = nps()
                                ct_, cb_ = cbc[ti % 2]
                                mm(lambda pc_=pc_: ps[pc_][:, :n], lambda e=e: esel[:, e, :], lambda off=off, n=n: combT[:, off:off + n], True, True, [B_esel, B_combTi[ti]], psB[pc_])
                                P.op("dve", lambda pc_=pc_, ct_=ct_: nc.vector.tensor_copy(out=ct_[:, :n], in_=ps[pc_][:, :n]), reads=[psB[pc_]], writes=[cb_])
                            for f in range(nf):
                                pG, pU = nps(), nps()
                                for kc in range(8):
                                    mm(lambda pG=pG: ps[pG][:, :n], lambda kc=kc, f=f: wt[:, kc, 0, f * 128:(f + 1) * 128],
                                       lambda kc=kc: h2[:, kc, h2off:h2off + n], kc == 0, kc == 7, [wb, hB2[ti]], psB[pG])
                                for kc in range(8):
                                    mm(lambda pU=pU: ps[pU][:, :n], lambda kc=kc, f=f: wt[:, kc, 1, f * 128:(f + 1) * 128],
                                       lambda kc=kc: h2[:, kc, h2off:h2off + n], kc == 0, kc == 7, [wb, hB2[ti]], psB[pU])
                                st2, sb2 = sgl[fq[0] % 2]
                                ft2, fb2 = ftm[fq[0] % 2]
                                fq[0] += 1
                                P.op("act", lambda pG=pG, st2=st2: nc.scalar.activation(out=st2[:, :n], in_=ps[pG][:, :n], func=AF.Silu),
                                     reads=[psB[pG]], writes=[sb2])
                                if e is None:
                                    P.op("dve", lambda pU=pU, st2=st2, at=at, f=f: nc.vector.tensor_tensor(out=at[:, f, :n], in0=ps[pU][:, :n], in1=st2[:, :n], op=ALU.mult),
                                         reads=[psB[pU], sb2], writes=[ab])
                                else:
                                    P.op("dve", lambda pU=pU, st2=st2, ft2=ft2: nc.vector.tensor_tensor(out=ft2[:, :n], in0=ps[pU][:, :n], in1=st2[:, :n], op=ALU.mult),
                                         reads=[psB[pU], sb2], writes=[fb2])
                                    P.op("pool", lambda ft2=ft2, ct_=ct_, at=at, f=f: nc.gpsimd.tensor_tensor(out=at[:, f, :n], in0=ft2[:, :n], in1=ct_[:, :n], op=ALU.mult),
                                         reads=[fb2, cb_], writes=[ab])
                            if pend is not None:
                                down(*pend)
                            pend = (ti, at, ab)
                            if not last and mstep[0] < 12:
                                mods_piece(1, mstep[0], wm1, mpi1)
                                mstep[0] += 1
                        down(*pend)
                    if not last:
                        while mstep[0] < 12:
                            mods_piece(1, mstep[0], wm1, mpi1)
                            mstep[0] += 1
                        mods_fin(1, mpi1)
                        pslist[0] = list(range(8))
                    P.barrier()
                stop_pt("ffn%d" % l)

            P.skip = False
            P.final = True
            with ExitStack() as ph:
                nb = norm_bufs(ph)
                oT, B_oT = SB(ph, "oT", [128, 8, 512], F32)
                sqo = SB(ph, "sqo", [128, 8, 512], BF16)
                ost = [SB(ph, "ost%d" % i, [128, D], F32) for i in range(2)]
                B_out = Buf("out")
                for (g, off, n) in LAT["tgs"]:
                    norm_tile(nb, LAT, g, off, n, lambda c: fing[:, c:c + 1], None, oT, B_oT, [B_fing], sqb=sqo)
                    for b in range(4):
                        gb = g * 4 + b
                        o_t, o_b = ost[gb % 2]
                        for half in range(2):
                            pi = nps()
                            for c in range(4):
                                cc = half * 4 + c
                                P.op("pe", lambda pi=pi, c=c, cc=cc, b=b: nc.tensor.transpose(
                                    out=ps[pi][:, c * 128:(c + 1) * 128], in_=oT[:, cc, b * 128:(b + 1) * 128], identity=ident[:]),
                                    reads=[B_oT, B_ident], writes=[psB[pi]])
                            P.op("dve", lambda pi=pi, o_t=o_t, half=half: nc.vector.tensor_copy(out=o_t[:, half * 512:(half + 1) * 512], in_=ps[pi][:]),
                                 reads=[psB[pi]], writes=[o_b])
                        P.op("sp", lambda o_t=o_t, gb=gb: nc.sync.dma_start(out=out_d[gb * 128:(gb + 1) * 128, :], in_=o_t[:]),
                             reads=[o_b], writes=[B_out], dma=True)
                if not P.dry:
                    for q in ("sp",):
                        m = P.dma_no[q]
                        for i in range(NDMASEM):
                            cnt = (m - i + NDMASEM - 1) // NDMASEM if m > i else 0
                            if cnt > 0:
                                P._wait("sp", (q, i), 16 * cnt)
    return nc


_CACHE = {}


def _consts():
    if "c" in _CACHE:
        return _CACHE["c"]
    c = {}
    c["ident"] = np.eye(128, dtype=np.float32)
    _a = 2 * np.pi * (np.outer(np.arange(128), np.arange(128)) % 128) / 128
    c["c128"] = np.concatenate([np.cos(_a), -np.sin(_a)], axis=1).astype(NPBF)
    c["ones1024"] = np.full((128, 128), 1.0 / 1024, dtype=NPBF)
    d = np.arange(64)
    partner = np.where((d % 32) < 16, d + 16, d - 16)
    c["partner"] = partner
    sign = np.where((d % 32) < 16, -1.0, 1.0)
    inv = 10000.0 ** (-np.arange(16, dtype=np.float64) / 16)
    c["rope"] = (sign, inv)
    kq = np.arange(128)
    mprev = (kq[:, None] >= kq[None, :]).astype(np.float32)
    mnext = (kq[:, None] <= kq[None, :]).astype(np.float32)
    c["mprev"] = np.tile(mprev, (1, 4))
    c["mnext"] = np.tile(mnext, (1, 4))
    ch = np.arange(128)
    ang = 2 * np.pi * ((np.outer(ch, ch)) % 128) / 128
    c["ccsc"] = np.concatenate([np.cos(ang), np.sin(ang)], axis=1) / np.sqrt(128.0)
    n = np.arange(CT)
    ang = 2 * np.pi * ((np.outer(n, n)) % CT) / CT
    c["tabcc"] = (np.cos(ang) / np.sqrt(CT)).astype(NPBF)
    c["tabsc"] = (-np.sin(ang) / np.sqrt(CT)).astype(NPBF)
    es = np.zeros((8, 8, 128), np.float32)
    for e in range(8):
        es[e, e, :] = 1.0
    c["esel"] = es.reshape(8, 1024).astype(NPBF)
    for j in range(4):
        n2 = np.arange(64, dtype=np.int64)
        k1 = np.arange(128, dtype=np.int64)
        k2 = np.arange(16, dtype=np.int64) + 16 * j
        kk = k1[:, None] + 128 * k2[None, :]
        ph_ = 2 * np.pi * ((n2[:, None, None] * kk[None, :, :]) % T).astype(np.float64) / T
        cph, sph = np.cos(ph_) / np.sqrt(T), np.sin(ph_) / np.sqrt(T)
        tb = np.zeros((128, 128, 32), np.float64)
        tb[:64, :, :16] = cph
        tb[64:, :, :16] = sph
        tb[:64, :, 16:] = -sph
        tb[64:, :, 16:] = cph
        c["tab23_%d" % j] = tb.reshape(128, 128 * 32).astype(NPBF)
        t = np.arange(NT) + NT * j
        rows = (t // 64).astype(np.float64)
        cols = (t % 64).astype(np.float64)
        dd = np.arange(128) % 64
        fi = dd % 16
        pos = np.where((dd < 32)[:, None], rows[None, :], cols[None, :])
        a = pos * inv[fi][:, None]
        c["cos%d" % j] = np.cos(a).astype(NPBF)
        c["sin%d" % j] = (np.sin(a) * sign[dd][:, None]).astype(NPBF)
    _CACHE["c"] = c
    return c


def _fm(v, nch):
    return np.ascontiguousarray(np.asarray(v, np.float32).reshape(nch, 128).T)


def kernel(x, c, ctx, c_ctx, w_mod, b_mod, norm1_g, norm2_g, w_in, sink, w_fourier, w_attn, w_out,
           w_gate_d, w_up_d, w_down_d, w_router, w_gate_e, w_up_e, w_down_e, final_g):
    K = _consts()
    f = lambda a: np.ascontiguousarray(np.asarray(a, dtype=np.float32))
    x, c, ctx, c_ctx = f(x), f(c), f(ctx), f(c_ctx)
    w_in = f(w_in)
    partner = K["partner"]
    qcols = np.arange(512)
    qpcols = (qcols // 64) * 64 + partner[qcols % 64]
    kcols = []
    for h in range(2):
        kcols += list(512 + 512 + h * 64 + np.arange(64)) * 2
    kpcols = []
    for h in range(2):
        kpcols += list(512 + 512 + h * 64 + partner) * 2
    cols = np.concatenate([np.arange(512), np.array(kcols), np.array(kpcols), 1152 + np.arange(128),
                           512 + qcols, 512 + qpcols, 1280 + np.arange(2048)]).astype(np.int64)
    assert cols.shape[0] == NCEXT
    w_in_ext = np.ascontiguousarray(w_in[:, :, cols])
    bmod = np.stack([np.repeat(_fm(b_mod[l], 48)[:, :, None], 2, axis=2).reshape(128, 96) for l in range(2)], axis=1).reshape(128, 192)
    n1g = np.stack([np.repeat(_fm(norm1_g[l], 8)[:, :, None], 2, axis=2).reshape(128, 16) for l in range(2)], axis=1).reshape(128, 32)
    n2g = np.stack([np.repeat(_fm(norm2_g[l], 8)[:, :, None], 2, axis=2).reshape(128, 16) for l in range(2)], axis=1).reshape(128, 32)
    fing = _fm(final_g, 8)
    sinkb = np.ascontiguousarray(np.broadcast_to(f(sink).reshape(1, 16), (128, 16)))
    shared = dict(w_mod=f(w_mod), bmod=np.ascontiguousarray(bmod), n1g=np.ascontiguousarray(n1g), n2g=np.ascontiguousarray(n2g),
                  fing=fing, w_in_ext=w_in_ext, sinkb=sinkb, w_fourier=f(w_fourier), w_attn=f(w_attn), w_out=f(w_out),
                  w_gate_d=f(w_gate_d)[0], w_up_d=f(w_up_d)[0], w_down_d=f(w_down_d)[0], w_router=f(w_router)[0],
                  w_gate_e=f(w_gate_e)[0], w_up_e=f(w_up_e)[0], w_down_e=f(w_down_e)[0],
                  ident=K["ident"], ones1024=K["ones1024"], tabcc=K["tabcc"], tabsc=K["tabsc"],
                  ccsc=K["ccsc"].astype(NPBF), esel=K["esel"])
    in_maps = []
    zeros = np.zeros((128, 512), np.float32)
    for i in range(8):
        b, j = i // 4, i % 4
        m = dict(shared)
        m["x"] = np.ascontiguousarray(x[b, j * NT:(j + 1) * NT])
        m["ctx"] = np.ascontiguousarray(ctx[b])
        cv = np.stack([_fm(c[b], 8), _fm(c_ctx, 8)], axis=2).reshape(128, 16)
        m["cvec"] = np.ascontiguousarray(cv)
        m["ropecos"] = K["cos%d" % j]
        m["ropesin"] = K["sin%d" % j]
        m["masks"] = np.concatenate([K["mprev"], K["mnext"], K["mprev"] if j > 0 else zeros, K["mnext"] if j < 3 else zeros], axis=1).astype(NPBF)
        sl = np.zeros((128, 8), np.float32)
        if j > 0:
            sl[:, j - 1] = 1.0
        if j < 3:
            sl[:, 4 + j + 1] = 1.0
        m["sel"] = sl
        m["tab23"] = K["tab23_%d" % j]
        m["c128"] = K["c128"]
        in_maps.append(m)

    if "nc" not in _CACHE:
        nc0 = bass.Bass("TRN2", target_bir_lowering=False)
        P0 = Prog(nc0)
        build(nc0, P0)
        nc = bass.Bass("TRN2", target_bir_lowering=False)
        P1 = Prog(nc, sigset=P0.need)
        build(nc, P1)
        _CACHE["nc"] = nc
    nc = _CACHE["nc"]
    in_maps = [{k: v for k, v in m.items() if k in nc._mk_declared} for m in in_maps]
    res = run_bass_kernel_spmd(nc, in_maps, core_ids=list(range(8)))
    out = np.zeros((2, T, D), np.float32)
    for i in range(8):
        b, j = i // 4, i % 4
        out[b, j * NT:(j + 1) * NT] = np.asarray(res.results[i]["out"], dtype=np.float32)
    return out
```
